# Optimizing a Trainium2 kernel written in Bass

```python
import math
import jax
import jax.numpy as jnp
from jax import lax
import numpy as np

D_MODEL = 4096
BATCH = 2
SEQ = 8192
DEPTH = 2

GRID_W = 64
CTX_LEN = 256
N_MIXERS = 2
N_LAYERS_A = (DEPTH + N_MIXERS - 1) // N_MIXERS
N_LAYERS_B = DEPTH // N_MIXERS
NA_HEAD_DIM = 128
NA_HEADS = D_MODEL // NA_HEAD_DIM
WIN_ROWS = 8
WIN_COLS = 16
DA_HEAD_DIM = 128
DA_HEADS = D_MODEL // (2 * DA_HEAD_DIM)
ROPE_BASE = 10000.0
Q_BLOCK = 128
N_EXPERTS = 16
EC_CAPACITY_FACTOR = 2
EXPERT_FF = D_MODEL // 4
N_MOD = 6
NORM_EPS = 1e-6
SUBLN_EPS = 1e-5
NEG_INF = -1e30

kernel_name = "hybrid_natten_diffattn_ecmoe_dit"


def rmsnorm(x, g, eps=NORM_EPS):
    x32 = x.astype(jnp.float32)
    y = x32 * lax.rsqrt(jnp.mean(x32 * x32, axis=-1, keepdims=True) + eps)
    return (y * g.astype(jnp.float32)).astype(x.dtype)


def modulate(h, shift, scale):
    return h * (1 + scale) + shift


def axial_rope_tables(n):
    t = jnp.arange(n, dtype=jnp.int32)
    row = (t // GRID_W).astype(jnp.float32)
    col = (t % GRID_W).astype(jnp.float32)
    pairs_per_axis = DA_HEAD_DIM // 4
    freq = ROPE_BASE ** (-jnp.arange(pairs_per_axis, dtype=jnp.float32) / pairs_per_axis)
    ang = jnp.concatenate([row[:, None] * freq, col[:, None] * freq], axis=-1)
    return jnp.cos(ang), jnp.sin(ang)


def apply_rope(x, cos, sin):
    xr = x.reshape(x.shape[:-1] + (x.shape[-1] // 2, 2))
    xe, xo = xr[..., 0], xr[..., 1]
    cs = cos[None, :, None, None, :].astype(x.dtype)
    sn = sin[None, :, None, None, :].astype(x.dtype)
    return jnp.stack([xe * cs - xo * sn, xe * sn + xo * cs], axis=-1).reshape(x.shape)


def softmax_attend(q, k, v):
    s = jnp.einsum('bqhd,bkhd->bhqk', q, k).astype(jnp.float32) * (q.shape[-1] ** -0.5)
    p = jax.nn.softmax(s, axis=-1).astype(v.dtype)
    return jnp.einsum('bhqk,bkhd->bqhd', p, v)


def natten_mixer(h_l, h_c, w_qkv, w_o, rpb, need_ctx):
    b, n, _ = h_l.shape
    rows = n // GRID_W
    kh = min(WIN_ROWS, rows)
    scale = NA_HEAD_DIM ** -0.5

    def proj(h):
        q, k, v = jnp.split(h @ w_qkv, 3, axis=-1)
        shp = h.shape[:2] + (NA_HEADS, NA_HEAD_DIM)
        return q.reshape(shp), k.reshape(shp), v.reshape(shp)

    q_l, k_l, v_l = proj(h_l)
    q_c, k_c, v_c = proj(h_c)
    grid = (b, rows, GRID_W, NA_HEADS, NA_HEAD_DIM)
    q_g, k_g, v_g = q_l.reshape(grid), k_l.reshape(grid), v_l.reshape(grid)

    qc = jnp.arange(GRID_W)
    col_start = jnp.clip(qc - WIN_COLS // 2, 0, GRID_W - WIN_COLS)
    col_mask = (qc[None, :] >= col_start[:, None]) & (qc[None, :] < col_start[:, None] + WIN_COLS)
    col_idx = jnp.clip(qc[None, :] - qc[:, None] + WIN_COLS - 1, 0, 2 * WIN_COLS - 2)

    def row_block(r):
        rs = jnp.clip(r - kh // 2, 0, rows - kh)
        q_r = lax.dynamic_index_in_dim(q_g, r, axis=1, keepdims=False)
        k_r = lax.dynamic_slice_in_dim(k_g, rs, kh, axis=1)
        v_r = lax.dynamic_slice_in_dim(v_g, rs, kh, axis=1)
        row_idx = rs + jnp.arange(kh) - r + (WIN_ROWS - 1)
        bias = rpb[:, row_idx][:, :, col_idx].transpose(0, 2, 1, 3).astype(jnp.float32)
        s_win = jnp.einsum('bqhd,bkwhd->bhqkw', q_r, k_r).astype(jnp.float32) * scale + bias
        s_win = jnp.where(col_mask[:, None, :], s_win, NEG_INF)
        s_ctx = jnp.einsum('bqhd,blhd->bhql', q_r, k_c).astype(jnp.float32) * scale
        s = jnp.concatenate([s_win.reshape(b, NA_HEADS, GRID_W, kh * GRID_W), s_ctx], axis=-1)
        p = jax.nn.softmax(s, axis=-1).astype(v_l.dtype)
        p_win = p[..., :kh * GRID_W].reshape(b, NA_HEADS, GRID_W, kh, GRID_W)
        p_ctx = p[..., kh * GRID_W:]
        return (jnp.einsum('bhqkw,bkwhd->bqhd', p_win, v_r)
                + jnp.einsum('bhql,blhd->bqhd', p_ctx, v_c))

    o_l = lax.map(row_block, jnp.arange(rows)).swapaxes(0, 1).reshape(b, n, D_MODEL)
    y_l = o_l @ w_o
    y_c = softmax_attend(q_c, k_c, v_c).reshape(h_c.shape) @ w_o if need_ctx else None
    return y_l, y_c


def diff_attend(q, k, v, lam):
    s = jnp.einsum('bqhcd,bkhcd->bhcqk', q, k).astype(jnp.float32) * (q.shape[-1] ** -0.5)
    p = jax.nn.softmax(s, axis=-1)
    a = (p[:, :, 0] - lam * p[:, :, 1]).astype(v.dtype)
    return jnp.einsum('bhqk,bkhe->bqhe', a, v)


def diff_mixer(h_l, h_c, w_qkv, w_o, lq1, lk1, lq2, lk2, subln_g, lambda_init, cos, sin, need_ctx):
    b, n, _ = h_l.shape

    def proj(h):
        q, k, v = jnp.split(h @ w_qkv, 3, axis=-1)
        m = h.shape[1]
        qk_shape = (b, m, DA_HEADS, 2, DA_HEAD_DIM)
        return q.reshape(qk_shape), k.reshape(qk_shape), v.reshape(b, m, DA_HEADS, 2 * DA_HEAD_DIM)

    q_l, k_l, v_l = proj(h_l)
    q_c, k_c, v_c = proj(h_c)
    q_l = apply_rope(q_l, cos, sin)
    k_l = apply_rope(k_l, cos, sin)
    f32 = jnp.float32
    lam = (jnp.exp(jnp.sum(lq1.astype(f32) * lk1.astype(f32)))
           - jnp.exp(jnp.sum(lq2.astype(f32) * lk2.astype(f32))) + lambda_init)

    def head_out(o):
        o = rmsnorm(o, subln_g, SUBLN_EPS) * (1.0 - lambda_init)
        return o.reshape(o.shape[0], o.shape[1], D_MODEL) @ w_o

    k_all = jnp.concatenate([k_l, k_c], axis=1)
    v_all = jnp.concatenate([v_l, v_c], axis=1)
    nb = n // Q_BLOCK
    q_blocks = q_l.reshape(b, nb, Q_BLOCK, DA_HEADS, 2, DA_HEAD_DIM).swapaxes(0, 1)
    o_l = lax.map(lambda qb: diff_attend(qb, k_all, v_all, lam), q_blocks)
    o_l = o_l.swapaxes(0, 1).reshape(b, n, DA_HEADS, 2 * DA_HEAD_DIM)
    y_l = head_out(o_l)
    y_c = head_out(diff_attend(q_c, k_c, v_c, lam)) if need_ctx else None
    return y_l, y_c


def ec_moe(h, w_router, w_gate, w_up, w_down):
    b, n, d = h.shape
    cap = EC_CAPACITY_FACTOR * n // N_EXPERTS
    aff = jax.nn.softmax(jnp.einsum('bnd,de->bne', h, w_router).astype(jnp.float32), axis=-1)
    gate, idx = lax.top_k(aff.swapaxes(1, 2), cap)
    xg = jax.vmap(lambda hb, ib: hb[ib])(h, idx)
    a = jax.nn.silu(jnp.einsum('becd,edf->becf', xg, w_gate)) * jnp.einsum('becd,edf->becf', xg, w_up)
    y = jnp.einsum('becf,efd->becd', a, w_down) * gate[..., None].astype(h.dtype)
    return jax.vmap(lambda yb, ib: jnp.zeros((n, d), h.dtype).at[ib.reshape(-1)].add(yb.reshape(-1, d)))(y, idx)


def lambda_init_fn(layer_idx):
    return 0.8 - 0.6 * math.exp(-0.3 * layer_idx)


def setup_inputs(seed: int = 0) -> dict:
    key = jax.random.key(seed)
    ks = jax.random.split(key, 24)
    D = D_MODEL

    def nrm(k, shape, scale):
        return jax.random.normal(k, shape, jnp.float32) * scale

    return {
        "x": nrm(ks[0], (BATCH, SEQ, D), 1.0),
        "c": nrm(ks[1], (BATCH, D), 1.0),
        "ctx": nrm(ks[2], (BATCH, CTX_LEN, D), 1.0),
        "c_ctx": nrm(ks[3], (D,), 1.0),
        "ada_w": nrm(ks[4], (DEPTH, D, N_MOD * D), 0.5 * D ** -0.5),
        "ada_b": nrm(ks[5], (DEPTH, N_MOD * D), 0.01),
        "norm1_g": 1.0 + nrm(ks[6], (DEPTH, D), 0.01),
        "norm2_g": 1.0 + nrm(ks[7], (DEPTH, D), 0.01),
        "na_w_qkv": nrm(ks[8], (N_LAYERS_A, D, 3 * D), D ** -0.5),
        "na_w_o": nrm(ks[9], (N_LAYERS_A, D, D), D ** -0.5),
        "na_rpb": nrm(ks[10], (N_LAYERS_A, NA_HEADS, 2 * WIN_ROWS - 1, 2 * WIN_COLS - 1), 0.02),
        "da_w_qkv": nrm(ks[11], (N_LAYERS_B, D, 3 * D), D ** -0.5),
        "da_w_o": nrm(ks[12], (N_LAYERS_B, D, D), D ** -0.5),
        "da_lambda_q1": nrm(ks[13], (N_LAYERS_B, DA_HEAD_DIM), 0.1),
        "da_lambda_k1": nrm(ks[14], (N_LAYERS_B, DA_HEAD_DIM), 0.1),
        "da_lambda_q2": nrm(ks[15], (N_LAYERS_B, DA_HEAD_DIM), 0.1),
        "da_lambda_k2": nrm(ks[16], (N_LAYERS_B, DA_HEAD_DIM), 0.1),
        "da_subln_g": 1.0 + nrm(ks[17], (N_LAYERS_B, 2 * DA_HEAD_DIM), 0.01),
        "moe_w_router": nrm(ks[18], (DEPTH, D, N_EXPERTS), D ** -0.5),
        "moe_w_gate": nrm(ks[19], (DEPTH, N_EXPERTS, D, EXPERT_FF), D ** -0.5),
        "moe_w_up": nrm(ks[20], (DEPTH, N_EXPERTS, D, EXPERT_FF), D ** -0.5),
        "moe_w_down": nrm(ks[21], (DEPTH, N_EXPERTS, EXPERT_FF, D), EXPERT_FF ** -0.5),
        "final_g": 1.0 + nrm(ks[22], (D,), 0.01),
    }


def reference(x, c, ctx, c_ctx, ada_w, ada_b, norm1_g, norm2_g, na_w_qkv, na_w_o, na_rpb,
              da_w_qkv, da_w_o, da_lambda_q1, da_lambda_k1, da_lambda_q2, da_lambda_k2, da_subln_g,
              moe_w_router, moe_w_gate, moe_w_up, moe_w_down, final_g):
    n = x.shape[1]
    cos, sin = axial_rope_tables(n)
    silu_c = jax.nn.silu(c)
    silu_cc = jax.nn.silu(c_ctx)
    for i in range(DEPTH):
        last = i == DEPTH - 1
        j = i // N_MIXERS
        mod_l = jnp.split((silu_c @ ada_w[i] + ada_b[i])[:, None, :], N_MOD, axis=-1)
        mod_c = jnp.split((silu_cc @ ada_w[i] + ada_b[i])[None, None, :], N_MOD, axis=-1)
        h_l = modulate(rmsnorm(x, norm1_g[i]), mod_l[0], mod_l[1])
        h_c = modulate(rmsnorm(ctx, norm1_g[i]), mod_c[0], mod_c[1])
        if i % N_MIXERS == 0:
            y_l, y_c = natten_mixer(h_l, h_c, na_w_qkv[j], na_w_o[j], na_rpb[j], not last)
        else:
            y_l, y_c = diff_mixer(h_l, h_c, da_w_qkv[j], da_w_o[j], da_lambda_q1[j], da_lambda_k1[j],
                                  da_lambda_q2[j], da_lambda_k2[j], da_subln_g[j], lambda_init_fn(i),
                                  cos, sin, not last)
        x = x + mod_l[2] * y_l
        x = x + mod_l[5] * ec_moe(modulate(rmsnorm(x, norm2_g[i]), mod_l[3], mod_l[4]),
                                  moe_w_router[i], moe_w_gate[i], moe_w_up[i], moe_w_down[i])
        if not last:
            ctx = ctx + mod_c[2] * y_c
            ctx = ctx + mod_c[5] * ec_moe(modulate(rmsnorm(ctx, norm2_g[i]), mod_c[3], mod_c[4]),
                                          moe_w_router[i], moe_w_gate[i], moe_w_up[i], moe_w_down[i])
    return rmsnorm(x, final_g)
```

```python
import numpy as np
from contextlib import ExitStack
import concourse.bass as bass
import concourse.mybir as mybir
from concourse.bass_utils import run_bass_kernel_spmd

F32 = mybir.dt.float32
BF16 = mybir.dt.bfloat16
I32 = mybir.dt.int32
AF = mybir.ActivationFunctionType
ALU = mybir.AluOpType
AX = mybir.AxisListType

D = 4096
KC = D // 128
N = 8192
NCX = 256
NT = N + NCX
NTL = N // 128
NTC = NCX // 128
NE = 16
FF = 1024
CAP = 1024
CAPC = 32
SLOTS = CAP + CAPC
GRID_W = 64
NROWS = N // GRID_W
EPS = 1e-6
SUBLN_EPS = 1e-5
BIG = 1.0e6


class Res:
    __slots__ = ("name", "w", "r")

    def __init__(self, name):
        self.name = name
        self.w = None
        self.r = {}


class Sched:
    CE = ("pe", "act", "dve", "pool")

    def __init__(self, nc, es, ndma=12):
        self.nc = nc
        self.semobj = {}
        self.cnt = {}
        for e in self.CE:
            self.semobj[e] = es.enter_context(nc.semaphore("s_" + e))
            self.cnt[e] = 0
        self.queues = ("sp", "pool")
        self.dkeys = {}
        self.dnext = {}
        for q in self.queues:
            ks = []
            for k in range(ndma):
                key = (q, k)
                self.semobj[key] = es.enter_context(nc.semaphore("d_%s%d" % (q, k)))
                self.cnt[key] = 0
                ks.append(key)
            self.dkeys[q] = ks
            self.dnext[q] = 0
        self.issuers = ("pe", "act", "dve", "pool", "sp")
        self.waited = {i: {} for i in self.issuers}
        self.thunks = {i: [] for i in self.issuers}
        self.ninst = 0

    def _wait(self, issuer, ev):
        if ev is None:
            return
        key, val = ev
        if self.waited[issuer].get(key, 0) >= val:
            return
        self.waited[issuer][key] = val
        sem = self.semobj[key]
        self.thunks[issuer].append(lambda e, sem=sem, val=val: e.wait_ge(sem, val))

    def _deps(self, issuer, reads, writes):
        for r in reads:
            if r.w is not None and not (issuer == "pe" and r.w[0] == "pe"):
                self._wait(issuer, r.w)
        for w in writes:
            if w.w is not None and not (issuer == "pe" and w.w[0] == "pe"):
                self._wait(issuer, w.w)
            for key, val in w.r.items():
                if not (issuer == "pe" and key == "pe"):
                    self._wait(issuer, (key, val))

    def _mark(self, ev, reads, writes):
        key, val = ev
        for r in reads:
            if r.r.get(key, 0) < val:
                r.r[key] = val
        for w in writes:
            w.w = ev
            w.r = {}

    def op(self, eng, fn, reads=(), writes=()):
        self._deps(eng, reads, writes)
        self.cnt[eng] += 1
        val = self.cnt[eng]
        sem = self.semobj[eng]
        self.thunks[eng].append(lambda e, fn=fn, sem=sem: fn(e).then_inc(sem, 1))
        ev = (eng, val)
        self._mark(ev, reads, writes)
        self.ninst += 1
        return ev

    def raw(self, issuer, fn):
        self.thunks[issuer].append(lambda e, fn=fn: fn(e))

    def pe_quiet(self, fn):
        self.thunks["pe"].append(lambda e, fn=fn: fn(e))
        self.ninst += 1

    def dma(self, q, fn, reads=(), writes=()):
        self._deps(q, reads, writes)
        ks = self.dkeys[q]
        key = ks[self.dnext[q] % len(ks)]
        self.dnext[q] += 1
        if self.cnt[key] > 0:
            self._wait(q, (key, self.cnt[key]))
        self.cnt[key] += 16
        val = self.cnt[key]
        sem = self.semobj[key]
        self.thunks[q].append(lambda e, fn=fn, sem=sem: fn(e).then_inc(sem, 16))
        ev = (key, val)
        self._mark(ev, reads, writes)
        self.ninst += 1
        return ev

    def flush(self, name):
        nc = self.nc
        for q in self.queues:
            for key in self.dkeys[q]:
                if self.cnt[key] > 0:
                    self._wait(q, (key, self.cnt[key]))
        th = self.thunks
        with nc.Block(name) as block:
            if th["sp"]:
                @block.sync
                def _(e):
                    for t in th["sp"]:
                        t(e)
            if th["pe"]:
                @block.tensor
                def _(e):
                    for t in th["pe"]:
                        t(e)
            if th["act"]:
                @block.scalar
                def _(e):
                    for t in th["act"]:
                        t(e)
            if th["dve"]:
                @block.vector
                def _(e):
                    for t in th["dve"]:
                        t(e)
            if th["pool"]:
                @block.gpsimd
                def _(e):
                    for t in th["pool"]:
                        t(e)
        self.thunks = {i: [] for i in self.issuers}
        for i in self.issuers:
            for key, val in self.cnt.items():
                self.waited[i][key] = val


def R(*names):
    return [Res(n) for n in names]


def build_program(stop_after=None, debug=(), inject=(), phases=None):
    nc = bass.Bass("TRN2", target_bir_lowering=False)
    es = ExitStack()
    S = Sched(nc, es)

    declared = {}

    class _Lazy:
        def __init__(self, name, shape):
            self.name, self.shape, self.t = name, list(shape), None

        def _get(self):
            if self.t is None:
                self.t = nc.dram_tensor(self.name, self.shape, F32, kind="ExternalInput")
                declared[self.name] = self.t
            return self.t

        def __getitem__(self, key):
            return self._get()[key]

    def din(name, shape):
        return _Lazy(name, shape)

    def dscr(name, shape, dt):
        kind = "ExternalOutput" if name in debug else ("ExternalInput" if name in inject else "Internal")
        return nc.dram_tensor(name, list(shape), dt, kind=kind)

    x_in = din("x", [N, D])
    ctx_in = din("ctx", [NCX, D])
    cT_in = din("cT", [128, KC, 2])
    ada_w = din("ada_w", [2, D, 6 * D])
    ada_b = din("ada_b", [2, 6 * D])
    n1g = din("norm1_g", [2, D])
    n2g = din("norm2_g", [2, D])
    fing = din("final_g", [1, D])
    wqkv = [din("na_w_qkv", [D, 3 * D]), din("da_w_qkv", [D, 3 * D])]
    wo = [din("na_w_o", [D, D]), din("da_w_o", [D, D])]
    nbias = din("nbias", [32, 64, 15 * 64])
    nmask = din("nmask", [64, 64])
    lamp = din("lam", [4, 128])
    sublng = din("subln_g", [1, 256])
    wr = din("w_router", [2, D, NE])
    wg = din("w_gate", [2, NE, D, FF])
    wu = din("w_up", [2, NE, D, FF])
    wd = din("w_down", [2, NE, FF, D])
    ident_in = din("ident", [128, 128])
    lstrict_in = din("lstrict", [128, 128])
    rope_cos = din("rope_cos", [N, 64])
    rope_sin = din("rope_sin", [N, 64])
    out_d = nc.dram_tensor("out", [N, D], F32, kind="ExternalOutput")

    mod_d = dscr("mod_d", [2, 2, 6 * D], F32)
    hT_d = dscr("hT_d", [128, KC, NT], BF16)
    qkv_d = dscr("qkv_d", [NT, 3 * D], BF16)
    oT_d = dscr("oT_d", [128, KC, NT], BF16)
    x1_d = dscr("x1_d", [NT, D], F32)
    x2_d = dscr("x2_d", [NT, D], F32)
    h2_d = dscr("h2_d", [NT, D], BF16)
    aff_d = dscr("aff_d", [NT, NE], F32)
    xg_d = dscr("xg_d", [NE * SLOTS, D], BF16)
    y_d = dscr("y_d", [NE * SLOTS, D], BF16)

    state = {"done": False, "uid": 0}

    def U(n):
        return "%s_u%d" % (n, state["uid"])

    def phase_end(name):
        S.flush(name)
        state["uid"] += 1
        if stop_after == name:
            state["done"] = True
        return state["done"]

    def phase_A():
        with ExitStack() as ps:
            sb = lambda n, s, d: ps.enter_context(nc.sbuf_tensor(U(n), s, d))
            cT = sb("a_cT", [128, KC, 2], F32)
            scT = sb("a_scT", [128, KC, 2], F32)
            wbuf = [sb("a_w%d" % i, [128, KC, 512], F32) for i in range(2)]
            bt = [sb("a_b%d" % i, [2, 512], F32) for i in range(2)]
            ot = [sb("a_o%d" % i, [2, 512], F32) for i in range(2)]
            pacc = [ps.enter_context(nc.psum_tensor(U("a_p%d" % i), [128, 512], F32)) for i in range(2)]
            r_cT, r_scT = R("cT", "scT")
            r_w = R("w0", "w1"); r_b = R("b0", "b1"); r_o = R("o0", "o1"); r_p = R("p0", "p1")
            S.dma("sp", lambda e: e.dma_start(out=cT[:, :, :], in_=cT_in[:, :, :]), [], [r_cT])
            S.op("act", lambda e: e.activation(out=scT[:, :, :], in_=cT[:, :, :], func=AF.Silu), [r_cT], [r_scT])
            it = 0
            for i in range(2):
                for cb in range(6 * D // 512):
                    k = it % 2
                    it += 1
                    c0 = cb * 512
                    S.dma("sp", lambda e, i=i, c0=c0, k=k: e.dma_start(
                        out=wbuf[k][:, :, :],
                        in_=ada_w[i, :, c0:c0 + 512].rearrange("(kc p) n -> p kc n", p=128)), [], [r_w[k]])
                    S.dma("sp", lambda e, i=i, c0=c0, k=k: e.dma_start(
                        out=bt[k][:, :], in_=ada_b[i:i + 1, c0:c0 + 512].partition_broadcast(2)), [], [r_b[k]])
                    for kc in range(KC):
                        fn = lambda e, k=k, kc=kc: e.matmul(pacc[k][0:2, :], scT[:, kc, :], wbuf[k][:, kc, :],
                                                            start=(kc == 0), stop=(kc == KC - 1))
                        if kc < KC - 1:
                            if kc == 0:
                                S.op("pe", fn, [r_scT, r_w[k]], [r_p[k]])
                            else:
                                S.pe_quiet(fn)
                        else:
                            S.op("pe", fn, [r_scT, r_w[k]], [r_p[k]])
                    S.op("dve", lambda e, k=k: e.tensor_tensor(out=ot[k][:, :], in0=pacc[k][0:2, :], in1=bt[k][:, :],
                                                               op=ALU.add), [r_p[k], r_b[k]], [r_o[k]])
                    S.dma("sp", lambda e, i=i, c0=c0, k=k: e.dma_start(out=mod_d[i, :, c0:c0 + 512], in_=ot[k][:, :]),
                          [r_o[k]], [])
        return phase_end("A")

    def src_rows(layer, t0, n):
        if layer == 0:
            if t0 < N:
                return x_in[t0:t0 + n, :]
            return ctx_in[t0 - N:t0 - N + n, :]
        return x2_d[t0:t0 + n, :]

    def load_rows_bcast(q, dst, src_ap, res):
        S.dma(q, lambda e: e.dma_start(out=dst, in_=src_ap.partition_broadcast(128)), [], [res])

    def phase_T1(layer):
        with ExitStack() as ps:
            sb = lambda n, s, d: ps.enter_context(nc.sbuf_tensor(U(n), s, d))
            xt = [sb("t1_x%d" % i, [128, D], F32) for i in range(2)]
            hb = [sb("t1_h%d" % i, [128, D], BF16) for i in range(2)]
            hT4 = [sb("t1_hT%d" % i, [128, KC, 512], BF16) for i in range(2)]
            Arow = sb("t1_A", [128, D], F32)
            Brow = sb("t1_B", [128, D], F32)
            tmp = sb("t1_tmp", [128, D], F32)
            st = [sb("t1_st%d" % i, [128, 2], F32) for i in range(2)]
            ident = sb("t1_id", [128, 128], BF16)
            ptr = [ps.enter_context(nc.psum_tensor(U("t1_p%d" % i), [128, 1024], BF16)) for i in range(4)]
            r_x = R("x0", "x1"); r_h = R("h0", "h1"); r_hT = R("hT0", "hT1"); r_st = R("st0", "st1")
            r_A, r_B, r_tmp, r_id = R("A", "B", "tmp", "id")
            r_p = R("p0", "p1", "p2", "p3")
            S.dma("pool", lambda e: e.dma_start(out=ident[:, :], in_=ident_in[:, :]), [], [r_id])
            it = 0
            si = 0
            for grp, (tok0, ntile, row) in enumerate(((0, NTL, 0), (N, NTC, 1))):
                load_rows_bcast("sp", Arow[:, :], mod_d[layer, row:row + 1, D:2 * D], r_A)
                load_rows_bcast("sp", tmp[:, :], n1g[layer:layer + 1, :], r_tmp)
                load_rows_bcast("sp", Brow[:, :], mod_d[layer, row:row + 1, 0:D], r_B)
                S.op("dve", lambda e: e.scalar_tensor_tensor(out=Arow[:, :], in0=Arow[:, :], scalar=1.0, in1=tmp[:, :],
                                                             op0=ALU.add, op1=ALU.mult), [r_A, r_tmp], [r_A])
                for tt in range(ntile):
                    k = it % 2
                    sti = si % 2
                    sub = tt % 4
                    t0 = tok0 + tt * 128
                    S.dma("sp", lambda e, k=k, t0=t0: e.dma_start(out=xt[k][:, :], in_=src_rows(layer, t0, 128)),
                          [], [r_x[k]])
                    S.op("dve", lambda e, k=k: e.memset(st[k][:, :], 0.0), [], [r_st[k]])
                    S.op("act", lambda e, k=k: e.activation(out=hb[k][:, :], in_=xt[k][:, :], func=AF.Square,
                                                            accum_out=st[k][:, 0:1]), [r_x[k], r_st[k]], [r_h[k], r_st[k]])
                    S.op("dve", lambda e, k=k: e.tensor_scalar(out=st[k][:, 1:2], in0=st[k][:, 0:1], scalar1=1.0 / D,
                                                               scalar2=EPS, op0=ALU.mult, op1=ALU.add), [r_st[k]], [r_st[k]])
                    S.op("act", lambda e, k=k: e.activation(out=st[k][:, 1:2], in_=st[k][:, 1:2], func=AF.Sqrt),
                         [r_st[k]], [r_st[k]])
                    S.op("dve", lambda e, k=k: e.reciprocal(out=st[k][:, 1:2], in_=st[k][:, 1:2]), [r_st[k]], [r_st[k]])
                    S.op("dve", lambda e, k=k: e.scalar_tensor_tensor(out=xt[k][:, :], in0=xt[k][:, :],
                                                                      scalar=st[k][:, 1:2], in1=Arow[:, :],
                                                                      op0=ALU.mult, op1=ALU.mult),
                         [r_x[k], r_st[k], r_A], [r_x[k]])
                    S.op("pool", lambda e, k=k: e.tensor_tensor(out=hb[k][:, :], in0=xt[k][:, :], in1=Brow[:, :],
                                                                op=ALU.add), [r_x[k], r_B], [r_h[k]])
                    for g in range(4):
                        for j in range(8):
                            kc = g * 8 + j
                            fn = lambda e, k=k, g=g, j=j, kc=kc: e.transpose(ptr[g][:, j * 128:(j + 1) * 128],
                                                                             hb[k][:, kc * 128:(kc + 1) * 128], ident[:, :])
                            if j == 0 or j == 7:
                                S.op("pe", fn, [r_h[k], r_id], [r_p[g]])
                            else:
                                S.pe_quiet(fn)
                        eng = "act" if g % 2 == 0 else "dve"
                        if eng == "act":
                            S.op("act", lambda e, g=g, sti=sti, sub=sub: e.activation(
                                out=hT4[sti][:, g * 8:(g + 1) * 8, sub * 128:(sub + 1) * 128],
                                in_=ptr[g][:, :].rearrange("p (j t) -> p j t", j=8), func=AF.Copy), [r_p[g]], [r_hT[sti]])
                        else:
                            S.op("dve", lambda e, g=g, sti=sti, sub=sub: e.tensor_copy(
                                out=hT4[sti][:, g * 8:(g + 1) * 8, sub * 128:(sub + 1) * 128],
                                in_=ptr[g][:, :].rearrange("p (j t) -> p j t", j=8)), [r_p[g]], [r_hT[sti]])
                    it += 1
                    if sub == 3 or tt == ntile - 1:
                        nn = (sub + 1) * 128
                        s0 = t0 - sub * 128
                        S.dma("sp", lambda e, sti=sti, s0=s0, nn=nn: e.dma_start(out=hT_d[:, :, s0:s0 + nn],
                                                                                 in_=hT4[sti][:, :, 0:nn]), [r_hT[sti]], [])
                        si += 1
        return phase_end("T1_%d" % layer)


    SCALE = 128.0 ** -0.5

    def mm_group(out_ap, pairs, reads, wres):
        n = len(pairs)
        for i, (l, r) in enumerate(pairs):
            fn = lambda e, l=l, r=r, i=i: e.matmul(out_ap, l, r, start=(i == 0), stop=(i == n - 1))
            if i == 0 or i == n - 1:
                S.op("pe", fn, reads, [wres])
            else:
                S.pe_quiet(fn)

    def phase_H1(layer):
        W = wqkv[layer]
        with ExitStack() as ps:
            sb = lambda n, s, d: ps.enter_context(nc.sbuf_tensor(U(n), s, d))
            wb = sb("h1_w", [128, KC, 1024], BF16)
            hT4 = [sb("h1_hT%d" % i, [128, KC, 512], BF16) for i in range(2)]
            stage = [sb("h1_st%d" % i, [128, 1024], BF16) for i in range(2)]
            pacc = [ps.enter_context(nc.psum_tensor(U("h1_p%d" % i), [128, 512], F32)) for i in range(4)]
            r_w, = R("w"); r_hT = R("hT0", "hT1"); r_st = R("st0", "st1"); r_p = R("p0", "p1", "p2", "p3")
            if layer == 1:
                cs = [sb("h1_cos%d" % i, [128, 64], F32) for i in range(2)]
                sn = [sb("h1_sin%d" % i, [128, 64], F32) for i in range(2)]
                tm = [sb("h1_tm%d" % i, [128, 4, 64], F32) for i in range(4)]
                r_cs = R("cs0", "cs1"); r_tm = R("tm0", "tm1", "tm2", "tm3")
            hi = 0; si = 0; pi = 0; ci = 0
            for cb in range(12):
                S.dma("pool", lambda e, cb=cb: e.dma_start(
                    out=wb[:, :, :], in_=W[:, cb * 1024:(cb + 1) * 1024].rearrange("(kc p) n -> p kc n", p=128)),
                    [], [r_w])
                def load_hT(st_, k_):
                    nt_ = 4 if st_ < 16 else 2
                    S.dma("sp", lambda e, k_=k_, st_=st_, nt_=nt_: e.dma_start(
                        out=hT4[k_][:, :, 0:nt_ * 128], in_=hT_d[:, :, st_ * 512:st_ * 512 + nt_ * 128]), [], [r_hT[k_]])
                if cb == 0:
                    load_hT(0, hi % 2)
                for st in range(17):
                    ntile = 4 if st < 16 else 2
                    k = hi % 2; hi += 1
                    if st + 1 < 17:
                        load_hT(st + 1, hi % 2)
                    elif cb + 1 < 12:
                        load_hT(0, hi % 2)
                    for ts in range(ntile):
                        t0 = st * 512 + ts * 128
                        sk = si % 2; si += 1
                        rope = (layer == 1 and cb < 8 and st < 16)
                        if rope:
                            ck = ci % 2; ci += 1
                            S.dma("sp", lambda e, ck=ck, t0=t0: e.dma_start(out=cs[ck][:, :], in_=rope_cos[t0:t0 + 128, :]),
                                  [], [r_cs[ck]])
                            S.dma("sp", lambda e, ck=ck, t0=t0: e.dma_start(out=sn[ck][:, :], in_=rope_sin[t0:t0 + 128, :]),
                                  [], [r_cs[ck]])
                        for half in range(2):
                            p = pi % 4; pi += 1
                            mm_group(pacc[p][:, :],
                                     [(hT4[k][:, kc, ts * 128:(ts + 1) * 128], wb[:, kc, half * 512:(half + 1) * 512])
                                      for kc in range(KC)], [r_hT[k], r_w], r_p[p])
                            dst = stage[sk][:, half * 512:(half + 1) * 512]
                            if not rope:
                                if half == 0:
                                    S.op("act", lambda e, dst=dst, p=p: e.activation(out=dst, in_=pacc[p][:, :], func=AF.Copy),
                                         [r_p[p]], [r_st[sk]])
                                else:
                                    S.op("dve", lambda e, dst=dst, p=p: e.tensor_copy(out=dst, in_=pacc[p][:, :]),
                                         [r_p[p]], [r_st[sk]])
                            else:
                                pv = pacc[p][:, :].rearrange("p (g i two) -> p g i two", g=4, i=64, two=2)
                                dv = dst.rearrange("p (g i two) -> p g i two", g=4, i=64, two=2)
                                cb_ = cs[ck][:, :].unsqueeze(1).to_broadcast([128, 4, 64])
                                sb_ = sn[ck][:, :].unsqueeze(1).to_broadcast([128, 4, 64])
                                xe, xo = pv[:, :, :, 0], pv[:, :, :, 1]
                                S.op("dve", lambda e, xe=xe, cb_=cb_: e.tensor_tensor(out=tm[0][:, :, :], in0=xe, in1=cb_, op=ALU.mult),
                                     [r_p[p], r_cs[ck]], [r_tm[0]])
                                S.op("dve", lambda e, xo=xo, sb_=sb_: e.tensor_tensor(out=tm[1][:, :, :], in0=xo, in1=sb_, op=ALU.mult),
                                     [r_p[p], r_cs[ck]], [r_tm[1]])
                                S.op("dve", lambda e, xe=xe, sb_=sb_: e.tensor_tensor(out=tm[2][:, :, :], in0=xe, in1=sb_, op=ALU.mult),
                                     [r_p[p], r_cs[ck]], [r_tm[2]])
                                S.op("dve", lambda e, xo=xo, cb_=cb_: e.tensor_tensor(out=tm[3][:, :, :], in0=xo, in1=cb_, op=ALU.mult),
                                     [r_p[p], r_cs[ck]], [r_tm[3]])
                                S.op("pool", lambda e, dv=dv: e.tensor_tensor(out=dv[:, :, :, 0], in0=tm[0][:, :, :], in1=tm[1][:, :, :],
                                                                              op=ALU.subtract), [r_tm[0], r_tm[1]], [r_st[sk]])
                                S.op("pool", lambda e, dv=dv: e.tensor_tensor(out=dv[:, :, :, 1], in0=tm[2][:, :, :], in1=tm[3][:, :, :],
                                                                              op=ALU.add), [r_tm[2], r_tm[3]], [r_st[sk]])
                        S.dma("sp", lambda e, sk=sk, t0=t0, cb=cb: e.dma_start(
                            out=qkv_d[t0:t0 + 128, cb * 1024:(cb + 1) * 1024], in_=stage[sk][:, :]), [r_st[sk]], [])
        return phase_end("H1_%d" % layer)

    def phase_N():
        with ExitStack() as ps:
            sb = lambda n, s, d: ps.enter_context(nc.sbuf_tensor(U(n), s, d))
            qtm = sb("n_qtm", [128, 66, 128], BF16)
            ktm = sb("n_ktm", [128, 66, 128], BF16)
            QT = sb("n_QT", [128, NT], BF16)
            KT = sb("n_KT", [128, NT], BF16)
            Va = [sb("n_va%d" % i, [128, 64, 128], BF16) for i in range(2)]
            Vb = [sb("n_vb%d" % i, [128, 63, 128], BF16) for i in range(2)]
            Vc = [sb("n_vc%d" % i, [128, 2, 128], BF16) for i in range(2)]
            bias = [sb("n_bias%d" % i, [64, 960], F32) for i in range(2)]
            oTh = sb("n_oT", [128, NT], BF16)
            sbs = [sb("n_s%d" % i, [128, 768], F32) for i in range(2)]
            pbf = [sb("n_p%d" % i, [128, 768], BF16) for i in range(2)]
            pT = [sb("n_pT%d" % i, [128, 384], BF16) for i in range(2)]
            stt = [sb("n_stt%d" % i, [128, 4], F32) for i in range(2)]
            stt2 = [sb("n_stt2%d" % i, [128, 4], F32) for i in range(2)]
            obf = [sb("n_o%d" % i, [128, 128], BF16) for i in range(2)]
            ident = sb("n_id", [128, 128], BF16)
            msk = sb("n_msk", [64, 64], F32)
            ptq0 = ps.enter_context(nc.psum_tensor(U("n_ptq0"), [128, 1024], BF16))
            ptq1 = ps.enter_context(nc.psum_tensor(U("n_ptq1"), [128, 1024], BF16))
            ps_a = [ps.enter_context(nc.psum_tensor(U("n_pa%d" % i), [128, 512], F32)) for i in range(2)]
            ps_b = [ps.enter_context(nc.psum_tensor(U("n_pb%d" % i), [128, 512], F32)) for i in range(2)]
            ps_o = [ps.enter_context(nc.psum_tensor(U("n_po%d" % i), [128, 512], F32)) for i in range(2)]
            r_qtm, r_ktm, r_QT, r_KT, r_oTh, r_id, r_msk, r_q0, r_q1a, r_q1b = R(
                "qtm", "ktm", "QT", "KT", "oTh", "id", "msk", "q0", "q1a", "q1b")
            r_V = R("V0", "V1"); r_bias = R("b0", "b1")
            r_s = R("s0", "s1"); r_p = R("p0", "p1"); r_pT = R("pT0", "pT1"); r_stt = R("t0", "t1"); r_o = R("o0", "o1")
            r_pa = R("pa0", "pa1"); r_pb = R("pb0", "pb1"); r_po = R("po0", "po1"); r_stt2 = R("u0", "u1")
            S.dma("pool", lambda e: e.dma_start(out=ident[:, :], in_=ident_in[:, :]), [], [r_id])
            S.dma("sp", lambda e: e.dma_start(out=msk[:, :], in_=nmask[:, :]), [], [r_msk])
            wi = 0
            for hh in range(32):
                hb = hh % 2
                qs = lambda c0: qkv_d[:, c0:c0 + 128]
                S.dma("sp", lambda e, hh=hh: e.dma_start(
                    out=qtm[:, :, :], in_=qkv_d[:, hh * 128:(hh + 1) * 128].rearrange("(c p) d -> p c d", p=128)), [], [r_qtm])
                S.dma("sp", lambda e, hh=hh: e.dma_start(
                    out=ktm[:, :, :], in_=qkv_d[:, D + hh * 128:D + (hh + 1) * 128].rearrange("(c p) d -> p c d", p=128)),
                    [], [r_ktm])
                vcol = 2 * D + hh * 128
                S.dma("sp", lambda e, hb=hb, vcol=vcol: e.dma_start(
                    out=Va[hb][:, :, :], in_=qkv_d[0:N, vcol:vcol + 128].rearrange("(c p) d -> p c d", p=128)), [], [r_V[hb]])
                S.dma("sp", lambda e, hb=hb, vcol=vcol: e.dma_start(
                    out=Vb[hb][:, :, :], in_=qkv_d[64:64 + 63 * 128, vcol:vcol + 128].rearrange("(c p) d -> p c d", p=128)),
                    [], [r_V[hb]])
                S.dma("sp", lambda e, hb=hb, vcol=vcol: e.dma_start(
                    out=Vc[hb][:, :, :], in_=qkv_d[N:NT, vcol:vcol + 128].rearrange("(c p) d -> p c d", p=128)), [], [r_V[hb]])
                S.dma("sp", lambda e, hb=hb, hh=hh: e.dma_start(out=bias[hb][:, :], in_=nbias[hh, :, :]), [], [r_bias[hb]])
                S.op("dve", lambda e, hb=hb: e.tensor_tensor(
                    out=bias[hb][:, :].rearrange("p (j k) -> p j k", j=15),
                    in0=bias[hb][:, :].rearrange("p (j k) -> p j k", j=15),
                    in1=msk[:, :].unsqueeze(1).to_broadcast([64, 15, 64]), op=ALU.add), [r_bias[hb], r_msk], [r_bias[hb]])
                for (src, r_src, dstT, r_dst) in ((qtm, r_qtm, QT, r_QT), (ktm, r_ktm, KT, r_KT)):
                    for c0 in range(0, 66, 8):
                        nb = min(8, 66 - c0)
                        for j in range(nb):
                            fn = lambda e, src=src, c=c0 + j, j=j: e.transpose(ptq0[:, j * 128:(j + 1) * 128], src[:, c, :], ident[:, :])
                            if j == 0 or j == nb - 1:
                                S.op("pe", fn, [r_src, r_id], [r_q0])
                            else:
                                S.pe_quiet(fn)
                        S.op("dve", lambda e, dstT=dstT, c0=c0, nb=nb: e.tensor_copy(
                            out=dstT[:, c0 * 128:(c0 + nb) * 128], in_=ptq0[:, 0:nb * 128]), [r_q0], [r_dst])
                def row_info(r):
                    is_ctx = r >= NROWS
                    if not is_ctx:
                        rs = min(max(r - 4, 0), NROWS - 8)
                        return dict(is_ctx=False, P=64, rs=rs, j0=rs - r + 7, q0=r * 64, nk=768)
                    return dict(is_ctx=True, P=128, rs=0, j0=0, q0=N + (r - NROWS) * 128, nk=256)

                def emit_S(r, k):
                    ri = row_info(r)
                    q0 = ri["q0"]
                    if not ri["is_ctx"]:
                        rs, j0 = ri["rs"], ri["j0"]
                        S.op("pe", lambda e, k=k, q0=q0, rs=rs: e.matmul(ps_a[k][0:64, 0:512], QT[:, q0:q0 + 64],
                                                                       KT[:, rs * 64:rs * 64 + 512], start=True, stop=True),
                             [r_QT, r_KT], [r_pa[k]])
                        S.op("pe", lambda e, k=k, q0=q0: e.matmul(ps_b[k][0:64, 0:256], QT[:, q0:q0 + 64], KT[:, N:NT],
                                                                start=True, stop=True), [r_QT, r_KT], [r_pb[k]])
                    else:
                        S.op("pe", lambda e, k=k, q0=q0: e.matmul(ps_a[k][:, 0:256], QT[:, q0:q0 + 128], KT[:, N:NT],
                                                                start=True, stop=True), [r_QT, r_KT], [r_pa[k]])

                def emit_softmax(r, k):
                    ri = row_info(r)
                    P_, nk = ri["P"], ri["nk"]
                    if not ri["is_ctx"]:
                        j0 = ri["j0"]
                        S.op("dve", lambda e, k=k, hb=hb, j0=j0: e.scalar_tensor_tensor(
                            out=sbs[k][0:64, 0:512], in0=ps_a[k][0:64, 0:512], scalar=SCALE,
                            in1=bias[hb][:, j0 * 64:j0 * 64 + 512], op0=ALU.mult, op1=ALU.add),
                            [r_pa[k], r_bias[hb]], [r_s[k]])
                        S.op("act", lambda e, k=k: e.activation(out=sbs[k][0:64, 512:768], in_=ps_b[k][0:64, 0:256],
                                                                func=AF.Copy, scale=SCALE), [r_pb[k]], [r_s[k]])
                    else:
                        S.op("act", lambda e, k=k: e.activation(out=sbs[k][:, 0:256], in_=ps_a[k][:, 0:256],
                                                                func=AF.Copy, scale=SCALE), [r_pa[k]], [r_s[k]])
                    S.op("dve", lambda e, k=k, P_=P_, nk=nk: e.tensor_reduce(out=stt[k][0:P_, 0:1], in_=sbs[k][0:P_, 0:nk],
                                                                           axis=AX.X, op=ALU.max), [r_s[k]], [r_stt[k]])
                    S.op("dve", lambda e, k=k, P_=P_: e.tensor_scalar(out=stt[k][0:P_, 1:2], in0=stt[k][0:P_, 0:1],
                                                                      scalar1=-1.0, scalar2=None, op0=ALU.mult),
                         [r_stt[k]], [r_stt[k]])
                    S.op("pool", lambda e, k=k, P_=P_: e.memset(stt2[k][0:P_, 0:1], 0.0), [], [r_stt2[k]])
                    S.op("act", lambda e, k=k, P_=P_, nk=nk: e.activation(
                        out=pbf[k][0:P_, 0:nk], in_=sbs[k][0:P_, 0:nk], func=AF.Exp, bias=stt[k][0:P_, 1:2], scale=1.0,
                        accum_out=stt2[k][0:P_, 0:1]), [r_s[k], r_stt[k], r_stt2[k]], [r_p[k], r_stt2[k]])
                    S.op("dve", lambda e, k=k, P_=P_: e.reciprocal(out=stt2[k][0:P_, 1:2], in_=stt2[k][0:P_, 0:1]),
                         [r_stt2[k]], [r_stt2[k]])

                def emit_PV(r, k):
                    ri = row_info(r)
                    P_, nk, rs, q0, is_ctx = ri["P"], ri["nk"], ri["rs"], ri["q0"], ri["is_ctx"]
                    nch = nk // 128
                    for c in range(nch):
                        fn = lambda e, k=k, c=c, P_=P_: e.transpose(ptq1[:, c * P_:(c + 1) * P_],
                                                                    pbf[k][0:P_, c * 128:(c + 1) * 128], ident[0:P_, 0:P_])
                        if c == 0 or c == nch - 1:
                            S.op("pe", fn, [r_p[k], r_id], [r_q1a])
                        else:
                            S.pe_quiet(fn)
                    S.op("act", lambda e, k=k, w_=nch * P_: e.activation(out=pT[k][:, 0:w_], in_=ptq1[:, 0:w_], func=AF.Copy),
                         [r_q1a], [r_pT[k]])
                    pairs = []
                    for c in range(nch):
                        if is_ctx:
                            vch = Vc[hb][:, c, :]
                        elif c < 4:
                            vch = Va[hb][:, rs // 2 + c, :] if rs % 2 == 0 else Vb[hb][:, (rs - 1) // 2 + c, :]
                        else:
                            vch = Vc[hb][:, c - 4, :]
                        pairs.append((pT[k][:, c * P_:(c + 1) * P_], vch))
                    mm_group(ps_o[k][0:P_, 0:128], pairs, [r_pT[k], r_V[hb]], r_po[k])
                    S.op("act", lambda e, k=k, P_=P_: e.activation(out=obf[k][0:P_, :], in_=ps_o[k][0:P_, 0:128], func=AF.Copy,
                                                                   scale=stt2[k][0:P_, 1:2]), [r_po[k], r_stt2[k]], [r_o[k]])
                    S.op("pe", lambda e, k=k, P_=P_: e.transpose(ptq1[:, 512:512 + P_], obf[k][0:P_, :], ident[0:P_, 0:P_]),
                         [r_o[k], r_id], [r_q1b])
                    S.op("dve", lambda e, q0=q0, P_=P_: e.tensor_copy(out=oTh[:, q0:q0 + P_], in_=ptq1[:, 512:512 + P_]),
                         [r_q1b], [r_oTh])

                NR = NROWS + 2
                emit_S(0, wi % 2)
                for r in range(NR):
                    k = wi % 2
                    emit_softmax(r, k)
                    if r + 1 < NR:
                        emit_S(r + 1, (wi + 1) % 2)
                    emit_PV(r, k)
                    wi += 1
                S.dma("sp", lambda e, hh=hh: e.dma_start(out=oT_d[:, hh, :], in_=oTh[:, :]), [r_oTh], [])
        return phase_end("N")

    def phase_T2a(layer):
        W = wo[layer]
        with ExitStack() as ps:
            sb = lambda n, s, d: ps.enter_context(nc.sbuf_tensor(U(n), s, d))
            wob = [sb("t2_w%d" % i, [128, KC, 512], BF16) for i in range(2)]
            oT4 = [sb("t2_oT%d" % i, [128, KC, 512], BF16) for i in range(2)]
            xb = [sb("t2_x%d" % i, [128, 512], F32) for i in range(2)]
            yb = [sb("t2_y%d" % i, [128, 512], F32) for i in range(2)]
            g2 = [sb("t2_g%d" % i, [128, D], F32) for i in range(2)]
            pacc = [ps.enter_context(nc.psum_tensor(U("t2_p%d" % i), [128, 512], F32)) for i in range(4)]
            r_w = R("w0", "w1"); r_oT = R("o0", "o1"); r_x = R("x0", "x1"); r_y = R("y0", "y1"); r_g = R("g0", "g1")
            r_p = R("p0", "p1", "p2", "p3")
            for row in range(2):
                load_rows_bcast("sp", g2[row][:, :], mod_d[layer, row:row + 1, 2 * D:3 * D], r_g[row])
            oi = 0; xi = 0; pi = 0
            for nb in range(8):
                wk = nb % 2
                S.dma("pool", lambda e, wk=wk, nb=nb: e.dma_start(
                    out=wob[wk][:, :, :], in_=W[:, nb * 512:(nb + 1) * 512].rearrange("(kc p) n -> p kc n", p=128)),
                    [], [r_w[wk]])
                NST = 17 if layer == 0 else 16

                def load_oT(st_, k_):
                    nt_ = 4 if st_ < 16 else 2
                    S.dma("sp", lambda e, k_=k_, st_=st_, nt_=nt_: e.dma_start(
                        out=oT4[k_][:, :, 0:nt_ * 128], in_=oT_d[:, :, st_ * 512:st_ * 512 + nt_ * 128]), [], [r_oT[k_]])
                if nb == 0:
                    load_oT(0, oi % 2)
                for st in range(NST):
                    ntile = 4 if st < 16 else 2
                    row = 0 if st < 16 else 1
                    k = oi % 2; oi += 1
                    if st + 1 < NST:
                        load_oT(st + 1, oi % 2)
                    elif nb + 1 < 8:
                        load_oT(0, oi % 2)
                    for ts in range(ntile):
                        t0 = st * 512 + ts * 128
                        j = xi % 2; xi += 1
                        p = pi % 4; pi += 1
                        S.dma("sp", lambda e, j=j, t0=t0, nb=nb: e.dma_start(
                            out=xb[j][:, :], in_=src_rows(layer, t0, 128)[:, nb * 512:(nb + 1) * 512]), [], [r_x[j]])
                        mm_group(pacc[p][:, :], [(oT4[k][:, kc, ts * 128:(ts + 1) * 128], wob[wk][:, kc, :]) for kc in range(KC)],
                                 [r_oT[k], r_w[wk]], r_p[p])
                        S.op("dve", lambda e, j=j, p=p, row=row, nb=nb: e.tensor_tensor(
                            out=yb[j][:, :], in0=pacc[p][:, :], in1=g2[row][:, nb * 512:(nb + 1) * 512], op=ALU.mult),
                            [r_p[p], r_g[row]], [r_y[j]])
                        S.op("pool", lambda e, j=j: e.tensor_tensor(out=yb[j][:, :], in0=yb[j][:, :], in1=xb[j][:, :], op=ALU.add),
                             [r_y[j], r_x[j]], [r_y[j]])
                        S.dma("sp", lambda e, j=j, t0=t0, nb=nb: e.dma_start(
                            out=x1_d[t0:t0 + 128, nb * 512:(nb + 1) * 512], in_=yb[j][:, :]), [r_y[j]], [])
        return phase_end("T2a_%d" % layer)

    def phase_T2b(layer):
        with ExitStack() as ps:
            sb = lambda n, s, d: ps.enter_context(nc.sbuf_tensor(U(n), s, d))
            xt = [sb("tb_x%d" % i, [128, D], F32) for i in range(2)]
            hb = [sb("tb_h%d" % i, [128, D], BF16) for i in range(2)]
            Arow = sb("tb_A", [128, D], F32)
            Brow = sb("tb_B", [128, D], F32)
            tmp = sb("tb_tmp", [128, D], F32)
            st = [sb("tb_st%d" % i, [128, 8], F32) for i in range(2)]
            hfT = sb("tb_hfT", [128, KC, 128], F32)
            identf = sb("tb_id", [128, 128], F32)
            wrb = sb("tb_wr", [128, KC, NE], F32)
            lg = [sb("tb_lg%d" % i, [128, NE], F32) for i in range(2)]
            ptr = [ps.enter_context(nc.psum_tensor(U("tb_p%d" % i), [128, 512], F32)) for i in range(4)]
            pl = ps.enter_context(nc.psum_tensor(U("tb_pl"), [128, 512], F32))
            r_x = R("x0", "x1"); r_h = R("h0", "h1"); r_st = R("st0", "st1"); r_lg = R("lg0", "lg1")
            r_A, r_B, r_tmp, r_id, r_wr, r_hfT, r_pl = R("A", "B", "tmp", "id", "wr", "hfT", "pl")
            r_p = R("p0", "p1", "p2", "p3")
            S.dma("sp", lambda e: e.dma_start(out=identf[:, :], in_=ident_in[:, :]), [], [r_id])
            S.dma("sp", lambda e: e.dma_start(out=wrb[:, :, :], in_=wr[layer, :, :].rearrange("(kc p) n -> p kc n", p=128)),
                  [], [r_wr])
            it = 0
            for (tok0, ntile, row) in (((0, NTL, 0), (N, NTC, 1)) if layer == 0 else ((0, NTL, 0),)):
                load_rows_bcast("sp", Arow[:, :], mod_d[layer, row:row + 1, 4 * D:5 * D], r_A)
                load_rows_bcast("sp", tmp[:, :], n2g[layer:layer + 1, :], r_tmp)
                load_rows_bcast("sp", Brow[:, :], mod_d[layer, row:row + 1, 3 * D:4 * D], r_B)
                S.op("dve", lambda e: e.scalar_tensor_tensor(out=Arow[:, :], in0=Arow[:, :], scalar=1.0, in1=tmp[:, :],
                                                             op0=ALU.add, op1=ALU.mult), [r_A, r_tmp], [r_A])
                for tt in range(ntile):
                    k = it % 2; it += 1
                    t0 = tok0 + tt * 128
                    S.dma("sp", lambda e, k=k, t0=t0: e.dma_start(out=xt[k][:, :], in_=x1_d[t0:t0 + 128, :]), [], [r_x[k]])
                    S.op("dve", lambda e, k=k: e.memset(st[k][:, 0:1], 0.0), [], [r_st[k]])
                    S.op("act", lambda e, k=k: e.activation(out=hb[k][:, :], in_=xt[k][:, :], func=AF.Square,
                                                            accum_out=st[k][:, 0:1]), [r_x[k], r_st[k]], [r_h[k], r_st[k]])
                    S.op("dve", lambda e, k=k: e.tensor_scalar(out=st[k][:, 1:2], in0=st[k][:, 0:1], scalar1=1.0 / D,
                                                               scalar2=EPS, op0=ALU.mult, op1=ALU.add), [r_st[k]], [r_st[k]])
                    S.op("act", lambda e, k=k: e.activation(out=st[k][:, 1:2], in_=st[k][:, 1:2], func=AF.Sqrt),
                         [r_st[k]], [r_st[k]])
                    S.op("dve", lambda e, k=k: e.reciprocal(out=st[k][:, 1:2], in_=st[k][:, 1:2]), [r_st[k]], [r_st[k]])
                    S.op("dve", lambda e, k=k: e.scalar_tensor_tensor(out=xt[k][:, :], in0=xt[k][:, :], scalar=st[k][:, 1:2],
                                                                      in1=Arow[:, :], op0=ALU.mult, op1=ALU.mult),
                         [r_x[k], r_st[k], r_A], [r_x[k]])
                    S.op("pool", lambda e, k=k: e.tensor_tensor(out=xt[k][:, :], in0=xt[k][:, :], in1=Brow[:, :], op=ALU.add),
                         [r_x[k], r_B], [r_x[k]])
                    S.op("act", lambda e, k=k: e.activation(out=hb[k][:, :], in_=xt[k][:, :], func=AF.Copy), [r_x[k]], [r_h[k]])
                    S.dma("sp", lambda e, k=k, t0=t0: e.dma_start(out=h2_d[t0:t0 + 128, :], in_=hb[k][:, :]), [r_h[k]], [])
                    for g in range(8):
                        pg = g % 4
                        for j in range(4):
                            kc = g * 4 + j
                            fn = lambda e, k=k, pg=pg, j=j, kc=kc: e.transpose(ptr[pg][:, j * 128:(j + 1) * 128],
                                                                               xt[k][:, kc * 128:(kc + 1) * 128], identf[:, :])
                            if j == 0 or j == 3:
                                S.op("pe", fn, [r_x[k], r_id], [r_p[pg]])
                            else:
                                S.pe_quiet(fn)
                        if g % 2 == 0:
                            S.op("act", lambda e, g=g, pg=pg: e.activation(
                                out=hfT[:, g * 4:(g + 1) * 4, :], in_=ptr[pg][:, :].rearrange("p (j t) -> p j t", j=4),
                                func=AF.Copy), [r_p[pg]], [r_hfT])
                        else:
                            S.op("dve", lambda e, g=g, pg=pg: e.tensor_copy(
                                out=hfT[:, g * 4:(g + 1) * 4, :], in_=ptr[pg][:, :].rearrange("p (j t) -> p j t", j=4)),
                                [r_p[pg]], [r_hfT])
                    mm_group(pl[:, 0:NE], [(hfT[:, kc, :], wrb[:, kc, :]) for kc in range(KC)], [r_hfT, r_wr], r_pl)
                    S.op("dve", lambda e, k=k: e.tensor_reduce(out=st[k][:, 2:3], in_=pl[:, 0:NE], axis=AX.X, op=ALU.max),
                         [r_pl], [r_st[k]])
                    S.op("dve", lambda e, k=k: e.tensor_scalar(out=st[k][:, 3:4], in0=st[k][:, 2:3], scalar1=-1.0, scalar2=None,
                                                               op0=ALU.mult), [r_st[k]], [r_st[k]])
                    S.op("dve", lambda e, k=k: e.memset(st[k][:, 4:5], 0.0), [], [r_st[k]])
                    S.op("act", lambda e, k=k: e.activation(out=lg[k][:, :], in_=pl[:, 0:NE], func=AF.Exp, bias=st[k][:, 3:4],
                                                            scale=1.0, accum_out=st[k][:, 4:5]), [r_pl, r_st[k]], [r_lg[k], r_st[k]])
                    S.op("dve", lambda e, k=k: e.reciprocal(out=st[k][:, 5:6], in_=st[k][:, 4:5]), [r_st[k]], [r_st[k]])
                    S.op("dve", lambda e, k=k: e.tensor_scalar(out=lg[k][:, :], in0=lg[k][:, :], scalar1=st[k][:, 5:6],
                                                               scalar2=None, op0=ALU.mult), [r_lg[k], r_st[k]], [r_lg[k]])
                    S.dma("sp", lambda e, k=k, t0=t0: e.dma_start(out=aff_d[t0:t0 + 128, :], in_=lg[k][:, :]), [r_lg[k]], [])
        return phase_end("T2b_%d" % layer)

    def phase_DA():
        import math
        LAM_INIT = 0.8 - 0.6 * math.exp(-0.3 * 1)
        with ExitStack() as ps:
            sb = lambda n, s, d: ps.enter_context(nc.sbuf_tensor(U(n), s, d))
            tm = sb("da_tm", [128, 66, 256], BF16)
            QT = sb("da_QT", [128, 2, N], BF16)
            KT = sb("da_KT", [128, 2, NT], BF16)
            Vaug = sb("da_V", [128, 66, 257], BF16)
            PT = [sb("da_PT%d" % i, [128, 512], BF16) for i in range(2)]
            ident = sb("da_id", [128, 128], BF16)
            lrow = sb("da_lrow", [128, 4, 128], F32)
            ltmp = sb("da_ltmp", [128, 128], F32)
            lamt = sb("da_lam", [128, 8], F32)
            gsub = sb("da_gsub", [128, 256], F32)
            o32 = [sb("da_o32%d" % i, [128, 256], F32) for i in range(2)]
            obf = [sb("da_obf%d" % i, [128, 256], BF16) for i in range(2)]
            osb = [sb("da_osb%d" % i, [128, 2, 128], BF16) for i in range(2)]
            stt = [sb("da_stt%d" % i, [128, 8], F32) for i in range(2)]
            junk = sb("da_junk", [128, 256], BF16)
            ptq = ps.enter_context(nc.psum_tensor(U("da_ptq"), [128, 1024], BF16))
            pso = ps.enter_context(nc.psum_tensor(U("da_pso"), [128, 1024], BF16))
            ps_s = [ps.enter_context(nc.psum_tensor(U("da_ps%d" % i), [128, 512], F32)) for i in range(2)]
            po = [ps.enter_context(nc.psum_tensor(U("da_po%d" % i), [128, 512], F32)) for i in range(4)]
            r_tm, r_QT, r_KT, r_V, r_id, r_lrow, r_ltmp, r_lam, r_gsub, r_junk, r_ptq, r_pso = R(
                "tm", "QT", "KT", "V", "id", "lrow", "ltmp", "lam", "gsub", "junk", "ptq", "pso")
            r_PT = R("PT0", "PT1"); r_o32 = R("o0", "o1"); r_obf = R("ob0", "ob1"); r_osb = R("os0", "os1")
            r_stt = R("st0", "st1"); r_ps = R("ps0", "ps1"); r_po = R("po0", "po1", "po2", "po3")
            S.dma("pool", lambda e: e.dma_start(out=ident[:, :], in_=ident_in[:, :]), [], [r_id])
            for i in range(4):
                S.dma("sp", lambda e, i=i: e.dma_start(out=lrow[:, i, :], in_=lamp[i:i + 1, :].partition_broadcast(128)),
                      [], [r_lrow])
            for j in range(2):
                S.op("dve", lambda e, j=j: e.tensor_tensor(out=ltmp[:, :], in0=lrow[:, 2 * j, :], in1=lrow[:, 2 * j + 1, :],
                                                           op=ALU.mult), [r_lrow], [r_ltmp])
                S.op("dve", lambda e, j=j: e.tensor_reduce(out=lamt[:, j:j + 1], in_=ltmp[:, :], axis=AX.X, op=ALU.add),
                     [r_ltmp], [r_lam])
            S.op("act", lambda e: e.activation(out=lamt[:, 2:4], in_=lamt[:, 0:2], func=AF.Exp), [r_lam], [r_lam])
            S.op("dve", lambda e: e.tensor_tensor(out=lamt[:, 4:5], in0=lamt[:, 3:4], in1=lamt[:, 2:3], op=ALU.subtract),
                 [r_lam], [r_lam])
            S.op("dve", lambda e: e.tensor_scalar(out=lamt[:, 5:6], in0=lamt[:, 4:5], scalar1=-LAM_INIT, scalar2=None, op0=ALU.add),
                 [r_lam], [r_lam])
            load_rows_bcast("sp", gsub[:, :], sublng[0:1, :], r_gsub)
            S.op("dve", lambda e: e.tensor_scalar(out=gsub[:, :], in0=gsub[:, :], scalar1=1.0 - LAM_INIT, scalar2=None, op0=ALU.mult),
                 [r_gsub], [r_gsub])
            S.op("pool", lambda e: e.memset(Vaug[:, :, 256:257], 1.0), [], [r_V])
            step = 0; ei = 0
            for h in range(16):
                for (c_off, nchunk, dstT, r_dst) in ((h * 256, NTL, QT, r_QT), (D + h * 256, NTL + NTC, KT, r_KT)):
                    S.dma("sp", lambda e, c_off=c_off, nchunk=nchunk: e.dma_start(
                        out=tm[:, 0:nchunk, :],
                        in_=qkv_d[0:nchunk * 128, c_off:c_off + 256].rearrange("(c p) d -> p c d", p=128)), [], [r_tm])
                    for comp in range(2):
                        for c0 in range(0, nchunk, 8):
                            nb = min(8, nchunk - c0)
                            for j in range(nb):
                                fn = lambda e, c=c0 + j, j=j, comp=comp: e.transpose(
                                    ptq[:, j * 128:(j + 1) * 128], tm[:, c, comp * 128:(comp + 1) * 128], ident[:, :])
                                if j == 0 or j == nb - 1:
                                    S.op("pe", fn, [r_tm, r_id], [r_ptq])
                                else:
                                    S.pe_quiet(fn)
                            if (c0 // 8) % 2 == 0:
                                S.op("dve", lambda e, dstT=dstT, comp=comp, c0=c0, nb=nb: e.tensor_copy(
                                    out=dstT[:, comp, c0 * 128:(c0 + nb) * 128], in_=ptq[:, 0:nb * 128]), [r_ptq], [r_dst])
                            else:
                                S.op("act", lambda e, dstT=dstT, comp=comp, c0=c0, nb=nb: e.activation(
                                    out=dstT[:, comp, c0 * 128:(c0 + nb) * 128], in_=ptq[:, 0:nb * 128], func=AF.Copy),
                                    [r_ptq], [r_dst])
                vcol = 2 * D + h * 256
                S.dma("sp", lambda e, vcol=vcol: e.dma_start(
                    out=Vaug[:, :, 0:256], in_=qkv_d[:, vcol:vcol + 256].rearrange("(c p) d -> p c d", p=128)), [], [r_V])
                NKC = NTL + NTC

                def emit_scores(qb_, kc_, gi_):
                    sk_ = gi_ % 2
                    for comp in range(2):
                        S.op("pe", lambda e, sk_=sk_, comp=comp, kc_=kc_, q0_=qb_ * 256: e.matmul(
                            ps_s[sk_][:, comp * 256:(comp + 1) * 256], KT[:, comp, kc_ * 128:(kc_ + 1) * 128],
                            QT[:, comp, q0_:q0_ + 256], start=True, stop=True), [r_KT, r_QT], [r_ps[sk_]])

                def emit_exp_pv(kc_, gi_):
                    sk_ = gi_ % 2
                    S.op("act", lambda e, sk_=sk_: e.activation(out=PT[sk_][:, :], in_=ps_s[sk_][:, :], func=AF.Exp, scale=SCALE),
                         [r_ps[sk_]], [r_PT[sk_]])
                    for comp in range(2):
                        for qs in range(2):
                            pi = comp * 2 + qs
                            S.op("pe", lambda e, sk_=sk_, comp=comp, qs=qs, pi=pi, kc_=kc_: e.matmul(
                                po[pi][:, 0:257], PT[sk_][:, comp * 256 + qs * 128:comp * 256 + (qs + 1) * 128],
                                Vaug[:, kc_, :], start=(kc_ == 0), stop=(kc_ == NKC - 1)), [r_PT[sk_], r_V], [r_po[pi]])

                NQB = N // 256
                emit_scores(0, 0, step)
                for qb in range(NQB):
                    q0 = qb * 256
                    for kc in range(NKC):
                        if kc + 1 < NKC:
                            emit_scores(qb, kc + 1, step + 1)
                        elif qb + 1 < NQB:
                            emit_scores(qb + 1, 0, step + 1)
                        emit_exp_pv(kc, step)
                        step += 1
                    for qs in range(2):
                        k2 = ei % 2; ei += 1
                        S.op("dve", lambda e, k2=k2, qs=qs: e.reciprocal(out=stt[k2][:, 0:1], in_=po[qs][:, 256:257]),
                             [r_po[qs]], [r_stt[k2]])
                        S.op("dve", lambda e, k2=k2, qs=qs: e.reciprocal(out=stt[k2][:, 1:2], in_=po[2 + qs][:, 256:257]),
                             [r_po[2 + qs]], [r_stt[k2]])
                        S.op("dve", lambda e, k2=k2: e.tensor_tensor(out=stt[k2][:, 1:2], in0=stt[k2][:, 1:2], in1=lamt[:, 5:6],
                                                                     op=ALU.mult), [r_stt[k2], r_lam], [r_stt[k2]])
                        S.op("dve", lambda e, k2=k2, qs=qs: e.tensor_scalar(out=o32[k2][:, :], in0=po[qs][:, 0:256],
                                                                            scalar1=stt[k2][:, 0:1], scalar2=None, op0=ALU.mult),
                             [r_po[qs], r_stt[k2]], [r_o32[k2]])
                        S.op("dve", lambda e, k2=k2, qs=qs: e.scalar_tensor_tensor(
                            out=o32[k2][:, :], in0=po[2 + qs][:, 0:256], scalar=stt[k2][:, 1:2], in1=o32[k2][:, :],
                            op0=ALU.mult, op1=ALU.add), [r_po[2 + qs], r_stt[k2], r_o32[k2]], [r_o32[k2]])
                        S.op("dve", lambda e, k2=k2: e.memset(stt[k2][:, 2:3], 0.0), [], [r_stt[k2]])
                        S.op("act", lambda e, k2=k2: e.activation(out=junk[:, :], in_=o32[k2][:, :], func=AF.Square,
                                                                  accum_out=stt[k2][:, 2:3]), [r_o32[k2], r_stt[k2]],
                             [r_junk, r_stt[k2]])
                        S.op("dve", lambda e, k2=k2: e.tensor_scalar(out=stt[k2][:, 3:4], in0=stt[k2][:, 2:3], scalar1=1.0 / 256,
                                                                     scalar2=SUBLN_EPS, op0=ALU.mult, op1=ALU.add),
                             [r_stt[k2]], [r_stt[k2]])
                        S.op("act", lambda e, k2=k2: e.activation(out=stt[k2][:, 3:4], in_=stt[k2][:, 3:4], func=AF.Sqrt),
                             [r_stt[k2]], [r_stt[k2]])
                        S.op("dve", lambda e, k2=k2: e.reciprocal(out=stt[k2][:, 3:4], in_=stt[k2][:, 3:4]), [r_stt[k2]], [r_stt[k2]])
                        S.op("dve", lambda e, k2=k2: e.scalar_tensor_tensor(out=obf[k2][:, :], in0=o32[k2][:, :],
                                                                            scalar=stt[k2][:, 3:4], in1=gsub[:, :],
                                                                            op0=ALU.mult, op1=ALU.mult),
                             [r_o32[k2], r_stt[k2], r_gsub], [r_obf[k2]])
                        for j in range(2):
                            S.op("pe", lambda e, k2=k2, j=j: e.transpose(pso[:, j * 128:(j + 1) * 128],
                                                                         obf[k2][:, j * 128:(j + 1) * 128], ident[:, :]),
                                 [r_obf[k2], r_id], [r_pso])
                        S.op("dve", lambda e, k2=k2: e.tensor_copy(out=osb[k2][:, :, :],
                                                                   in_=pso[:, 0:256].rearrange("p (j t) -> p j t", j=2)),
                             [r_pso], [r_osb[k2]])
                        t0 = q0 + qs * 128
                        S.dma("sp", lambda e, k2=k2, h=h, t0=t0: e.dma_start(out=oT_d[:, 2 * h:2 * h + 2, t0:t0 + 128],
                                                                             in_=osb[k2][:, :, :]), [r_osb[k2]], [])
        return phase_end("DA")


    offs_l = es.enter_context(nc.sbuf_tensor("offs_l", [128, NTL, NE], I32))
    offs_c = es.enter_context(nc.sbuf_tensor("offs_c", [128, NTC, NE], I32))
    gsel_l = es.enter_context(nc.sbuf_tensor("gsel_l", [128, NTL, NE], F32))
    gsel_c = es.enter_context(nc.sbuf_tensor("gsel_c", [128, NTC, NE], F32))
    r_offs, r_gsel = R("offs", "gsel")
    NROW_XG = NE * SLOTS

    def phase_E1(layer):
        sets = [(0, NTL, CAP, 0, offs_l, gsel_l)]
        if layer == 0:
            sets.append((N, NTC, CAPC, CAP, offs_c, gsel_c))
        with ExitStack() as ps:
            sb = lambda n, s, d: ps.enter_context(nc.sbuf_tensor(U(n), s, d))
            A = sb("e1_A", [128, NTL, NE], F32)
            cmp = sb("e1_cmp", [128, NTL, NE], F32)
            M = sb("e1_M", [128, NTL, NE], F32)
            s0 = sb("e1_s0", [128, NTL, NE], F32)
            s1 = sb("e1_s1", [128, NTL, NE], F32)
            cc = sb("e1_cc", [128, NTL, NE], F32)
            sm = {n: sb("e1_" + n, [128, NE], F32) for n in ("lo", "hi", "mid", "d1", "d2", "pred", "cnt", "erow")}
            ones = sb("e1_ones", [128, 128], F32)
            lst = sb("e1_lst", [128, 128], F32)
            ptot = ps.enter_context(nc.psum_tensor(U("e1_pt"), [128, 512], F32))
            pcc = [ps.enter_context(nc.psum_tensor(U("e1_pc%d" % i), [128, 512], F32)) for i in range(2)]
            pwi = [ps.enter_context(nc.psum_tensor(U("e1_pw%d" % i), [128, 512], F32)) for i in range(2)]
            r_A, r_cmp, r_M, r_s0, r_s1, r_cc, r_ones, r_lst, r_pt = R("A", "cmp", "M", "s0", "s1", "cc", "ones", "lst", "pt")
            r_pc = R("pc0", "pc1"); r_pw = R("pw0", "pw1")
            rs = {n: Res(n) for n in sm}
            S.op("dve", lambda e: e.memset(ones[:, :], 1.0), [], [r_ones])
            S.dma("sp", lambda e: e.dma_start(out=lst[:, :], in_=lstrict_in[:, :]), [], [r_lst])
            D_ = lambda fn, rd, wr_: S.op("dve", fn, rd, wr_)
            for (tok0, nt, cap, sbase, offs, gsel) in sets:
                Av = A[:, 0:nt, :]
                S.dma("sp", lambda e, Av=Av, tok0=tok0, nt=nt: e.dma_start(
                    out=Av, in_=aff_d[tok0:tok0 + nt * 128, :].rearrange("(c p) e -> p c e", p=128)), [], [r_A])
                D_(lambda e: e.memset(sm["lo"][:, :], 0.0), [], [rs["lo"]])
                D_(lambda e: e.memset(sm["hi"][:, :], 2.0), [], [rs["hi"]])
                for e_ in range(NE):
                    D_(lambda e, e_=e_, sbase=sbase: e.memset(sm["erow"][:, e_:e_ + 1], float(e_ * SLOTS + sbase)), [], [rs["erow"]])
                bc = lambda t, nt=nt: t[:, :].unsqueeze(1).to_broadcast([128, nt, NE])
                for itr in range(36):
                    D_(lambda e: e.tensor_tensor(out=sm["mid"][:, :], in0=sm["lo"][:, :], in1=sm["hi"][:, :], op=ALU.add),
                       [rs["lo"], rs["hi"]], [rs["mid"]])
                    D_(lambda e: e.tensor_scalar(out=sm["mid"][:, :], in0=sm["mid"][:, :], scalar1=0.5, scalar2=None, op0=ALU.mult),
                       [rs["mid"]], [rs["mid"]])
                    D_(lambda e, Av=Av, nt=nt, bc=bc: e.tensor_tensor(out=cmp[:, 0:nt, :], in0=Av, in1=bc(sm["mid"]), op=ALU.is_ge),
                       [r_A, rs["mid"]], [r_cmp])
                    D_(lambda e, nt=nt: e.tensor_reduce(out=sm["cnt"][:, :], in_=cmp[:, 0:nt, :].rearrange("p c e -> p e c"),
                                                        axis=AX.X, op=ALU.add), [r_cmp], [rs["cnt"]])
                    S.op("pe", lambda e: e.matmul(ptot[:, 0:NE], ones[:, :], sm["cnt"][:, :], start=True, stop=True),
                         [r_ones, rs["cnt"]], [r_pt])
                    D_(lambda e, cap=cap: e.tensor_scalar(out=sm["pred"][:, :], in0=ptot[:, 0:NE], scalar1=float(cap) - 0.5,
                                                          scalar2=None, op0=ALU.is_ge), [r_pt], [rs["pred"]])
                    D_(lambda e: e.tensor_tensor(out=sm["d1"][:, :], in0=sm["mid"][:, :], in1=sm["lo"][:, :], op=ALU.subtract),
                       [rs["mid"], rs["lo"]], [rs["d1"]])
                    D_(lambda e: e.tensor_tensor(out=sm["d1"][:, :], in0=sm["d1"][:, :], in1=sm["pred"][:, :], op=ALU.mult),
                       [rs["d1"], rs["pred"]], [rs["d1"]])
                    D_(lambda e: e.tensor_tensor(out=sm["d2"][:, :], in0=sm["hi"][:, :], in1=sm["mid"][:, :], op=ALU.subtract),
                       [rs["mid"], rs["hi"]], [rs["d2"]])
                    D_(lambda e: e.tensor_tensor(out=sm["d2"][:, :], in0=sm["d2"][:, :], in1=sm["pred"][:, :], op=ALU.mult),
                       [rs["d2"], rs["pred"]], [rs["d2"]])
                    D_(lambda e: e.tensor_tensor(out=sm["lo"][:, :], in0=sm["lo"][:, :], in1=sm["d1"][:, :], op=ALU.add),
                       [rs["lo"], rs["d1"]], [rs["lo"]])
                    D_(lambda e: e.tensor_tensor(out=sm["hi"][:, :], in0=sm["mid"][:, :], in1=sm["d2"][:, :], op=ALU.add),
                       [rs["mid"], rs["d2"]], [rs["hi"]])
                D_(lambda e, Av=Av, nt=nt, bc=bc: e.tensor_tensor(out=M[:, 0:nt, :], in0=Av, in1=bc(sm["lo"]), op=ALU.is_ge),
                   [r_A, rs["lo"]], [r_M])
                ncol = nt * NE
                Mf = M[:, :, :].rearrange("p c e -> p (c e)")
                nh = (ncol + 511) // 512
                for h in range(nh):
                    w_ = min(512, ncol - h * 512)
                    S.op("pe", lambda e, h=h, w_=w_: e.matmul(pcc[h][:, 0:w_], ones[:, :], Mf[:, h * 512:h * 512 + w_],
                                                             start=True, stop=True), [r_ones, r_M], [r_pc[h]])
                    S.op("pe", lambda e, h=h, w_=w_: e.matmul(pwi[h][:, 0:w_], lst[:, :], Mf[:, h * 512:h * 512 + w_],
                                                             start=True, stop=True), [r_lst, r_M], [r_pw[h]])
                    D_(lambda e, h=h, w_=w_: e.tensor_copy(out=cc[:, :, :].rearrange("p c e -> p (c e)")[:, h * 512:h * 512 + w_],
                                                          in_=pcc[h][:, 0:w_]), [r_pc[h]], [r_cc])
                D_(lambda e, nt=nt: e.tensor_copy(out=s0[:, 0:nt, :], in_=cc[:, 0:nt, :]), [r_cc], [r_s0])
                src, dst, r_src, r_dst = s0, s1, r_s0, r_s1
                d = 1
                while d < nt:
                    D_(lambda e, src=src, dst=dst, d=d, nt=nt: e.tensor_tensor(out=dst[:, d:nt, :], in0=src[:, d:nt, :],
                                                                             in1=src[:, 0:nt - d, :], op=ALU.add), [r_src], [r_dst])
                    D_(lambda e, src=src, dst=dst, d=d: e.tensor_copy(out=dst[:, 0:d, :], in_=src[:, 0:d, :]), [r_src], [r_dst])
                    src, dst, r_src, r_dst = dst, src, r_dst, r_src
                    d *= 2
                D_(lambda e, src=src, nt=nt: e.tensor_tensor(out=src[:, 0:nt, :], in0=src[:, 0:nt, :], in1=cc[:, 0:nt, :],
                                                            op=ALU.subtract), [r_src, r_cc], [r_src])
                srcf = src[:, :, :].rearrange("p c e -> p (c e)")
                for h in range(nh):
                    w_ = min(512, ncol - h * 512)
                    D_(lambda e, h=h, w_=w_, srcf=srcf: e.tensor_tensor(out=srcf[:, h * 512:h * 512 + w_],
                                                                       in0=srcf[:, h * 512:h * 512 + w_], in1=pwi[h][:, 0:w_],
                                                                       op=ALU.add), [r_src, r_pw[h]], [r_src])
                D_(lambda e, src=src, nt=nt, cap=cap: e.tensor_scalar(out=cmp[:, 0:nt, :], in0=src[:, 0:nt, :],
                                                                     scalar1=float(cap) - 0.5, scalar2=None, op0=ALU.is_lt),
                   [r_src], [r_cmp])
                D_(lambda e, nt=nt: e.tensor_tensor(out=M[:, 0:nt, :], in0=M[:, 0:nt, :], in1=cmp[:, 0:nt, :], op=ALU.mult),
                   [r_M, r_cmp], [r_M])
                D_(lambda e, gsel=gsel, Av=Av, nt=nt: e.tensor_tensor(out=gsel[:, :, :], in0=Av, in1=M[:, 0:nt, :], op=ALU.mult),
                   [r_A, r_M], [r_gsel])
                D_(lambda e, src=src, nt=nt, bc=bc: e.tensor_tensor(out=src[:, 0:nt, :], in0=src[:, 0:nt, :], in1=bc(sm["erow"]),
                                                                   op=ALU.add), [r_src, rs["erow"]], [r_src])
                D_(lambda e, src=src, nt=nt: e.tensor_scalar(out=src[:, 0:nt, :], in0=src[:, 0:nt, :], scalar1=-BIG, scalar2=None,
                                                            op0=ALU.add), [r_src], [r_src])
                D_(lambda e, src=src, nt=nt: e.tensor_tensor(out=src[:, 0:nt, :], in0=src[:, 0:nt, :], in1=M[:, 0:nt, :],
                                                            op=ALU.mult), [r_src, r_M], [r_src])
                D_(lambda e, src=src, nt=nt: e.tensor_scalar(out=src[:, 0:nt, :], in0=src[:, 0:nt, :], scalar1=BIG, scalar2=None,
                                                            op0=ALU.add), [r_src], [r_src])
                D_(lambda e, src=src, nt=nt, offs=offs: e.tensor_copy(out=offs[:, :, :], in_=src[:, 0:nt, :]), [r_src], [r_offs])
        return phase_end("E1_%d" % layer)

    def phase_E2(layer):
        sets = [(0, NTL, offs_l)]
        if layer == 0:
            sets.append((N, NTC, offs_c))
        with ExitStack() as ps:
            sb = lambda n, s, d: ps.enter_context(nc.sbuf_tensor(U(n), s, d))
            h2t = [sb("e2_h%d" % i, [128, D], BF16) for i in range(3)]
            r_h = R("h0", "h1", "h2")
            bcr = {}

            def mkreg(e):
                bcr["r"] = e.alloc_register(U("e2_bc"))
                return e.reg_mov(bcr["r"], NROW_XG - 1)
            S.raw("pool", mkreg)
            it = 0
            for (tok0, nt, offs) in sets:
                for c in range(nt):
                    k = it % 3; it += 1
                    t0 = tok0 + c * 128
                    S.dma("sp", lambda e, k=k, t0=t0: e.dma_start(out=h2t[k][:, :], in_=h2_d[t0:t0 + 128, :]), [], [r_h[k]])
                    for e_ in range(NE):
                        S.dma("pool", lambda e, k=k, c=c, e_=e_, offs=offs: e.indirect_dma_start(
                            out=xg_d[:, :], out_offset=bass.IndirectOffsetOnAxis(ap=offs[:, c, e_:e_ + 1], axis=0),
                            in_=h2t[k][:, :], in_offset=None, bounds_check=bcr["r"], oob_is_err=False),
                            [r_h[k], r_offs], [])
            S.raw("pool", lambda e: (e.free_register(bcr["r"]), None)[1])
        return phase_end("E2_%d" % layer)

    def phase_E3(layer):
        SL = SLOTS if layer == 0 else CAP
        stiles = [(i * 128, 128) for i in range(8)] + ([(CAP, CAPC)] if layer == 0 else [])
        chunks = [(0, 512), (512, 512)] + ([(CAP, CAPC)] if layer == 0 else [])
        with ExitStack() as ps:
            sb = lambda n, s, d: ps.enter_context(nc.sbuf_tensor(U(n), s, d))
            xgt = [sb("e3_x%d" % i, [128, D], BF16) for i in range(2)]
            xgT = sb("e3_xT", [128, KC, SLOTS], BF16)
            wgb = [sb("e3_wg%d" % i, [128, KC, 256], BF16) for i in range(2)]
            wub = [sb("e3_wu%d" % i, [128, KC, 256], BF16) for i in range(2)]
            aT = sb("e3_aT", [128, 8, SLOTS], BF16)
            wdb = [sb("e3_wd%d" % i, [128, 8, 512], BF16) for i in range(2)]
            sg = [sb("e3_sg%d" % i, [128, 512], F32) for i in range(2)]
            ysb = [sb("e3_y%d" % i, [128, 512], BF16) for i in range(2)]
            ident = sb("e3_id", [128, 128], BF16)
            ptr = [ps.enter_context(nc.psum_tensor(U("e3_pt%d" % i), [128, 1024], BF16)) for i in range(2)]
            pg = [ps.enter_context(nc.psum_tensor(U("e3_pg%d" % i), [128, 512], F32)) for i in range(2)]
            pu = [ps.enter_context(nc.psum_tensor(U("e3_pu%d" % i), [128, 512], F32)) for i in range(2)]
            py = [ps.enter_context(nc.psum_tensor(U("e3_py%d" % i), [128, 512], F32)) for i in range(2)]
            r_x = R("x0", "x1"); r_wg = R("wg0", "wg1"); r_wu = R("wu0", "wu1"); r_wd = R("wd0", "wd1")
            r_sg = R("sg0", "sg1"); r_y = R("y0", "y1"); r_pt = R("pt0", "pt1"); r_pg = R("pg0", "pg1")
            r_pu = R("pu0", "pu1"); r_py = R("py0", "py1")
            r_xT, r_aT, r_id = R("xT", "aT", "id")
            S.dma("pool", lambda e: e.dma_start(out=ident[:, :], in_=ident_in[:, :]), [], [r_id])
            xi = 0; ti = 0; wi = 0; gi = 0; di = 0; yi = 0
            for ex in range(NE):
                row0 = ex * SLOTS
                for (s0_, P_) in stiles:
                    k = xi % 2; xi += 1
                    S.dma("sp", lambda e, k=k, row0=row0, s0_=s0_, P_=P_: e.dma_start(
                        out=xgt[k][0:P_, :], in_=xg_d[row0 + s0_:row0 + s0_ + P_, :]), [], [r_x[k]])
                    for g in range(4):
                        tk = ti % 2; ti += 1
                        for j in range(8):
                            kc = g * 8 + j
                            fn = lambda e, k=k, tk=tk, j=j, kc=kc, P_=P_: e.transpose(
                                ptr[tk][:, j * P_:(j + 1) * P_], xgt[k][0:P_, kc * 128:(kc + 1) * 128], ident[0:P_, 0:P_])
                            if j == 0 or j == 7:
                                S.op("pe", fn, [r_x[k], r_id], [r_pt[tk]])
                            else:
                                S.pe_quiet(fn)
                        eng = "act" if g % 2 == 0 else "dve"
                        o_ = xgT[:, g * 8:(g + 1) * 8, s0_:s0_ + P_]
                        i_ = ptr[tk][:, 0:8 * P_].rearrange("p (j t) -> p j t", j=8)
                        if eng == "act":
                            S.op("act", lambda e, o_=o_, i_=i_: e.activation(out=o_, in_=i_, func=AF.Copy), [r_pt[tk]], [r_xT])
                        else:
                            S.op("dve", lambda e, o_=o_, i_=i_: e.tensor_copy(out=o_, in_=i_), [r_pt[tk]], [r_xT])
                for fb in range(4):
                    wk = wi % 2; wi += 1
                    S.dma("pool", lambda e, wk=wk, ex=ex, fb=fb: e.dma_start(
                        out=wgb[wk][:, :, :], in_=wg[layer, ex, :, fb * 256:(fb + 1) * 256].rearrange("(kc p) n -> p kc n", p=128)),
                        [], [r_wg[wk]])
                    S.dma("pool", lambda e, wk=wk, ex=ex, fb=fb: e.dma_start(
                        out=wub[wk][:, :, :], in_=wu[layer, ex, :, fb * 256:(fb + 1) * 256].rearrange("(kc p) n -> p kc n", p=128)),
                        [], [r_wu[wk]])
                    for sub in range(2):
                        f8 = fb * 2 + sub
                        for (c0, cn) in chunks:
                            g_ = gi % 2; gi += 1
                            mm_group(pg[g_][:, 0:cn], [(wgb[wk][:, kc, sub * 128:(sub + 1) * 128], xgT[:, kc, c0:c0 + cn])
                                                       for kc in range(KC)], [r_wg[wk], r_xT], r_pg[g_])
                            mm_group(pu[g_][:, 0:cn], [(wub[wk][:, kc, sub * 128:(sub + 1) * 128], xgT[:, kc, c0:c0 + cn])
                                                       for kc in range(KC)], [r_wu[wk], r_xT], r_pu[g_])
                            S.op("act", lambda e, g_=g_, cn=cn: e.activation(out=sg[g_][:, 0:cn], in_=pg[g_][:, 0:cn], func=AF.Silu),
                                 [r_pg[g_]], [r_sg[g_]])
                            S.op("dve", lambda e, g_=g_, cn=cn, c0=c0, f8=f8: e.tensor_tensor(
                                out=aT[:, f8, c0:c0 + cn], in0=sg[g_][:, 0:cn], in1=pu[g_][:, 0:cn], op=ALU.mult),
                                [r_sg[g_], r_pu[g_]], [r_aT])
                for nb in range(8):
                    dk = di % 2; di += 1
                    S.dma("pool", lambda e, dk=dk, ex=ex, nb=nb: e.dma_start(
                        out=wdb[dk][:, :, :], in_=wd[layer, ex, :, nb * 512:(nb + 1) * 512].rearrange("(fc p) n -> p fc n", p=128)),
                        [], [r_wd[dk]])
                    for (s0_, P_) in stiles:
                        yk = yi % 2; yi += 1
                        mm_group(py[yk][0:P_, :], [(aT[:, fc, s0_:s0_ + P_], wdb[dk][:, fc, :]) for fc in range(8)],
                                 [r_aT, r_wd[dk]], r_py[yk])
                        if yk == 0:
                            S.op("act", lambda e, yk=yk, P_=P_: e.activation(out=ysb[yk][0:P_, :], in_=py[yk][0:P_, :], func=AF.Copy),
                                 [r_py[yk]], [r_y[yk]])
                        else:
                            S.op("dve", lambda e, yk=yk, P_=P_: e.tensor_copy(out=ysb[yk][0:P_, :], in_=py[yk][0:P_, :]),
                                 [r_py[yk]], [r_y[yk]])
                        S.dma("sp", lambda e, yk=yk, row0=row0, s0_=s0_, P_=P_, nb=nb: e.dma_start(
                            out=y_d[row0 + s0_:row0 + s0_ + P_, nb * 512:(nb + 1) * 512], in_=ysb[yk][0:P_, :]), [r_y[yk]], [])
        return phase_end("E3_%d" % layer)

    def phase_T3(layer):
        last = layer == 1
        sets = [(0, NTL, 0, offs_l, gsel_l)]
        if not last:
            sets.append((N, NTC, 1, offs_c, gsel_c))
        with ExitStack() as ps:
            sb = lambda n, s, d: ps.enter_context(nc.sbuf_tensor(U(n), s, d))
            x1t = [sb("t3_x%d" % i, [128, D], F32) for i in range(2)]
            macc = [sb("t3_m%d" % i, [128, D], F32) for i in range(2)]
            G = [sb("t3_g%d" % i, [128, D], BF16) for i in range(4)]
            g5 = sb("t3_g5", [128, D], F32)
            r_x = R("x0", "x1"); r_m = R("m0", "m1"); r_G = R("G0", "G1", "G2", "G3"); r_g5, = R("g5")
            if last:
                fg = sb("t3_fg", [128, D], F32)
                junk = sb("t3_junk", [128, D], BF16)
                st = [sb("t3_st%d" % i, [128, 2], F32) for i in range(2)]
                r_fg, r_junk = R("fg", "junk"); r_st = R("st0", "st1")
                load_rows_bcast("sp", fg[:, :], fing[0:1, :], r_fg)
            for i in range(4):
                S.op("pool", lambda e, i=i: e.memset(G[i][:, :], 0.0), [], [r_G[i]])
            bcr = {}

            def mkreg(e):
                bcr["r"] = e.alloc_register(U("t3_bc"))
                return e.reg_mov(bcr["r"], NROW_XG - 1)
            S.raw("pool", mkreg)
            it = 0; gi = 0
            for (tok0, nt, row, offs, gsel) in sets:
                load_rows_bcast("sp", g5[:, :], mod_d[layer, row:row + 1, 5 * D:6 * D], r_g5)
                for c in range(nt):
                    k = it % 2; it += 1
                    t0 = tok0 + c * 128
                    S.dma("sp", lambda e, k=k, t0=t0: e.dma_start(out=x1t[k][:, :], in_=x1_d[t0:t0 + 128, :]), [], [r_x[k]])
                    for e_ in range(NE):
                        g = gi % 4; gi += 1
                        S.dma("pool", lambda e, g=g, c=c, e_=e_, offs=offs: e.indirect_dma_start(
                            out=G[g][:, :], out_offset=None, in_=y_d[:, :],
                            in_offset=bass.IndirectOffsetOnAxis(ap=offs[:, c, e_:e_ + 1], axis=0),
                            bounds_check=bcr["r"], oob_is_err=False), [r_offs], [r_G[g]])
                        if e_ == 0:
                            S.op("dve", lambda e, g=g, k=k, c=c, gsel=gsel: e.tensor_scalar(
                                out=macc[k][:, :], in0=G[g][:, :], scalar1=gsel[:, c, 0:1], scalar2=None, op0=ALU.mult),
                                [r_G[g], r_gsel], [r_m[k]])
                        else:
                            S.op("dve", lambda e, g=g, k=k, c=c, e_=e_, gsel=gsel: e.scalar_tensor_tensor(
                                out=macc[k][:, :], in0=G[g][:, :], scalar=gsel[:, c, e_:e_ + 1], in1=macc[k][:, :],
                                op0=ALU.mult, op1=ALU.add), [r_G[g], r_gsel, r_m[k]], [r_m[k]])
                    S.op("dve", lambda e, k=k: e.tensor_tensor(out=macc[k][:, :], in0=macc[k][:, :], in1=g5[:, :], op=ALU.mult),
                         [r_m[k], r_g5], [r_m[k]])
                    S.op("pool", lambda e, k=k: e.tensor_tensor(out=x1t[k][:, :], in0=x1t[k][:, :], in1=macc[k][:, :], op=ALU.add),
                         [r_x[k], r_m[k]], [r_x[k]])
                    if not last:
                        S.dma("sp", lambda e, k=k, t0=t0: e.dma_start(out=x2_d[t0:t0 + 128, :], in_=x1t[k][:, :]), [r_x[k]], [])
                    else:
                        S.op("dve", lambda e, k=k: e.memset(st[k][:, 0:1], 0.0), [], [r_st[k]])
                        S.op("act", lambda e, k=k: e.activation(out=junk[:, :], in_=x1t[k][:, :], func=AF.Square,
                                                                accum_out=st[k][:, 0:1]), [r_x[k], r_st[k]], [r_junk, r_st[k]])
                        S.op("dve", lambda e, k=k: e.tensor_scalar(out=st[k][:, 1:2], in0=st[k][:, 0:1], scalar1=1.0 / D,
                                                                   scalar2=EPS, op0=ALU.mult, op1=ALU.add), [r_st[k]], [r_st[k]])
                        S.op("act", lambda e, k=k: e.activation(out=st[k][:, 1:2], in_=st[k][:, 1:2], func=AF.Sqrt),
                             [r_st[k]], [r_st[k]])
                        S.op("dve", lambda e, k=k: e.reciprocal(out=st[k][:, 1:2], in_=st[k][:, 1:2]), [r_st[k]], [r_st[k]])
                        S.op("dve", lambda e, k=k: e.scalar_tensor_tensor(out=x1t[k][:, :], in0=x1t[k][:, :], scalar=st[k][:, 1:2],
                                                                          in1=fg[:, :], op0=ALU.mult, op1=ALU.mult),
                             [r_x[k], r_st[k], r_fg], [r_x[k]])
                        S.dma("sp", lambda e, k=k, t0=t0: e.dma_start(out=out_d[t0:t0 + 128, :], in_=x1t[k][:, :]), [r_x[k]], [])
            S.raw("pool", lambda e: (e.free_register(bcr["r"]), None)[1])
        return phase_end("T3_%d" % layer)

    plan = [("A", phase_A), ("T1_0", lambda: phase_T1(0)), ("H1_0", lambda: phase_H1(0)), ("N", phase_N),
            ("T2a_0", lambda: phase_T2a(0)), ("T2b_0", lambda: phase_T2b(0)),
            ("E1_0", lambda: phase_E1(0)), ("E2_0", lambda: phase_E2(0)), ("E3_0", lambda: phase_E3(0)),
            ("T3_0", lambda: phase_T3(0)),
            ("T1_1", lambda: phase_T1(1)), ("H1_1", lambda: phase_H1(1)), ("DA", phase_DA),
            ("T2a_1", lambda: phase_T2a(1)), ("T2b_1", lambda: phase_T2b(1)),
            ("E1_1", lambda: phase_E1(1)), ("E2_1", lambda: phase_E2(1)), ("E3_1", lambda: phase_E3(1)),
            ("T3_1", lambda: phase_T3(1))]
    for name, fn in plan:
        if phases is not None and name not in phases:
            continue
        if fn():
            break

    es.close()
    nc._declared_inputs = list(declared.keys())
    return nc, list(declared.keys())


def _host_constants():
    ident = np.eye(128, dtype=np.float32)
    lstrict = np.triu(np.ones((128, 128), np.float32), 1)
    qc = np.arange(GRID_W)
    col_start = np.clip(qc - 8, 0, GRID_W - 16)
    col_mask = (qc[None, :] >= col_start[:, None]) & (qc[None, :] < col_start[:, None] + 16)
    nmask = np.where(col_mask, 0.0, -1e30).astype(np.float32)
    col_idx = np.clip(qc[None, :] - qc[:, None] + 15, 0, 30)
    t = np.arange(N)
    row = (t // GRID_W).astype(np.float32)
    col = (t % GRID_W).astype(np.float32)
    freq = (10000.0 ** (-np.arange(32, dtype=np.float32) / 32)).astype(np.float32)
    ang = np.concatenate([row[:, None] * freq, col[:, None] * freq], axis=-1).astype(np.float32)
    return dict(ident=ident, lstrict=lstrict, nmask=nmask, col_idx=col_idx,
                rope_cos=np.cos(ang).astype(np.float32), rope_sin=np.sin(ang).astype(np.float32))


def make_in_maps(inp):
    cst = _host_constants()
    f = lambda a: np.ascontiguousarray(np.asarray(a, dtype=np.float32))
    rpb = f(inp["na_rpb"])[0]
    nb = rpb[:, :, cst["col_idx"]]
    nb = np.ascontiguousarray(nb.transpose(0, 2, 1, 3)).reshape(32, 64, 15 * 64)
    lam = np.concatenate([f(inp[k]) for k in ("da_lambda_q1", "da_lambda_k1", "da_lambda_q2", "da_lambda_k2")], 0)
    shared = dict(
        ada_w=f(inp["ada_w"]), ada_b=f(inp["ada_b"]), norm1_g=f(inp["norm1_g"]), norm2_g=f(inp["norm2_g"]),
        final_g=f(inp["final_g"]).reshape(1, D), na_w_qkv=f(inp["na_w_qkv"])[0], da_w_qkv=f(inp["da_w_qkv"])[0],
        na_w_o=f(inp["na_w_o"])[0], da_w_o=f(inp["da_w_o"])[0], nbias=nb, nmask=cst["nmask"], lam=lam,
        subln_g=f(inp["da_subln_g"]).reshape(1, 256), w_router=f(inp["moe_w_router"]), w_gate=f(inp["moe_w_gate"]),
        w_up=f(inp["moe_w_up"]), w_down=f(inp["moe_w_down"]), ident=cst["ident"], lstrict=cst["lstrict"],
        rope_cos=cst["rope_cos"], rope_sin=cst["rope_sin"])
    maps = []
    x = f(inp["x"]); c = f(inp["c"]); ctx = f(inp["ctx"]); cc = f(inp["c_ctx"])
    for b in range(2):
        cv = np.stack([c[b], cc], axis=-1)
        cT = np.ascontiguousarray(cv.reshape(KC, 128, 2).transpose(1, 0, 2))
        m = dict(shared)
        m.update(x=x[b], ctx=ctx[b], cT=cT)
        maps.append(m)
    return maps


def kernel(**inputs):
    nc, names = build_program()
    maps = [{k: m[k] for k in names} for m in make_in_maps(inputs)]
    res = run_bass_kernel_spmd(nc, maps, core_ids=[0, 1])
    return np.stack([np.asarray(res.results[b]["out"], dtype=np.float32) for b in range(2)], 0)
```

```python
import numpy as np
from contextlib import ExitStack
import concourse.bass as bass
import concourse.mybir as mybir
from concourse.bass_utils import run_bass_kernel_spmd

F32 = mybir.dt.float32
BF16 = mybir.dt.bfloat16
I32 = mybir.dt.int32
AF = mybir.ActivationFunctionType
ALU = mybir.AluOpType
AX = mybir.AxisListType

D = 4096
KC = D // 128
N = 8192
NCX = 256
NT = N + NCX
NTL = N // 128
NTC = NCX // 128
NE = 16
FF = 1024
CAP = 1024
CAPC = 32
SLOTS = CAP + CAPC
GRID_W = 64
NROWS = N // GRID_W
EPS = 1e-6
SUBLN_EPS = 1e-5
BIG = 1.0e6


class Res:
    __slots__ = ("name", "w", "r")

    def __init__(self, name):
        self.name = name
        self.w = None
        self.r = {}


class Sched:
    CE = ("pe", "act", "dve", "pool")

    def __init__(self, nc, es, ndma=12):
        self.nc = nc
        self.semobj = {}
        self.cnt = {}
        for e in self.CE:
            self.semobj[e] = es.enter_context(nc.semaphore("s_" + e))
            self.cnt[e] = 0
        self.queues = ("sp", "pool")
        self.dkeys = {}
        self.dnext = {}
        for q in self.queues:
            ks = []
            for k in range(ndma):
                key = (q, k)
                self.semobj[key] = es.enter_context(nc.semaphore("d_%s%d" % (q, k)))
                self.cnt[key] = 0
                ks.append(key)
            self.dkeys[q] = ks
            self.dnext[q] = 0
        self.issuers = ("pe", "act", "dve", "pool", "sp")
        self.waited = {i: {} for i in self.issuers}
        self.thunks = {i: [] for i in self.issuers}
        self.ninst = 0

    def _wait(self, issuer, ev):
        if ev is None:
            return
        key, val = ev
        if self.waited[issuer].get(key, 0) >= val:
            return
        self.waited[issuer][key] = val
        sem = self.semobj[key]
        self.thunks[issuer].append(lambda e, sem=sem, val=val: e.wait_ge(sem, val))

    def _deps(self, issuer, reads, writes):
        for r in reads:
            if r.w is not None and not (issuer == "pe" and r.w[0] == "pe"):
                self._wait(issuer, r.w)
        for w in writes:
            if w.w is not None and not (issuer == "pe" and w.w[0] == "pe"):
                self._wait(issuer, w.w)
            for key, val in w.r.items():
                if not (issuer == "pe" and key == "pe"):
                    self._wait(issuer, (key, val))

    def _mark(self, ev, reads, writes):
        key, val = ev
        for r in reads:
            if r.r.get(key, 0) < val:
                r.r[key] = val
        for w in writes:
            w.w = ev
            w.r = {}

    def op(self, eng, fn, reads=(), writes=()):
        self._deps(eng, reads, writes)
        self.cnt[eng] += 1
        val = self.cnt[eng]
        sem = self.semobj[eng]
        self.thunks[eng].append(lambda e, fn=fn, sem=sem: fn(e).then_inc(sem, 1))
        ev = (eng, val)
        self._mark(ev, reads, writes)
        self.ninst += 1
        return ev

    def raw(self, issuer, fn):
        self.thunks[issuer].append(lambda e, fn=fn: fn(e))

    def pe_quiet(self, fn):
        self.thunks["pe"].append(lambda e, fn=fn: fn(e))
        self.ninst += 1

    def dma(self, q, fn, reads=(), writes=()):
        self._deps(q, reads, writes)
        ks = self.dkeys[q]
        key = ks[self.dnext[q] % len(ks)]
        self.dnext[q] += 1
        if self.cnt[key] > 0:
            self._wait(q, (key, self.cnt[key]))
        self.cnt[key] += 16
        val = self.cnt[key]
        sem = self.semobj[key]
        self.thunks[q].append(lambda e, fn=fn, sem=sem: fn(e).then_inc(sem, 16))
        ev = (key, val)
        self._mark(ev, reads, writes)
        self.ninst += 1
        return ev

    def flush(self, name):
        nc = self.nc
        for q in self.queues:
            for key in self.dkeys[q]:
                if self.cnt[key] > 0:
                    self._wait(q, (key, self.cnt[key]))
        th = self.thunks
        with nc.Block(name) as block:
            if th["sp"]:
                @block.sync
                def _(e):
                    for t in th["sp"]:
                        t(e)
            if th["pe"]:
                @block.tensor
                def _(e):
                    for t in th["pe"]:
                        t(e)
            if th["act"]:
                @block.scalar
                def _(e):
                    for t in th["act"]:
                        t(e)
            if th["dve"]:
                @block.vector
                def _(e):
                    for t in th["dve"]:
                        t(e)
            if th["pool"]:
                @block.gpsimd
                def _(e):
                    for t in th["pool"]:
                        t(e)
        self.thunks = {i: [] for i in self.issuers}
        for i in self.issuers:
            for key, val in self.cnt.items():
                self.waited[i][key] = val


def R(*names):
    return [Res(n) for n in names]


def build_program(stop_after=None, debug=(), inject=(), phases=None):
    nc = bass.Bass("TRN2", target_bir_lowering=False)
    es = ExitStack()
    S = Sched(nc, es)

    declared = {}

    class _Lazy:
        def __init__(self, name, shape):
            self.name, self.shape, self.t = name, list(shape), None

        def _get(self):
            if self.t is None:
                self.t = nc.dram_tensor(self.name, self.shape, F32, kind="ExternalInput")
                declared[self.name] = self.t
            return self.t

        def __getitem__(self, key):
            return self._get()[key]

    def din(name, shape):
        return _Lazy(name, shape)

    def dscr(name, shape, dt):
        kind = "ExternalOutput" if name in debug else ("ExternalInput" if name in inject else "Internal")
        return nc.dram_tensor(name, list(shape), dt, kind=kind)

    x_in = din("x", [N, D])
    ctx_in = din("ctx", [NCX, D])
    cT_in = din("cT", [128, KC, 2])
    ada_w = din("ada_w", [2, D, 6 * D])
    ada_b = din("ada_b", [2, 6 * D])
    n1g = din("norm1_g", [2, D])
    n2g = din("norm2_g", [2, D])
    fing = din("final_g", [1, D])
    wqkv = [din("na_w_qkv", [D, 3 * D]), din("da_w_qkv", [D, 3 * D])]
    wo = [din("na_w_o", [D, D]), din("da_w_o", [D, D])]
    nbias = din("nbias", [32, 64, 15 * 64])
    nmask = din("nmask", [64, 64])
    lamp = din("lam", [4, 128])
    sublng = din("subln_g", [1, 256])
    wr = din("w_router", [2, D, NE])
    wg = din("w_gate", [2, NE, D, FF])
    wu = din("w_up", [2, NE, D, FF])
    wd = din("w_down", [2, NE, FF, D])
    ident_in = din("ident", [128, 128])
    lstrict_in = din("lstrict", [128, 128])
    rope_cos = din("rope_cos", [N, 64])
    rope_sin = din("rope_sin", [N, 64])
    out_d = nc.dram_tensor("out", [N, D], F32, kind="ExternalOutput")

    mod_d = dscr("mod_d", [2, 2, 6 * D], F32)
    hT_d = dscr("hT_d", [128, KC, NT], BF16)
    qkv_d = dscr("qkv_d", [NT, 3 * D], BF16)
    oT_d = dscr("oT_d", [128, KC, NT], BF16)
    x1_d = dscr("x1_d", [NT, D], F32)
    x2_d = dscr("x2_d", [NT, D], F32)
    h2_d = dscr("h2_d", [NT, D], BF16)
    aff_d = dscr("aff_d", [NT, NE], F32)
    xg_d = dscr("xg_d", [NE * SLOTS, D], BF16)
    y_d = dscr("y_d", [NE * SLOTS, D], BF16)

    state = {"done": False, "uid": 0}

    def U(n):
        return "%s_u%d" % (n, state["uid"])

    def phase_end(name):
        S.flush(name)
        state["uid"] += 1
        if stop_after == name:
            state["done"] = True
        return state["done"]

    def phase_A():
        with ExitStack() as ps:
            sb = lambda n, s, d: ps.enter_context(nc.sbuf_tensor(U(n), s, d))
            cT = sb("a_cT", [128, KC, 2], F32)
            scT = sb("a_scT", [128, KC, 2], F32)
            wbuf = [sb("a_w%d" % i, [128, KC, 512], F32) for i in range(2)]
            bt = [sb("a_b%d" % i, [2, 512], F32) for i in range(2)]
            ot = [sb("a_o%d" % i, [2, 512], F32) for i in range(2)]
            pacc = [ps.enter_context(nc.psum_tensor(U("a_p%d" % i), [128, 512], F32)) for i in range(2)]
            r_cT, r_scT = R("cT", "scT")
            r_w = R("w0", "w1"); r_b = R("b0", "b1"); r_o = R("o0", "o1"); r_p = R("p0", "p1")
            S.dma("sp", lambda e: e.dma_start(out=cT[:, :, :], in_=cT_in[:, :, :]), [], [r_cT])
            S.op("act", lambda e: e.activation(out=scT[:, :, :], in_=cT[:, :, :], func=AF.Silu), [r_cT], [r_scT])
            it = 0
            for i in range(2):
                for cb in range(6 * D // 512):
                    k = it % 2
                    it += 1
                    c0 = cb * 512
                    S.dma("sp", lambda e, i=i, c0=c0, k=k: e.dma_start(
                        out=wbuf[k][:, :, :],
                        in_=ada_w[i, :, c0:c0 + 512].rearrange("(kc p) n -> p kc n", p=128)), [], [r_w[k]])
                    S.dma("sp", lambda e, i=i, c0=c0, k=k: e.dma_start(
                        out=bt[k][:, :], in_=ada_b[i:i + 1, c0:c0 + 512].partition_broadcast(2)), [], [r_b[k]])
                    for kc in range(KC):
                        fn = lambda e, k=k, kc=kc: e.matmul(pacc[k][0:2, :], scT[:, kc, :], wbuf[k][:, kc, :],
                                                            start=(kc == 0), stop=(kc == KC - 1))
                        if kc < KC - 1:
                            if kc == 0:
                                S.op("pe", fn, [r_scT, r_w[k]], [r_p[k]])
                            else:
                                S.pe_quiet(fn)
                        else:
                            S.op("pe", fn, [r_scT, r_w[k]], [r_p[k]])
                    S.op("dve", lambda e, k=k: e.tensor_tensor(out=ot[k][:, :], in0=pacc[k][0:2, :], in1=bt[k][:, :],
                                                               op=ALU.add), [r_p[k], r_b[k]], [r_o[k]])
                    S.dma("sp", lambda e, i=i, c0=c0, k=k: e.dma_start(out=mod_d[i, :, c0:c0 + 512], in_=ot[k][:, :]),
                          [r_o[k]], [])
        return phase_end("A")

    def src_rows(layer, t0, n):
        if layer == 0:
            if t0 < N:
                return x_in[t0:t0 + n, :]
            return ctx_in[t0 - N:t0 - N + n, :]
        return x2_d[t0:t0 + n, :]

    def load_rows_bcast(q, dst, src_ap, res):
        S.dma(q, lambda e: e.dma_start(out=dst, in_=src_ap.partition_broadcast(128)), [], [res])

    def phase_T1(layer):
        with ExitStack() as ps:
            sb = lambda n, s, d: ps.enter_context(nc.sbuf_tensor(U(n), s, d))
            xt = [sb("t1_x%d" % i, [128, D], F32) for i in range(2)]
            hb = [sb("t1_h%d" % i, [128, D], BF16) for i in range(2)]
            hT4 = [sb("t1_hT%d" % i, [128, KC, 512], BF16) for i in range(2)]
            Arow = sb("t1_A", [128, D], F32)
            Brow = sb("t1_B", [128, D], F32)
            tmp = sb("t1_tmp", [128, D], F32)
            st = [sb("t1_st%d" % i, [128, 2], F32) for i in range(2)]
            ident = sb("t1_id", [128, 128], BF16)
            ptr = [ps.enter_context(nc.psum_tensor(U("t1_p%d" % i), [128, 1024], BF16)) for i in range(4)]
            r_x = R("x0", "x1"); r_h = R("h0", "h1"); r_hT = R("hT0", "hT1"); r_st = R("st0", "st1")
            r_A, r_B, r_tmp, r_id = R("A", "B", "tmp", "id")
            r_p = R("p0", "p1", "p2", "p3")
            S.dma("pool", lambda e: e.dma_start(out=ident[:, :], in_=ident_in[:, :]), [], [r_id])
            it = 0
            si = 0
            for grp, (tok0, ntile, row) in enumerate(((0, NTL, 0), (N, NTC, 1))):
                load_rows_bcast("sp", Arow[:, :], mod_d[layer, row:row + 1, D:2 * D], r_A)
                load_rows_bcast("sp", tmp[:, :], n1g[layer:layer + 1, :], r_tmp)
                load_rows_bcast("sp", Brow[:, :], mod_d[layer, row:row + 1, 0:D], r_B)
                S.op("dve", lambda e: e.scalar_tensor_tensor(out=Arow[:, :], in0=Arow[:, :], scalar=1.0, in1=tmp[:, :],
                                                             op0=ALU.add, op1=ALU.mult), [r_A, r_tmp], [r_A])
                for tt in range(ntile):
                    k = it % 2
                    sti = si % 2
                    sub = tt % 4
                    t0 = tok0 + tt * 128
                    S.dma("sp", lambda e, k=k, t0=t0: e.dma_start(out=xt[k][:, :], in_=src_rows(layer, t0, 128)),
                          [], [r_x[k]])
                    S.op("dve", lambda e, k=k: e.memset(st[k][:, :], 0.0), [], [r_st[k]])
                    S.op("act", lambda e, k=k: e.activation(out=hb[k][:, :], in_=xt[k][:, :], func=AF.Square,
                                                            accum_out=st[k][:, 0:1]), [r_x[k], r_st[k]], [r_h[k], r_st[k]])
                    S.op("dve", lambda e, k=k: e.tensor_scalar(out=st[k][:, 1:2], in0=st[k][:, 0:1], scalar1=1.0 / D,
                                                               scalar2=EPS, op0=ALU.mult, op1=ALU.add), [r_st[k]], [r_st[k]])
                    S.op("act", lambda e, k=k: e.activation(out=st[k][:, 1:2], in_=st[k][:, 1:2], func=AF.Sqrt),
                         [r_st[k]], [r_st[k]])
                    S.op("dve", lambda e, k=k: e.reciprocal(out=st[k][:, 1:2], in_=st[k][:, 1:2]), [r_st[k]], [r_st[k]])
                    S.op("dve", lambda e, k=k: e.scalar_tensor_tensor(out=xt[k][:, :], in0=xt[k][:, :],
                                                                      scalar=st[k][:, 1:2], in1=Arow[:, :],
                                                                      op0=ALU.mult, op1=ALU.mult),
                         [r_x[k], r_st[k], r_A], [r_x[k]])
                    S.op("pool", lambda e, k=k: e.tensor_tensor(out=hb[k][:, :], in0=xt[k][:, :], in1=Brow[:, :],
                                                                op=ALU.add), [r_x[k], r_B], [r_h[k]])
                    for g in range(4):
                        for j in range(8):
                            kc = g * 8 + j
                            fn = lambda e, k=k, g=g, j=j, kc=kc: e.transpose(ptr[g][:, j * 128:(j + 1) * 128],
                                                                             hb[k][:, kc * 128:(kc + 1) * 128], ident[:, :])
                            if j == 0 or j == 7:
                                S.op("pe", fn, [r_h[k], r_id], [r_p[g]])
                            else:
                                S.pe_quiet(fn)
                        eng = "act" if g % 2 == 0 else "dve"
                        if eng == "act":
                            S.op("act", lambda e, g=g, sti=sti, sub=sub: e.activation(
                                out=hT4[sti][:, g * 8:(g + 1) * 8, sub * 128:(sub + 1) * 128],
                                in_=ptr[g][:, :].rearrange("p (j t) -> p j t", j=8), func=AF.Copy), [r_p[g]], [r_hT[sti]])
                        else:
                            S.op("dve", lambda e, g=g, sti=sti, sub=sub: e.tensor_copy(
                                out=hT4[sti][:, g * 8:(g + 1) * 8, sub * 128:(sub + 1) * 128],
                                in_=ptr[g][:, :].rearrange("p (j t) -> p j t", j=8)), [r_p[g]], [r_hT[sti]])
                    it += 1
                    if sub == 3 or tt == ntile - 1:
                        nn = (sub + 1) * 128
                        s0 = t0 - sub * 128
                        S.dma("sp", lambda e, sti=sti, s0=s0, nn=nn: e.dma_start(out=hT_d[:, :, s0:s0 + nn],
                                                                                 in_=hT4[sti][:, :, 0:nn]), [r_hT[sti]], [])
                        si += 1
        return phase_end("T1_%d" % layer)


    SCALE = 128.0 ** -0.5

    def mm_group(out_ap, pairs, reads, wres):
        n = len(pairs)
        for i, (l, r) in enumerate(pairs):
            fn = lambda e, l=l, r=r, i=i: e.matmul(out_ap, l, r, start=(i == 0), stop=(i == n - 1))
            if i == 0 or i == n - 1:
                S.op("pe", fn, reads, [wres])
            else:
                S.pe_quiet(fn)

    def phase_H1(layer):
        W = wqkv[layer]
        with ExitStack() as ps:
            sb = lambda n, s, d: ps.enter_context(nc.sbuf_tensor(U(n), s, d))
            wb = sb("h1_w", [128, KC, 1024], BF16)
            hT4 = [sb("h1_hT%d" % i, [128, KC, 512], BF16) for i in range(2)]
            stage = [sb("h1_st%d" % i, [128, 1024], BF16) for i in range(2)]
            pacc = [ps.enter_context(nc.psum_tensor(U("h1_p%d" % i), [128, 512], F32)) for i in range(4)]
            r_w, = R("w"); r_hT = R("hT0", "hT1"); r_st = R("st0", "st1"); r_p = R("p0", "p1", "p2", "p3")
            if layer == 1:
                cs = [sb("h1_cos%d" % i, [128, 64], F32) for i in range(2)]
                sn = [sb("h1_sin%d" % i, [128, 64], F32) for i in range(2)]
                tm = [sb("h1_tm%d" % i, [128, 4, 64], F32) for i in range(4)]
                r_cs = R("cs0", "cs1"); r_tm = R("tm0", "tm1", "tm2", "tm3")
            hi = 0; si = 0; pi = 0; ci = 0
            for cb in range(12):
                S.dma("pool", lambda e, cb=cb: e.dma_start(
                    out=wb[:, :, :], in_=W[:, cb * 1024:(cb + 1) * 1024].rearrange("(kc p) n -> p kc n", p=128)),
                    [], [r_w])
                def load_hT(st_, k_):
                    nt_ = 4 if st_ < 16 else 2
                    S.dma("sp", lambda e, k_=k_, st_=st_, nt_=nt_: e.dma_start(
                        out=hT4[k_][:, :, 0:nt_ * 128], in_=hT_d[:, :, st_ * 512:st_ * 512 + nt_ * 128]), [], [r_hT[k_]])
                if cb == 0:
                    load_hT(0, hi % 2)
                for st in range(17):
                    ntile = 4 if st < 16 else 2
                    k = hi % 2; hi += 1
                    if st + 1 < 17:
                        load_hT(st + 1, hi % 2)
                    elif cb + 1 < 12:
                        load_hT(0, hi % 2)
                    for ts in range(ntile):
                        t0 = st * 512 + ts * 128
                        sk = si % 2; si += 1
                        rope = (layer == 1 and cb < 8 and st < 16)
                        if rope:
                            ck = ci % 2; ci += 1
                            S.dma("sp", lambda e, ck=ck, t0=t0: e.dma_start(out=cs[ck][:, :], in_=rope_cos[t0:t0 + 128, :]),
                                  [], [r_cs[ck]])
                            S.dma("sp", lambda e, ck=ck, t0=t0: e.dma_start(out=sn[ck][:, :], in_=rope_sin[t0:t0 + 128, :]),
                                  [], [r_cs[ck]])
                        for half in range(2):
                            p = pi % 4; pi += 1
                            mm_group(pacc[p][:, :],
                                     [(hT4[k][:, kc, ts * 128:(ts + 1) * 128], wb[:, kc, half * 512:(half + 1) * 512])
                                      for kc in range(KC)], [r_hT[k], r_w], r_p[p])
                            dst = stage[sk][:, half * 512:(half + 1) * 512]
                            if not rope:
                                if half == 0:
                                    S.op("act", lambda e, dst=dst, p=p: e.activation(out=dst, in_=pacc[p][:, :], func=AF.Copy),
                                         [r_p[p]], [r_st[sk]])
                                else:
                                    S.op("dve", lambda e, dst=dst, p=p: e.tensor_copy(out=dst, in_=pacc[p][:, :]),
                                         [r_p[p]], [r_st[sk]])
                            else:
                                pv = pacc[p][:, :].rearrange("p (g i two) -> p g i two", g=4, i=64, two=2)
                                dv = dst.rearrange("p (g i two) -> p g i two", g=4, i=64, two=2)
                                cb_ = cs[ck][:, :].unsqueeze(1).to_broadcast([128, 4, 64])
                                sb_ = sn[ck][:, :].unsqueeze(1).to_broadcast([128, 4, 64])
                                xe, xo = pv[:, :, :, 0], pv[:, :, :, 1]
                                S.op("dve", lambda e, xe=xe, cb_=cb_: e.tensor_tensor(out=tm[0][:, :, :], in0=xe, in1=cb_, op=ALU.mult),
                                     [r_p[p], r_cs[ck]], [r_tm[0]])
                                S.op("dve", lambda e, xo=xo, sb_=sb_: e.tensor_tensor(out=tm[1][:, :, :], in0=xo, in1=sb_, op=ALU.mult),
                                     [r_p[p], r_cs[ck]], [r_tm[1]])
                                S.op("dve", lambda e, xe=xe, sb_=sb_: e.tensor_tensor(out=tm[2][:, :, :], in0=xe, in1=sb_, op=ALU.mult),
                                     [r_p[p], r_cs[ck]], [r_tm[2]])
                                S.op("dve", lambda e, xo=xo, cb_=cb_: e.tensor_tensor(out=tm[3][:, :, :], in0=xo, in1=cb_, op=ALU.mult),
                                     [r_p[p], r_cs[ck]], [r_tm[3]])
                                S.op("pool", lambda e, dv=dv: e.tensor_tensor(out=dv[:, :, :, 0], in0=tm[0][:, :, :], in1=tm[1][:, :, :],
                                                                              op=ALU.subtract), [r_tm[0], r_tm[1]], [r_st[sk]])
                                S.op("pool", lambda e, dv=dv: e.tensor_tensor(out=dv[:, :, :, 1], in0=tm[2][:, :, :], in1=tm[3][:, :, :],
                                                                              op=ALU.add), [r_tm[2], r_tm[3]], [r_st[sk]])
                        S.dma("sp", lambda e, sk=sk, t0=t0, cb=cb: e.dma_start(
                            out=qkv_d[t0:t0 + 128, cb * 1024:(cb + 1) * 1024], in_=stage[sk][:, :]), [r_st[sk]], [])
        return phase_end("H1_%d" % layer)

    def phase_N():
        with ExitStack() as ps:
            sb = lambda n, s, d: ps.enter_context(nc.sbuf_tensor(U(n), s, d))
            qtm = sb("n_qtm", [128, 66, 128], BF16)
            ktm = sb("n_ktm", [128, 66, 128], BF16)
            QT = sb("n_QT", [128, NT], BF16)
            KT = sb("n_KT", [128, NT], BF16)
            Va = [sb("n_va%d" % i, [128, 64, 128], BF16) for i in range(2)]
            Vb = [sb("n_vb%d" % i, [128, 63, 128], BF16) for i in range(2)]
            Vc = [sb("n_vc%d" % i, [128, 2, 128], BF16) for i in range(2)]
            bias = [sb("n_bias%d" % i, [64, 960], F32) for i in range(2)]
            oTh = sb("n_oT", [128, NT], BF16)
            sbs = [sb("n_s%d" % i, [128, 768], F32) for i in range(2)]
            pbf = [sb("n_p%d" % i, [128, 768], BF16) for i in range(2)]
            pT = [sb("n_pT%d" % i, [128, 384], BF16) for i in range(2)]
            stt = [sb("n_stt%d" % i, [128, 4], F32) for i in range(2)]
            stt2 = [sb("n_stt2%d" % i, [128, 4], F32) for i in range(2)]
            obf = [sb("n_o%d" % i, [128, 128], BF16) for i in range(2)]
            ident = sb("n_id", [128, 128], BF16)
            msk = sb("n_msk", [64, 64], F32)
            ptq0 = ps.enter_context(nc.psum_tensor(U("n_ptq0"), [128, 1024], BF16))
            ptq1 = ps.enter_context(nc.psum_tensor(U("n_ptq1"), [128, 1024], BF16))
            ps_a = [ps.enter_context(nc.psum_tensor(U("n_pa%d" % i), [128, 512], F32)) for i in range(2)]
            ps_b = [ps.enter_context(nc.psum_tensor(U("n_pb%d" % i), [128, 512], F32)) for i in range(2)]
            ps_o = [ps.enter_context(nc.psum_tensor(U("n_po%d" % i), [128, 512], F32)) for i in range(2)]
            r_qtm, r_ktm, r_QT, r_KT, r_oTh, r_id, r_msk, r_q0, r_q1a, r_q1b = R(
                "qtm", "ktm", "QT", "KT", "oTh", "id", "msk", "q0", "q1a", "q1b")
            r_V = R("V0", "V1"); r_bias = R("b0", "b1")
            r_s = R("s0", "s1"); r_p = R("p0", "p1"); r_pT = R("pT0", "pT1"); r_stt = R("t0", "t1"); r_o = R("o0", "o1")
            r_pa = R("pa0", "pa1"); r_pb = R("pb0", "pb1"); r_po = R("po0", "po1"); r_stt2 = R("u0", "u1")
            S.dma("pool", lambda e: e.dma_start(out=ident[:, :], in_=ident_in[:, :]), [], [r_id])
            S.dma("sp", lambda e: e.dma_start(out=msk[:, :], in_=nmask[:, :]), [], [r_msk])
            wi = 0
            for hh in range(32):
                hb = hh % 2
                qs = lambda c0: qkv_d[:, c0:c0 + 128]
                S.dma("sp", lambda e, hh=hh: e.dma_start(
                    out=qtm[:, :, :], in_=qkv_d[:, hh * 128:(hh + 1) * 128].rearrange("(c p) d -> p c d", p=128)), [], [r_qtm])
                S.dma("sp", lambda e, hh=hh: e.dma_start(
                    out=ktm[:, :, :], in_=qkv_d[:, D + hh * 128:D + (hh + 1) * 128].rearrange("(c p) d -> p c d", p=128)),
                    [], [r_ktm])
                vcol = 2 * D + hh * 128
                S.dma("sp", lambda e, hb=hb, vcol=vcol: e.dma_start(
                    out=Va[hb][:, :, :], in_=qkv_d[0:N, vcol:vcol + 128].rearrange("(c p) d -> p c d", p=128)), [], [r_V[hb]])
                S.dma("sp", lambda e, hb=hb, vcol=vcol: e.dma_start(
                    out=Vb[hb][:, :, :], in_=qkv_d[64:64 + 63 * 128, vcol:vcol + 128].rearrange("(c p) d -> p c d", p=128)),
                    [], [r_V[hb]])
                S.dma("sp", lambda e, hb=hb, vcol=vcol: e.dma_start(
                    out=Vc[hb][:, :, :], in_=qkv_d[N:NT, vcol:vcol + 128].rearrange("(c p) d -> p c d", p=128)), [], [r_V[hb]])
                S.dma("sp", lambda e, hb=hb, hh=hh: e.dma_start(out=bias[hb][:, :], in_=nbias[hh, :, :]), [], [r_bias[hb]])
                S.op("dve", lambda e, hb=hb: e.tensor_tensor(
                    out=bias[hb][:, :].rearrange("p (j k) -> p j k", j=15),
                    in0=bias[hb][:, :].rearrange("p (j k) -> p j k", j=15),
                    in1=msk[:, :].unsqueeze(1).to_broadcast([64, 15, 64]), op=ALU.add), [r_bias[hb], r_msk], [r_bias[hb]])
                for (src, r_src, dstT, r_dst) in ((qtm, r_qtm, QT, r_QT), (ktm, r_ktm, KT, r_KT)):
                    for c0 in range(0, 66, 8):
                        nb = min(8, 66 - c0)
                        for j in range(nb):
                            fn = lambda e, src=src, c=c0 + j, j=j: e.transpose(ptq0[:, j * 128:(j + 1) * 128], src[:, c, :], ident[:, :])
                            if j == 0 or j == nb - 1:
                                S.op("pe", fn, [r_src, r_id], [r_q0])
                            else:
                                S.pe_quiet(fn)
                        S.op("dve", lambda e, dstT=dstT, c0=c0, nb=nb: e.tensor_copy(
                            out=dstT[:, c0 * 128:(c0 + nb) * 128], in_=ptq0[:, 0:nb * 128]), [r_q0], [r_dst])
                def row_info(r):
                    is_ctx = r >= NROWS
                    if not is_ctx:
                        rs = min(max(r - 4, 0), NROWS - 8)
                        return dict(is_ctx=False, P=64, rs=rs, j0=rs - r + 7, q0=r * 64, nk=768)
                    return dict(is_ctx=True, P=128, rs=0, j0=0, q0=N + (r - NROWS) * 128, nk=256)

                def emit_S(r, k):
                    ri = row_info(r)
                    q0 = ri["q0"]
                    if not ri["is_ctx"]:
                        rs, j0 = ri["rs"], ri["j0"]
                        S.op("pe", lambda e, k=k, q0=q0, rs=rs: e.matmul(ps_a[k][0:64, 0:512], QT[:, q0:q0 + 64],
                                                                       KT[:, rs * 64:rs * 64 + 512], start=True, stop=True),
                             [r_QT, r_KT], [r_pa[k]])
                        S.op("pe", lambda e, k=k, q0=q0: e.matmul(ps_b[k][0:64, 0:256], QT[:, q0:q0 + 64], KT[:, N:NT],
                                                                start=True, stop=True), [r_QT, r_KT], [r_pb[k]])
                    else:
                        S.op("pe", lambda e, k=k, q0=q0: e.matmul(ps_a[k][:, 0:256], QT[:, q0:q0 + 128], KT[:, N:NT],
                                                                start=True, stop=True), [r_QT, r_KT], [r_pa[k]])

                def emit_softmax(r, k):
                    ri = row_info(r)
                    P_, nk = ri["P"], ri["nk"]
                    if not ri["is_ctx"]:
                        j0 = ri["j0"]
                        S.op("dve", lambda e, k=k, hb=hb, j0=j0: e.scalar_tensor_tensor(
                            out=sbs[k][0:64, 0:512], in0=ps_a[k][0:64, 0:512], scalar=SCALE,
                            in1=bias[hb][:, j0 * 64:j0 * 64 + 512], op0=ALU.mult, op1=ALU.add),
                            [r_pa[k], r_bias[hb]], [r_s[k]])
                        S.op("act", lambda e, k=k: e.activation(out=sbs[k][0:64, 512:768], in_=ps_b[k][0:64, 0:256],
                                                                func=AF.Copy, scale=SCALE), [r_pb[k]], [r_s[k]])
                    else:
                        S.op("act", lambda e, k=k: e.activation(out=sbs[k][:, 0:256], in_=ps_a[k][:, 0:256],
                                                                func=AF.Copy, scale=SCALE), [r_pa[k]], [r_s[k]])
                    S.op("dve", lambda e, k=k, P_=P_, nk=nk: e.tensor_reduce(out=stt[k][0:P_, 0:1], in_=sbs[k][0:P_, 0:nk],
                                                                           axis=AX.X, op=ALU.max), [r_s[k]], [r_stt[k]])
                    S.op("dve", lambda e, k=k, P_=P_: e.tensor_scalar(out=stt[k][0:P_, 1:2], in0=stt[k][0:P_, 0:1],
                                                                      scalar1=-1.0, scalar2=None, op0=ALU.mult),
                         [r_stt[k]], [r_stt[k]])
                    S.op("pool", lambda e, k=k, P_=P_: e.memset(stt2[k][0:P_, 0:1], 0.0), [], [r_stt2[k]])
                    S.op("act", lambda e, k=k, P_=P_, nk=nk: e.activation(
                        out=pbf[k][0:P_, 0:nk], in_=sbs[k][0:P_, 0:nk], func=AF.Exp, bias=stt[k][0:P_, 1:2], scale=1.0,
                        accum_out=stt2[k][0:P_, 0:1]), [r_s[k], r_stt[k], r_stt2[k]], [r_p[k], r_stt2[k]])
                    S.op("dve", lambda e, k=k, P_=P_: e.reciprocal(out=stt2[k][0:P_, 1:2], in_=stt2[k][0:P_, 0:1]),
                         [r_stt2[k]], [r_stt2[k]])

                def emit_PV(r, k):
                    ri = row_info(r)
                    P_, nk, rs, q0, is_ctx = ri["P"], ri["nk"], ri["rs"], ri["q0"], ri["is_ctx"]
                    nch = nk // 128
                    for c in range(nch):
                        fn = lambda e, k=k, c=c, P_=P_: e.transpose(ptq1[:, c * P_:(c + 1) * P_],
                                                                    pbf[k][0:P_, c * 128:(c + 1) * 128], ident[0:P_, 0:P_])
                        if c == 0 or c == nch - 1:
                            S.op("pe", fn, [r_p[k], r_id], [r_q1a])
                        else:
                            S.pe_quiet(fn)
                    S.op("act", lambda e, k=k, w_=nch * P_: e.activation(out=pT[k][:, 0:w_], in_=ptq1[:, 0:w_], func=AF.Copy),
                         [r_q1a], [r_pT[k]])
                    pairs = []
                    for c in range(nch):
                        if is_ctx:
                            vch = Vc[hb][:, c, :]
                        elif c < 4:
                            vch = Va[hb][:, rs // 2 + c, :] if rs % 2 == 0 else Vb[hb][:, (rs - 1) // 2 + c, :]
                        else:
                            vch = Vc[hb][:, c - 4, :]
                        pairs.append((pT[k][:, c * P_:(c + 1) * P_], vch))
                    mm_group(ps_o[k][0:P_, 0:128], pairs, [r_pT[k], r_V[hb]], r_po[k])
                    S.op("act", lambda e, k=k, P_=P_: e.activation(out=obf[k][0:P_, :], in_=ps_o[k][0:P_, 0:128], func=AF.Copy,
                                                                   scale=stt2[k][0:P_, 1:2]), [r_po[k], r_stt2[k]], [r_o[k]])
                    S.op("pe", lambda e, k=k, P_=P_: e.transpose(ptq1[:, 512:512 + P_], obf[k][0:P_, :], ident[0:P_, 0:P_]),
                         [r_o[k], r_id], [r_q1b])
                    S.op("dve", lambda e, q0=q0, P_=P_: e.tensor_copy(out=oTh[:, q0:q0 + P_], in_=ptq1[:, 512:512 + P_]),
                         [r_q1b], [r_oTh])

                NR = NROWS + 2
                emit_S(0, wi % 2)
                for r in range(NR):
                    k = wi % 2
                    emit_softmax(r, k)
                    if r + 1 < NR:
                        emit_S(r + 1, (wi + 1) % 2)
                    emit_PV(r, k)
                    wi += 1
                S.dma("sp", lambda e, hh=hh: e.dma_start(out=oT_d[:, hh, :], in_=oTh[:, :]), [r_oTh], [])
        return phase_end("N")

    def phase_T2a(layer):
        W = wo[layer]
        with ExitStack() as ps:
            sb = lambda n, s, d: ps.enter_context(nc.sbuf_tensor(U(n), s, d))
            wob = [sb("t2_w%d" % i, [128, KC, 512], BF16) for i in range(2)]
            oT4 = [sb("t2_oT%d" % i, [128, KC, 512], BF16) for i in range(2)]
            xb = [sb("t2_x%d" % i, [128, 512], F32) for i in range(2)]
            yb = [sb("t2_y%d" % i, [128, 512], F32) for i in range(2)]
            g2 = [sb("t2_g%d" % i, [128, D], F32) for i in range(2)]
            pacc = [ps.enter_context(nc.psum_tensor(U("t2_p%d" % i), [128, 512], F32)) for i in range(4)]
            r_w = R("w0", "w1"); r_oT = R("o0", "o1"); r_x = R("x0", "x1"); r_y = R("y0", "y1"); r_g = R("g0", "g1")
            r_p = R("p0", "p1", "p2", "p3")
            for row in range(2):
                load_rows_bcast("sp", g2[row][:, :], mod_d[layer, row:row + 1, 2 * D:3 * D], r_g[row])
            oi = 0; xi = 0; pi = 0
            for nb in range(8):
                wk = nb % 2
                S.dma("pool", lambda e, wk=wk, nb=nb: e.dma_start(
                    out=wob[wk][:, :, :], in_=W[:, nb * 512:(nb + 1) * 512].rearrange("(kc p) n -> p kc n", p=128)),
                    [], [r_w[wk]])
                NST = 17 if layer == 0 else 16

                def load_oT(st_, k_):
                    nt_ = 4 if st_ < 16 else 2
                    S.dma("sp", lambda e, k_=k_, st_=st_, nt_=nt_: e.dma_start(
                        out=oT4[k_][:, :, 0:nt_ * 128], in_=oT_d[:, :, st_ * 512:st_ * 512 + nt_ * 128]), [], [r_oT[k_]])
                if nb == 0:
                    load_oT(0, oi % 2)
                for st in range(NST):
                    ntile = 4 if st < 16 else 2
                    row = 0 if st < 16 else 1
                    k = oi % 2; oi += 1
                    if st + 1 < NST:
                        load_oT(st + 1, oi % 2)
                    elif nb + 1 < 8:
                        load_oT(0, oi % 2)
                    for ts in range(ntile):
                        t0 = st * 512 + ts * 128
                        j = xi % 2; xi += 1
                        p = pi % 4; pi += 1
                        S.dma("sp", lambda e, j=j, t0=t0, nb=nb: e.dma_start(
                            out=xb[j][:, :], in_=src_rows(layer, t0, 128)[:, nb * 512:(nb + 1) * 512]), [], [r_x[j]])
                        mm_group(pacc[p][:, :], [(oT4[k][:, kc, ts * 128:(ts + 1) * 128], wob[wk][:, kc, :]) for kc in range(KC)],
                                 [r_oT[k], r_w[wk]], r_p[p])
                        S.op("dve", lambda e, j=j, p=p, row=row, nb=nb: e.tensor_tensor(
                            out=yb[j][:, :], in0=pacc[p][:, :], in1=g2[row][:, nb * 512:(nb + 1) * 512], op=ALU.mult),
                            [r_p[p], r_g[row]], [r_y[j]])
                        S.op("pool", lambda e, j=j: e.tensor_tensor(out=yb[j][:, :], in0=yb[j][:, :], in1=xb[j][:, :], op=ALU.add),
                             [r_y[j], r_x[j]], [r_y[j]])
                        S.dma("sp", lambda e, j=j, t0=t0, nb=nb: e.dma_start(
                            out=x1_d[t0:t0 + 128, nb * 512:(nb + 1) * 512], in_=yb[j][:, :]), [r_y[j]], [])
        return phase_end("T2a_%d" % layer)

    def phase_T2b(layer):
        with ExitStack() as ps:
            sb = lambda n, s, d: ps.enter_context(nc.sbuf_tensor(U(n), s, d))
            xt = [sb("tb_x%d" % i, [128, D], F32) for i in range(2)]
            hb = [sb("tb_h%d" % i, [128, D], BF16) for i in range(2)]
            Arow = sb("tb_A", [128, D], F32)
            Brow = sb("tb_B", [128, D], F32)
            tmp = sb("tb_tmp", [128, D], F32)
            st = [sb("tb_st%d" % i, [128, 8], F32) for i in range(2)]
            hfT = sb("tb_hfT", [128, KC, 128], F32)
            identf = sb("tb_id", [128, 128], F32)
            wrb = sb("tb_wr", [128, KC, NE], F32)
            lg = [sb("tb_lg%d" % i, [128, NE], F32) for i in range(2)]
            ptr = [ps.enter_context(nc.psum_tensor(U("tb_p%d" % i), [128, 512], F32)) for i in range(4)]
            pl = ps.enter_context(nc.psum_tensor(U("tb_pl"), [128, 512], F32))
            r_x = R("x0", "x1"); r_h = R("h0", "h1"); r_st = R("st0", "st1"); r_lg = R("lg0", "lg1")
            r_A, r_B, r_tmp, r_id, r_wr, r_hfT, r_pl = R("A", "B", "tmp", "id", "wr", "hfT", "pl")
            r_p = R("p0", "p1", "p2", "p3")
            S.dma("sp", lambda e: e.dma_start(out=identf[:, :], in_=ident_in[:, :]), [], [r_id])
            S.dma("sp", lambda e: e.dma_start(out=wrb[:, :, :], in_=wr[layer, :, :].rearrange("(kc p) n -> p kc n", p=128)),
                  [], [r_wr])
            it = 0
            for (tok0, ntile, row) in (((0, NTL, 0), (N, NTC, 1)) if layer == 0 else ((0, NTL, 0),)):
                load_rows_bcast("sp", Arow[:, :], mod_d[layer, row:row + 1, 4 * D:5 * D], r_A)
                load_rows_bcast("sp", tmp[:, :], n2g[layer:layer + 1, :], r_tmp)
                load_rows_bcast("sp", Brow[:, :], mod_d[layer, row:row + 1, 3 * D:4 * D], r_B)
                S.op("dve", lambda e: e.scalar_tensor_tensor(out=Arow[:, :], in0=Arow[:, :], scalar=1.0, in1=tmp[:, :],
                                                             op0=ALU.add, op1=ALU.mult), [r_A, r_tmp], [r_A])
                for tt in range(ntile):
                    k = it % 2; it += 1
                    t0 = tok0 + tt * 128
                    S.dma("sp", lambda e, k=k, t0=t0: e.dma_start(out=xt[k][:, :], in_=x1_d[t0:t0 + 128, :]), [], [r_x[k]])
                    S.op("dve", lambda e, k=k: e.memset(st[k][:, 0:1], 0.0), [], [r_st[k]])
                    S.op("act", lambda e, k=k: e.activation(out=hb[k][:, :], in_=xt[k][:, :], func=AF.Square,
                                                            accum_out=st[k][:, 0:1]), [r_x[k], r_st[k]], [r_h[k], r_st[k]])
                    S.op("dve", lambda e, k=k: e.tensor_scalar(out=st[k][:, 1:2], in0=st[k][:, 0:1], scalar1=1.0 / D,
                                                               scalar2=EPS, op0=ALU.mult, op1=ALU.add), [r_st[k]], [r_st[k]])
                    S.op("act", lambda e, k=k: e.activation(out=st[k][:, 1:2], in_=st[k][:, 1:2], func=AF.Sqrt),
                         [r_st[k]], [r_st[k]])
                    S.op("dve", lambda e, k=k: e.reciprocal(out=st[k][:, 1:2], in_=st[k][:, 1:2]), [r_st[k]], [r_st[k]])
                    S.op("dve", lambda e, k=k: e.scalar_tensor_tensor(out=xt[k][:, :], in0=xt[k][:, :], scalar=st[k][:, 1:2],
                                                                      in1=Arow[:, :], op0=ALU.mult, op1=ALU.mult),
                         [r_x[k], r_st[k], r_A], [r_x[k]])
                    S.op("pool", lambda e, k=k: e.tensor_tensor(out=xt[k][:, :], in0=xt[k][:, :], in1=Brow[:, :], op=ALU.add),
                         [r_x[k], r_B], [r_x[k]])
                    S.op("act", lambda e, k=k: e.activation(out=hb[k][:, :], in_=xt[k][:, :], func=AF.Copy), [r_x[k]], [r_h[k]])
                    S.dma("sp", lambda e, k=k, t0=t0: e.dma_start(out=h2_d[t0:t0 + 128, :], in_=hb[k][:, :]), [r_h[k]], [])
                    for g in range(8):
                        pg = g % 4
                        for j in range(4):
                            kc = g * 4 + j
                            fn = lambda e, k=k, pg=pg, j=j, kc=kc: e.transpose(ptr[pg][:, j * 128:(j + 1) * 128],
                                                                               xt[k][:, kc * 128:(kc + 1) * 128], identf[:, :])
                            if j == 0 or j == 3:
                                S.op("pe", fn, [r_x[k], r_id], [r_p[pg]])
                            else:
                                S.pe_quiet(fn)
                        if g % 2 == 0:
                            S.op("act", lambda e, g=g, pg=pg: e.activation(
                                out=hfT[:, g * 4:(g + 1) * 4, :], in_=ptr[pg][:, :].rearrange("p (j t) -> p j t", j=4),
                                func=AF.Copy), [r_p[pg]], [r_hfT])
                        else:
                            S.op("dve", lambda e, g=g, pg=pg: e.tensor_copy(
                                out=hfT[:, g * 4:(g + 1) * 4, :], in_=ptr[pg][:, :].rearrange("p (j t) -> p j t", j=4)),
                                [r_p[pg]], [r_hfT])
                    mm_group(pl[:, 0:NE], [(hfT[:, kc, :], wrb[:, kc, :]) for kc in range(KC)], [r_hfT, r_wr], r_pl)
                    S.op("dve", lambda e, k=k: e.tensor_reduce(out=st[k][:, 2:3], in_=pl[:, 0:NE], axis=AX.X, op=ALU.max),
                         [r_pl], [r_st[k]])
                    S.op("dve", lambda e, k=k: e.tensor_scalar(out=st[k][:, 3:4], in0=st[k][:, 2:3], scalar1=-1.0, scalar2=None,
                                                               op0=ALU.mult), [r_st[k]], [r_st[k]])
                    S.op("dve", lambda e, k=k: e.memset(st[k][:, 4:5], 0.0), [], [r_st[k]])
                    S.op("act", lambda e, k=k: e.activation(out=lg[k][:, :], in_=pl[:, 0:NE], func=AF.Exp, bias=st[k][:, 3:4],
                                                            scale=1.0, accum_out=st[k][:, 4:5]), [r_pl, r_st[k]], [r_lg[k], r_st[k]])
                    S.op("dve", lambda e, k=k: e.reciprocal(out=st[k][:, 5:6], in_=st[k][:, 4:5]), [r_st[k]], [r_st[k]])
                    S.op("dve", lambda e, k=k: e.tensor_scalar(out=lg[k][:, :], in0=lg[k][:, :], scalar1=st[k][:, 5:6],
                                                               scalar2=None, op0=ALU.mult), [r_lg[k], r_st[k]], [r_lg[k]])
                    S.dma("sp", lambda e, k=k, t0=t0: e.dma_start(out=aff_d[t0:t0 + 128, :], in_=lg[k][:, :]), [r_lg[k]], [])
        return phase_end("T2b_%d" % layer)

    def phase_DA():
        import math
        LAM_INIT = 0.8 - 0.6 * math.exp(-0.3 * 1)
        with ExitStack() as ps:
            sb = lambda n, s, d: ps.enter_context(nc.sbuf_tensor(U(n), s, d))
            tm = sb("da_tm", [128, 66, 256], BF16)
            QT = sb("da_QT", [128, 2, N], BF16)
            KT = sb("da_KT", [128, 2, NT], BF16)
            Vaug = sb("da_V", [128, 66, 257], BF16)
            PT = [sb("da_PT%d" % i, [128, 512], BF16) for i in range(3)]
            ident = sb("da_id", [128, 128], BF16)
            lrow = sb("da_lrow", [128, 4, 128], F32)
            ltmp = sb("da_ltmp", [128, 128], F32)
            lamt = sb("da_lam", [128, 8], F32)
            gsub = sb("da_gsub", [128, 256], F32)
            o32 = [sb("da_o32%d" % i, [128, 256], F32) for i in range(2)]
            obf = [sb("da_obf%d" % i, [128, 256], BF16) for i in range(2)]
            osb = [sb("da_osb%d" % i, [128, 2, 128], BF16) for i in range(2)]
            stt = [sb("da_stt%d" % i, [128, 8], F32) for i in range(2)]
            junk = sb("da_junk", [128, 256], BF16)
            ptq = ps.enter_context(nc.psum_tensor(U("da_ptq"), [128, 1024], BF16))
            pso = ptq
            ps_s = [ps.enter_context(nc.psum_tensor(U("da_ps%d" % i), [128, 512], F32)) for i in range(3)]
            po = [ps.enter_context(nc.psum_tensor(U("da_po%d" % i), [128, 512], F32)) for i in range(4)]
            r_tm, r_QT, r_KT, r_V, r_id, r_lrow, r_ltmp, r_lam, r_gsub, r_junk, r_ptq, r_pso = R(
                "tm", "QT", "KT", "V", "id", "lrow", "ltmp", "lam", "gsub", "junk", "ptq", "pso")
            r_PT = R("PT0", "PT1", "PT2"); r_o32 = R("o0", "o1"); r_obf = R("ob0", "ob1"); r_osb = R("os0", "os1")
            r_stt = R("st0", "st1"); r_ps = R("ps0", "ps1", "ps2"); r_po = R("po0", "po1", "po2", "po3")
            r_pso = r_ptq
            S.dma("pool", lambda e: e.dma_start(out=ident[:, :], in_=ident_in[:, :]), [], [r_id])
            for i in range(4):
                S.dma("sp", lambda e, i=i: e.dma_start(out=lrow[:, i, :], in_=lamp[i:i + 1, :].partition_broadcast(128)),
                      [], [r_lrow])
            for j in range(2):
                S.op("dve", lambda e, j=j: e.tensor_tensor(out=ltmp[:, :], in0=lrow[:, 2 * j, :], in1=lrow[:, 2 * j + 1, :],
                                                           op=ALU.mult), [r_lrow], [r_ltmp])
                S.op("dve", lambda e, j=j: e.tensor_reduce(out=lamt[:, j:j + 1], in_=ltmp[:, :], axis=AX.X, op=ALU.add),
                     [r_ltmp], [r_lam])
            S.op("act", lambda e: e.activation(out=lamt[:, 2:4], in_=lamt[:, 0:2], func=AF.Exp), [r_lam], [r_lam])
            S.op("dve", lambda e: e.tensor_tensor(out=lamt[:, 4:5], in0=lamt[:, 3:4], in1=lamt[:, 2:3], op=ALU.subtract),
                 [r_lam], [r_lam])
            S.op("dve", lambda e: e.tensor_scalar(out=lamt[:, 5:6], in0=lamt[:, 4:5], scalar1=-LAM_INIT, scalar2=None, op0=ALU.add),
                 [r_lam], [r_lam])
            load_rows_bcast("sp", gsub[:, :], sublng[0:1, :], r_gsub)
            S.op("dve", lambda e: e.tensor_scalar(out=gsub[:, :], in0=gsub[:, :], scalar1=1.0 - LAM_INIT, scalar2=None, op0=ALU.mult),
                 [r_gsub], [r_gsub])
            S.op("pool", lambda e: e.memset(Vaug[:, :, 256:257], 1.0), [], [r_V])
            step = 0; ei = 0
            for h in range(16):
                for (c_off, nchunk, dstT, r_dst) in ((h * 256, NTL, QT, r_QT), (D + h * 256, NTL + NTC, KT, r_KT)):
                    S.dma("sp", lambda e, c_off=c_off, nchunk=nchunk: e.dma_start(
                        out=tm[:, 0:nchunk, :],
                        in_=qkv_d[0:nchunk * 128, c_off:c_off + 256].rearrange("(c p) d -> p c d", p=128)), [], [r_tm])
                    for comp in range(2):
                        for c0 in range(0, nchunk, 8):
                            nb = min(8, nchunk - c0)
                            for j in range(nb):
                                fn = lambda e, c=c0 + j, j=j, comp=comp: e.transpose(
                                    ptq[:, j * 128:(j + 1) * 128], tm[:, c, comp * 128:(comp + 1) * 128], ident[:, :])
                                if j == 0 or j == nb - 1:
                                    S.op("pe", fn, [r_tm, r_id], [r_ptq])
                                else:
                                    S.pe_quiet(fn)
                            if (c0 // 8) % 2 == 0:
                                S.op("dve", lambda e, dstT=dstT, comp=comp, c0=c0, nb=nb: e.tensor_copy(
                                    out=dstT[:, comp, c0 * 128:(c0 + nb) * 128], in_=ptq[:, 0:nb * 128]), [r_ptq], [r_dst])
                            else:
                                S.op("act", lambda e, dstT=dstT, comp=comp, c0=c0, nb=nb: e.activation(
                                    out=dstT[:, comp, c0 * 128:(c0 + nb) * 128], in_=ptq[:, 0:nb * 128], func=AF.Copy),
                                    [r_ptq], [r_dst])
                vcol = 2 * D + h * 256
                S.dma("sp", lambda e, vcol=vcol: e.dma_start(
                    out=Vaug[:, :, 0:256], in_=qkv_d[:, vcol:vcol + 256].rearrange("(c p) d -> p c d", p=128)), [], [r_V])
                NKC = NTL + NTC

                def emit_scores(qb_, kc_, gi_):
                    sk_ = gi_ % 3
                    for comp in range(2):
                        S.op("pe", lambda e, sk_=sk_, comp=comp, kc_=kc_, q0_=qb_ * 256: e.matmul(
                            ps_s[sk_][:, comp * 256:(comp + 1) * 256], KT[:, comp, kc_ * 128:(kc_ + 1) * 128],
                            QT[:, comp, q0_:q0_ + 256], start=True, stop=True), [r_KT, r_QT], [r_ps[sk_]])

                def emit_exp_pv(kc_, gi_):
                    sk_ = gi_ % 3
                    S.op("act", lambda e, sk_=sk_: e.activation(out=PT[sk_][:, :], in_=ps_s[sk_][:, :], func=AF.Exp, scale=SCALE),
                         [r_ps[sk_]], [r_PT[sk_]])
                    for comp in range(2):
                        for qs in range(2):
                            pi = comp * 2 + qs
                            S.op("pe", lambda e, sk_=sk_, comp=comp, qs=qs, pi=pi, kc_=kc_: e.matmul(
                                po[pi][:, 0:257], PT[sk_][:, comp * 256 + qs * 128:comp * 256 + (qs + 1) * 128],
                                Vaug[:, kc_, :], start=(kc_ == 0), stop=(kc_ == NKC - 1)), [r_PT[sk_], r_V], [r_po[pi]])

                NQB = N // 256
                flat = [(qb_, kc_) for qb_ in range(NQB) for kc_ in range(NKC)]
                emit_scores(flat[0][0], flat[0][1], step)
                emit_scores(flat[1][0], flat[1][1], step + 1)
                for fi, (qb, kc) in enumerate(flat):
                    q0 = qb * 256
                    if fi + 2 < len(flat):
                        emit_scores(flat[fi + 2][0], flat[fi + 2][1], step + 2)
                    emit_exp_pv(kc, step)
                    step += 1
                    if kc != NKC - 1:
                        continue
                    for qs in range(2):
                        k2 = ei % 2; ei += 1
                        S.op("dve", lambda e, k2=k2, qs=qs: e.reciprocal(out=stt[k2][:, 0:1], in_=po[qs][:, 256:257]),
                             [r_po[qs]], [r_stt[k2]])
                        S.op("dve", lambda e, k2=k2, qs=qs: e.reciprocal(out=stt[k2][:, 1:2], in_=po[2 + qs][:, 256:257]),
                             [r_po[2 + qs]], [r_stt[k2]])
                        S.op("dve", lambda e, k2=k2: e.tensor_tensor(out=stt[k2][:, 1:2], in0=stt[k2][:, 1:2], in1=lamt[:, 5:6],
                                                                     op=ALU.mult), [r_stt[k2], r_lam], [r_stt[k2]])
                        S.op("dve", lambda e, k2=k2, qs=qs: e.tensor_scalar(out=o32[k2][:, :], in0=po[qs][:, 0:256],
                                                                            scalar1=stt[k2][:, 0:1], scalar2=None, op0=ALU.mult),
                             [r_po[qs], r_stt[k2]], [r_o32[k2]])
                        S.op("dve", lambda e, k2=k2, qs=qs: e.scalar_tensor_tensor(
                            out=o32[k2][:, :], in0=po[2 + qs][:, 0:256], scalar=stt[k2][:, 1:2], in1=o32[k2][:, :],
                            op0=ALU.mult, op1=ALU.add), [r_po[2 + qs], r_stt[k2], r_o32[k2]], [r_o32[k2]])
                        S.op("dve", lambda e, k2=k2: e.memset(stt[k2][:, 2:3], 0.0), [], [r_stt[k2]])
                        S.op("act", lambda e, k2=k2: e.activation(out=junk[:, :], in_=o32[k2][:, :], func=AF.Square,
                                                                  accum_out=stt[k2][:, 2:3]), [r_o32[k2], r_stt[k2]],
                             [r_junk, r_stt[k2]])
                        S.op("dve", lambda e, k2=k2: e.tensor_scalar(out=stt[k2][:, 3:4], in0=stt[k2][:, 2:3], scalar1=1.0 / 256,
                                                                     scalar2=SUBLN_EPS, op0=ALU.mult, op1=ALU.add),
                             [r_stt[k2]], [r_stt[k2]])
                        S.op("act", lambda e, k2=k2: e.activation(out=stt[k2][:, 3:4], in_=stt[k2][:, 3:4], func=AF.Sqrt),
                             [r_stt[k2]], [r_stt[k2]])
                        S.op("dve", lambda e, k2=k2: e.reciprocal(out=stt[k2][:, 3:4], in_=stt[k2][:, 3:4]), [r_stt[k2]], [r_stt[k2]])
                        S.op("dve", lambda e, k2=k2: e.scalar_tensor_tensor(out=obf[k2][:, :], in0=o32[k2][:, :],
                                                                            scalar=stt[k2][:, 3:4], in1=gsub[:, :],
                                                                            op0=ALU.mult, op1=ALU.mult),
                             [r_o32[k2], r_stt[k2], r_gsub], [r_obf[k2]])
                        for j in range(2):
                            S.op("pe", lambda e, k2=k2, j=j: e.transpose(pso[:, j * 128:(j + 1) * 128],
                                                                         obf[k2][:, j * 128:(j + 1) * 128], ident[:, :]),
                                 [r_obf[k2], r_id], [r_pso])
                        S.op("dve", lambda e, k2=k2: e.tensor_copy(out=osb[k2][:, :, :],
                                                                   in_=pso[:, 0:256].rearrange("p (j t) -> p j t", j=2)),
                             [r_pso], [r_osb[k2]])
                        t0 = q0 + qs * 128
                        S.dma("sp", lambda e, k2=k2, h=h, t0=t0: e.dma_start(out=oT_d[:, 2 * h:2 * h + 2, t0:t0 + 128],
                                                                             in_=osb[k2][:, :, :]), [r_osb[k2]], [])
        return phase_end("DA")


    offs_l = es.enter_context(nc.sbuf_tensor("offs_l", [128, NTL, NE], I32))
    offs_c = es.enter_context(nc.sbuf_tensor("offs_c", [128, NTC, NE], I32))
    gsel_l = es.enter_context(nc.sbuf_tensor("gsel_l", [128, NTL, NE], F32))
    gsel_c = es.enter_context(nc.sbuf_tensor("gsel_c", [128, NTC, NE], F32))
    r_offs, r_gsel = R("offs", "gsel")
    NROW_XG = NE * SLOTS

    def phase_E1(layer):
        sets = [(0, NTL, CAP, 0, offs_l, gsel_l)]
        if layer == 0:
            sets.append((N, NTC, CAPC, CAP, offs_c, gsel_c))
        with ExitStack() as ps:
            sb = lambda n, s, d: ps.enter_context(nc.sbuf_tensor(U(n), s, d))
            A = sb("e1_A", [128, NTL, NE], F32)
            cmp = sb("e1_cmp", [128, NTL, NE], F32)
            M = sb("e1_M", [128, NTL, NE], F32)
            s0 = sb("e1_s0", [128, NTL, NE], F32)
            s1 = sb("e1_s1", [128, NTL, NE], F32)
            cc = sb("e1_cc", [128, NTL, NE], F32)
            sm = {n: sb("e1_" + n, [128, NE], F32) for n in ("lo", "hi", "mid", "d1", "d2", "pred", "cnt", "erow")}
            ones = sb("e1_ones", [128, 128], F32)
            lst = sb("e1_lst", [128, 128], F32)
            ptot = ps.enter_context(nc.psum_tensor(U("e1_pt"), [128, 512], F32))
            pcc = [ps.enter_context(nc.psum_tensor(U("e1_pc%d" % i), [128, 512], F32)) for i in range(2)]
            pwi = [ps.enter_context(nc.psum_tensor(U("e1_pw%d" % i), [128, 512], F32)) for i in range(2)]
            r_A, r_cmp, r_M, r_s0, r_s1, r_cc, r_ones, r_lst, r_pt = R("A", "cmp", "M", "s0", "s1", "cc", "ones", "lst", "pt")
            r_pc = R("pc0", "pc1"); r_pw = R("pw0", "pw1")
            rs = {n: Res(n) for n in sm}
            S.op("dve", lambda e: e.memset(ones[:, :], 1.0), [], [r_ones])
            S.dma("sp", lambda e: e.dma_start(out=lst[:, :], in_=lstrict_in[:, :]), [], [r_lst])
            D_ = lambda fn, rd, wr_: S.op("dve", fn, rd, wr_)
            for (tok0, nt, cap, sbase, offs, gsel) in sets:
                Av = A[:, 0:nt, :]
                S.dma("sp", lambda e, Av=Av, tok0=tok0, nt=nt: e.dma_start(
                    out=Av, in_=aff_d[tok0:tok0 + nt * 128, :].rearrange("(c p) e -> p c e", p=128)), [], [r_A])
                D_(lambda e: e.memset(sm["lo"][:, :], 0.0), [], [rs["lo"]])
                D_(lambda e: e.memset(sm["hi"][:, :], 2.0), [], [rs["hi"]])
                for e_ in range(NE):
                    D_(lambda e, e_=e_, sbase=sbase: e.memset(sm["erow"][:, e_:e_ + 1], float(e_ * SLOTS + sbase)), [], [rs["erow"]])
                bc = lambda t, nt=nt: t[:, :].unsqueeze(1).to_broadcast([128, nt, NE])
                for itr in range(36):
                    D_(lambda e: e.tensor_tensor(out=sm["mid"][:, :], in0=sm["lo"][:, :], in1=sm["hi"][:, :], op=ALU.add),
                       [rs["lo"], rs["hi"]], [rs["mid"]])
                    D_(lambda e: e.tensor_scalar(out=sm["mid"][:, :], in0=sm["mid"][:, :], scalar1=0.5, scalar2=None, op0=ALU.mult),
                       [rs["mid"]], [rs["mid"]])
                    D_(lambda e, Av=Av, nt=nt, bc=bc: e.tensor_tensor(out=cmp[:, 0:nt, :], in0=Av, in1=bc(sm["mid"]), op=ALU.is_ge),
                       [r_A, rs["mid"]], [r_cmp])
                    D_(lambda e, nt=nt: e.tensor_reduce(out=sm["cnt"][:, :], in_=cmp[:, 0:nt, :].rearrange("p c e -> p e c"),
                                                        axis=AX.X, op=ALU.add), [r_cmp], [rs["cnt"]])
                    S.op("pe", lambda e: e.matmul(ptot[:, 0:NE], ones[:, :], sm["cnt"][:, :], start=True, stop=True),
                         [r_ones, rs["cnt"]], [r_pt])
                    D_(lambda e, cap=cap: e.tensor_scalar(out=sm["pred"][:, :], in0=ptot[:, 0:NE], scalar1=float(cap) - 0.5,
                                                          scalar2=None, op0=ALU.is_ge), [r_pt], [rs["pred"]])
                    D_(lambda e: e.tensor_tensor(out=sm["d1"][:, :], in0=sm["mid"][:, :], in1=sm["lo"][:, :], op=ALU.subtract),
                       [rs["mid"], rs["lo"]], [rs["d1"]])
                    D_(lambda e: e.tensor_tensor(out=sm["d1"][:, :], in0=sm["d1"][:, :], in1=sm["pred"][:, :], op=ALU.mult),
                       [rs["d1"], rs["pred"]], [rs["d1"]])
                    D_(lambda e: e.tensor_tensor(out=sm["d2"][:, :], in0=sm["hi"][:, :], in1=sm["mid"][:, :], op=ALU.subtract),
                       [rs["mid"], rs["hi"]], [rs["d2"]])
                    D_(lambda e: e.tensor_tensor(out=sm["d2"][:, :], in0=sm["d2"][:, :], in1=sm["pred"][:, :], op=ALU.mult),
                       [rs["d2"], rs["pred"]], [rs["d2"]])
                    D_(lambda e: e.tensor_tensor(out=sm["lo"][:, :], in0=sm["lo"][:, :], in1=sm["d1"][:, :], op=ALU.add),
                       [rs["lo"], rs["d1"]], [rs["lo"]])
                    D_(lambda e: e.tensor_tensor(out=sm["hi"][:, :], in0=sm["mid"][:, :], in1=sm["d2"][:, :], op=ALU.add),
                       [rs["mid"], rs["d2"]], [rs["hi"]])
                D_(lambda e, Av=Av, nt=nt, bc=bc: e.tensor_tensor(out=M[:, 0:nt, :], in0=Av, in1=bc(sm["lo"]), op=ALU.is_ge),
                   [r_A, rs["lo"]], [r_M])
                ncol = nt * NE
                Mf = M[:, :, :].rearrange("p c e -> p (c e)")
                nh = (ncol + 511) // 512
                for h in range(nh):
                    w_ = min(512, ncol - h * 512)
                    S.op("pe", lambda e, h=h, w_=w_: e.matmul(pcc[h][:, 0:w_], ones[:, :], Mf[:, h * 512:h * 512 + w_],
                                                             start=True, stop=True), [r_ones, r_M], [r_pc[h]])
                    S.op("pe", lambda e, h=h, w_=w_: e.matmul(pwi[h][:, 0:w_], lst[:, :], Mf[:, h * 512:h * 512 + w_],
                                                             start=True, stop=True), [r_lst, r_M], [r_pw[h]])
                    D_(lambda e, h=h, w_=w_: e.tensor_copy(out=cc[:, :, :].rearrange("p c e -> p (c e)")[:, h * 512:h * 512 + w_],
                                                          in_=pcc[h][:, 0:w_]), [r_pc[h]], [r_cc])
                D_(lambda e, nt=nt: e.tensor_copy(out=s0[:, 0:nt, :], in_=cc[:, 0:nt, :]), [r_cc], [r_s0])
                src, dst, r_src, r_dst = s0, s1, r_s0, r_s1
                d = 1
                while d < nt:
                    D_(lambda e, src=src, dst=dst, d=d, nt=nt: e.tensor_tensor(out=dst[:, d:nt, :], in0=src[:, d:nt, :],
                                                                             in1=src[:, 0:nt - d, :], op=ALU.add), [r_src], [r_dst])
                    D_(lambda e, src=src, dst=dst, d=d: e.tensor_copy(out=dst[:, 0:d, :], in_=src[:, 0:d, :]), [r_src], [r_dst])
                    src, dst, r_src, r_dst = dst, src, r_dst, r_src
                    d *= 2
                D_(lambda e, src=src, nt=nt: e.tensor_tensor(out=src[:, 0:nt, :], in0=src[:, 0:nt, :], in1=cc[:, 0:nt, :],
                                                            op=ALU.subtract), [r_src, r_cc], [r_src])
                srcf = src[:, :, :].rearrange("p c e -> p (c e)")
                for h in range(nh):
                    w_ = min(512, ncol - h * 512)
                    D_(lambda e, h=h, w_=w_, srcf=srcf: e.tensor_tensor(out=srcf[:, h * 512:h * 512 + w_],
                                                                       in0=srcf[:, h * 512:h * 512 + w_], in1=pwi[h][:, 0:w_],
                                                                       op=ALU.add), [r_src, r_pw[h]], [r_src])
                D_(lambda e, src=src, nt=nt, cap=cap: e.tensor_scalar(out=cmp[:, 0:nt, :], in0=src[:, 0:nt, :],
                                                                     scalar1=float(cap) - 0.5, scalar2=None, op0=ALU.is_lt),
                   [r_src], [r_cmp])
                D_(lambda e, nt=nt: e.tensor_tensor(out=M[:, 0:nt, :], in0=M[:, 0:nt, :], in1=cmp[:, 0:nt, :], op=ALU.mult),
                   [r_M, r_cmp], [r_M])
                D_(lambda e, gsel=gsel, Av=Av, nt=nt: e.tensor_tensor(out=gsel[:, :, :], in0=Av, in1=M[:, 0:nt, :], op=ALU.mult),
                   [r_A, r_M], [r_gsel])
                D_(lambda e, src=src, nt=nt, bc=bc: e.tensor_tensor(out=src[:, 0:nt, :], in0=src[:, 0:nt, :], in1=bc(sm["erow"]),
                                                                   op=ALU.add), [r_src, rs["erow"]], [r_src])
                D_(lambda e, src=src, nt=nt: e.tensor_scalar(out=src[:, 0:nt, :], in0=src[:, 0:nt, :], scalar1=-BIG, scalar2=None,
                                                            op0=ALU.add), [r_src], [r_src])
                D_(lambda e, src=src, nt=nt: e.tensor_tensor(out=src[:, 0:nt, :], in0=src[:, 0:nt, :], in1=M[:, 0:nt, :],
                                                            op=ALU.mult), [r_src, r_M], [r_src])
                D_(lambda e, src=src, nt=nt: e.tensor_scalar(out=src[:, 0:nt, :], in0=src[:, 0:nt, :], scalar1=BIG, scalar2=None,
                                                            op0=ALU.add), [r_src], [r_src])
                D_(lambda e, src=src, nt=nt, offs=offs: e.tensor_copy(out=offs[:, :, :], in_=src[:, 0:nt, :]), [r_src], [r_offs])
        return phase_end("E1_%d" % layer)

    def phase_E2(layer):
        sets = [(0, NTL, offs_l)]
        if layer == 0:
            sets.append((N, NTC, offs_c))
        with ExitStack() as ps:
            sb = lambda n, s, d: ps.enter_context(nc.sbuf_tensor(U(n), s, d))
            h2t = [sb("e2_h%d" % i, [128, D], BF16) for i in range(3)]
            r_h = R("h0", "h1", "h2")
            bcr = {}

            def mkreg(e):
                bcr["r"] = e.alloc_register(U("e2_bc"))
                return e.reg_mov(bcr["r"], NROW_XG - 1)
            S.raw("pool", mkreg)
            it = 0
            for (tok0, nt, offs) in sets:
                for c in range(nt):
                    k = it % 3; it += 1
                    t0 = tok0 + c * 128
                    S.dma("sp", lambda e, k=k, t0=t0: e.dma_start(out=h2t[k][:, :], in_=h2_d[t0:t0 + 128, :]), [], [r_h[k]])
                    for e_ in range(NE):
                        S.dma("pool", lambda e, k=k, c=c, e_=e_, offs=offs: e.indirect_dma_start(
                            out=xg_d[:, :], out_offset=bass.IndirectOffsetOnAxis(ap=offs[:, c, e_:e_ + 1], axis=0),
                            in_=h2t[k][:, :], in_offset=None, bounds_check=bcr["r"], oob_is_err=False),
                            [r_h[k], r_offs], [])
            S.raw("pool", lambda e: (e.free_register(bcr["r"]), None)[1])
        return phase_end("E2_%d" % layer)

    def phase_E3(layer):
        SL = SLOTS if layer == 0 else CAP
        stiles = [(i * 128, 128) for i in range(8)] + ([(CAP, CAPC)] if layer == 0 else [])
        chunks = [(0, 512), (512, 512)] + ([(CAP, CAPC)] if layer == 0 else [])
        with ExitStack() as ps:
            sb = lambda n, s, d: ps.enter_context(nc.sbuf_tensor(U(n), s, d))
            xgt = [sb("e3_x%d" % i, [128, D], BF16) for i in range(2)]
            xgT = sb("e3_xT", [128, KC, SLOTS], BF16)
            wgb = [sb("e3_wg%d" % i, [128, KC, 256], BF16) for i in range(2)]
            wub = [sb("e3_wu%d" % i, [128, KC, 256], BF16) for i in range(2)]
            aT = sb("e3_aT", [128, 8, SLOTS], BF16)
            wdb = [sb("e3_wd%d" % i, [128, 8, 512], BF16) for i in range(2)]
            sg = [sb("e3_sg%d" % i, [128, 512], F32) for i in range(2)]
            ysb = [sb("e3_y%d" % i, [128, 512], BF16) for i in range(2)]
            ident = sb("e3_id", [128, 128], BF16)
            ptr = [ps.enter_context(nc.psum_tensor(U("e3_pt%d" % i), [128, 1024], BF16)) for i in range(2)]
            pg = [ps.enter_context(nc.psum_tensor(U("e3_pg%d" % i), [128, 512], F32)) for i in range(2)]
            pu = [ps.enter_context(nc.psum_tensor(U("e3_pu%d" % i), [128, 512], F32)) for i in range(2)]
            py = [ps.enter_context(nc.psum_tensor(U("e3_py%d" % i), [128, 512], F32)) for i in range(2)]
            r_x = R("x0", "x1"); r_wg = R("wg0", "wg1"); r_wu = R("wu0", "wu1"); r_wd = R("wd0", "wd1")
            r_sg = R("sg0", "sg1"); r_y = R("y0", "y1"); r_pt = R("pt0", "pt1"); r_pg = R("pg0", "pg1")
            r_pu = R("pu0", "pu1"); r_py = R("py0", "py1")
            r_xT, r_aT, r_id = R("xT", "aT", "id")
            S.dma("pool", lambda e: e.dma_start(out=ident[:, :], in_=ident_in[:, :]), [], [r_id])
            xi = 0; ti = 0; wi = 0; gi = 0; di = 0; yi = 0
            for ex in range(NE):
                row0 = ex * SLOTS
                for (s0_, P_) in stiles:
                    k = xi % 2; xi += 1
                    S.dma("sp", lambda e, k=k, row0=row0, s0_=s0_, P_=P_: e.dma_start(
                        out=xgt[k][0:P_, :], in_=xg_d[row0 + s0_:row0 + s0_ + P_, :]), [], [r_x[k]])
                    for g in range(4):
                        tk = ti % 2; ti += 1
                        for j in range(8):
                            kc = g * 8 + j
                            fn = lambda e, k=k, tk=tk, j=j, kc=kc, P_=P_: e.transpose(
                                ptr[tk][:, j * P_:(j + 1) * P_], xgt[k][0:P_, kc * 128:(kc + 1) * 128], ident[0:P_, 0:P_])
                            if j == 0 or j == 7:
                                S.op("pe", fn, [r_x[k], r_id], [r_pt[tk]])
                            else:
                                S.pe_quiet(fn)
                        eng = "act" if g % 2 == 0 else "dve"
                        o_ = xgT[:, g * 8:(g + 1) * 8, s0_:s0_ + P_]
                        i_ = ptr[tk][:, 0:8 * P_].rearrange("p (j t) -> p j t", j=8)
                        if eng == "act":
                            S.op("act", lambda e, o_=o_, i_=i_: e.activation(out=o_, in_=i_, func=AF.Copy), [r_pt[tk]], [r_xT])
                        else:
                            S.op("dve", lambda e, o_=o_, i_=i_: e.tensor_copy(out=o_, in_=i_), [r_pt[tk]], [r_xT])
                for fb in range(4):
                    wk = wi % 2; wi += 1
                    S.dma("pool", lambda e, wk=wk, ex=ex, fb=fb: e.dma_start(
                        out=wgb[wk][:, :, :], in_=wg[layer, ex, :, fb * 256:(fb + 1) * 256].rearrange("(kc p) n -> p kc n", p=128)),
                        [], [r_wg[wk]])
                    S.dma("pool", lambda e, wk=wk, ex=ex, fb=fb: e.dma_start(
                        out=wub[wk][:, :, :], in_=wu[layer, ex, :, fb * 256:(fb + 1) * 256].rearrange("(kc p) n -> p kc n", p=128)),
                        [], [r_wu[wk]])
                    for sub in range(2):
                        f8 = fb * 2 + sub
                        for (c0, cn) in chunks:
                            g_ = gi % 2; gi += 1
                            mm_group(pg[g_][:, 0:cn], [(wgb[wk][:, kc, sub * 128:(sub + 1) * 128], xgT[:, kc, c0:c0 + cn])
                                                       for kc in range(KC)], [r_wg[wk], r_xT], r_pg[g_])
                            mm_group(pu[g_][:, 0:cn], [(wub[wk][:, kc, sub * 128:(sub + 1) * 128], xgT[:, kc, c0:c0 + cn])
                                                       for kc in range(KC)], [r_wu[wk], r_xT], r_pu[g_])
                            S.op("act", lambda e, g_=g_, cn=cn: e.activation(out=sg[g_][:, 0:cn], in_=pg[g_][:, 0:cn], func=AF.Silu),
                                 [r_pg[g_]], [r_sg[g_]])
                            S.op("dve", lambda e, g_=g_, cn=cn, c0=c0, f8=f8: e.tensor_tensor(
                                out=aT[:, f8, c0:c0 + cn], in0=sg[g_][:, 0:cn], in1=pu[g_][:, 0:cn], op=ALU.mult),
                                [r_sg[g_], r_pu[g_]], [r_aT])
                for nb in range(8):
                    dk = di % 2; di += 1
                    S.dma("pool", lambda e, dk=dk, ex=ex, nb=nb: e.dma_start(
                        out=wdb[dk][:, :, :], in_=wd[layer, ex, :, nb * 512:(nb + 1) * 512].rearrange("(fc p) n -> p fc n", p=128)),
                        [], [r_wd[dk]])
                    for (s0_, P_) in stiles:
                        yk = yi % 2; yi += 1
                        mm_group(py[yk][0:P_, :], [(aT[:, fc, s0_:s0_ + P_], wdb[dk][:, fc, :]) for fc in range(8)],
                                 [r_aT, r_wd[dk]], r_py[yk])
                        if yk == 0:
                            S.op("act", lambda e, yk=yk, P_=P_: e.activation(out=ysb[yk][0:P_, :], in_=py[yk][0:P_, :], func=AF.Copy),
                                 [r_py[yk]], [r_y[yk]])
                        else:
                            S.op("dve", lambda e, yk=yk, P_=P_: e.tensor_copy(out=ysb[yk][0:P_, :], in_=py[yk][0:P_, :]),
                                 [r_py[yk]], [r_y[yk]])
                        S.dma("sp", lambda e, yk=yk, row0=row0, s0_=s0_, P_=P_, nb=nb: e.dma_start(
                            out=y_d[row0 + s0_:row0 + s0_ + P_, nb * 512:(nb + 1) * 512], in_=ysb[yk][0:P_, :]), [r_y[yk]], [])
        return phase_end("E3_%d" % layer)

    def phase_T3(layer):
        last = layer == 1
        sets = [(0, NTL, 0, offs_l, gsel_l)]
        if not last:
            sets.append((N, NTC, 1, offs_c, gsel_c))
        with ExitStack() as ps:
            sb = lambda n, s, d: ps.enter_context(nc.sbuf_tensor(U(n), s, d))
            x1t = [sb("t3_x%d" % i, [128, D], F32) for i in range(2)]
            macc = [sb("t3_m%d" % i, [128, D], F32) for i in range(2)]
            G = [sb("t3_g%d" % i, [128, D], BF16) for i in range(4)]
            g5 = sb("t3_g5", [128, D], F32)
            r_x = R("x0", "x1"); r_m = R("m0", "m1"); r_G = R("G0", "G1", "G2", "G3"); r_g5, = R("g5")
            if last:
                fg = sb("t3_fg", [128, D], F32)
                junk = sb("t3_junk", [128, D], BF16)
                st = [sb("t3_st%d" % i, [128, 2], F32) for i in range(2)]
                r_fg, r_junk = R("fg", "junk"); r_st = R("st0", "st1")
                load_rows_bcast("sp", fg[:, :], fing[0:1, :], r_fg)
            for i in range(4):
                S.op("pool", lambda e, i=i: e.memset(G[i][:, :], 0.0), [], [r_G[i]])
            bcr = {}

            def mkreg(e):
                bcr["r"] = e.alloc_register(U("t3_bc"))
                return e.reg_mov(bcr["r"], NROW_XG - 1)
            S.raw("pool", mkreg)
            it = 0; gi = 0
            for (tok0, nt, row, offs, gsel) in sets:
                load_rows_bcast("sp", g5[:, :], mod_d[layer, row:row + 1, 5 * D:6 * D], r_g5)
                for c in range(nt):
                    k = it % 2; it += 1
                    t0 = tok0 + c * 128
                    S.dma("sp", lambda e, k=k, t0=t0: e.dma_start(out=x1t[k][:, :], in_=x1_d[t0:t0 + 128, :]), [], [r_x[k]])
                    for e_ in range(NE):
                        g = gi % 4; gi += 1
                        S.dma("pool", lambda e, g=g, c=c, e_=e_, offs=offs: e.indirect_dma_start(
                            out=G[g][:, :], out_offset=None, in_=y_d[:, :],
                            in_offset=bass.IndirectOffsetOnAxis(ap=offs[:, c, e_:e_ + 1], axis=0),
                            bounds_check=bcr["r"], oob_is_err=False), [r_offs], [r_G[g]])
                        if e_ == 0:
                            S.op("dve", lambda e, g=g, k=k, c=c, gsel=gsel: e.tensor_scalar(
                                out=macc[k][:, :], in0=G[g][:, :], scalar1=gsel[:, c, 0:1], scalar2=None, op0=ALU.mult),
                                [r_G[g], r_gsel], [r_m[k]])
                        else:
                            S.op("dve", lambda e, g=g, k=k, c=c, e_=e_, gsel=gsel: e.scalar_tensor_tensor(
                                out=macc[k][:, :], in0=G[g][:, :], scalar=gsel[:, c, e_:e_ + 1], in1=macc[k][:, :],
                                op0=ALU.mult, op1=ALU.add), [r_G[g], r_gsel, r_m[k]], [r_m[k]])
                    S.op("dve", lambda e, k=k: e.tensor_tensor(out=macc[k][:, :], in0=macc[k][:, :], in1=g5[:, :], op=ALU.mult),
                         [r_m[k], r_g5], [r_m[k]])
                    S.op("pool", lambda e, k=k: e.tensor_tensor(out=x1t[k][:, :], in0=x1t[k][:, :], in1=macc[k][:, :], op=ALU.add),
                         [r_x[k], r_m[k]], [r_x[k]])
                    if not last:
                        S.dma("sp", lambda e, k=k, t0=t0: e.dma_start(out=x2_d[t0:t0 + 128, :], in_=x1t[k][:, :]), [r_x[k]], [])
                    else:
                        S.op("dve", lambda e, k=k: e.memset(st[k][:, 0:1], 0.0), [], [r_st[k]])
                        S.op("act", lambda e, k=k: e.activation(out=junk[:, :], in_=x1t[k][:, :], func=AF.Square,
                                                                accum_out=st[k][:, 0:1]), [r_x[k], r_st[k]], [r_junk, r_st[k]])
                        S.op("dve", lambda e, k=k: e.tensor_scalar(out=st[k][:, 1:2], in0=st[k][:, 0:1], scalar1=1.0 / D,
                                                                   scalar2=EPS, op0=ALU.mult, op1=ALU.add), [r_st[k]], [r_st[k]])
                        S.op("act", lambda e, k=k: e.activation(out=st[k][:, 1:2], in_=st[k][:, 1:2], func=AF.Sqrt),
                             [r_st[k]], [r_st[k]])
                        S.op("dve", lambda e, k=k: e.reciprocal(out=st[k][:, 1:2], in_=st[k][:, 1:2]), [r_st[k]], [r_st[k]])
                        S.op("dve", lambda e, k=k: e.scalar_tensor_tensor(out=x1t[k][:, :], in0=x1t[k][:, :], scalar=st[k][:, 1:2],
                                                                          in1=fg[:, :], op0=ALU.mult, op1=ALU.mult),
                             [r_x[k], r_st[k], r_fg], [r_x[k]])
                        S.dma("sp", lambda e, k=k, t0=t0: e.dma_start(out=out_d[t0:t0 + 128, :], in_=x1t[k][:, :]), [r_x[k]], [])
            S.raw("pool", lambda e: (e.free_register(bcr["r"]), None)[1])
        return phase_end("T3_%d" % layer)

    plan = [("A", phase_A), ("T1_0", lambda: phase_T1(0)), ("H1_0", lambda: phase_H1(0)), ("N", phase_N),
            ("T2a_0", lambda: phase_T2a(0)), ("T2b_0", lambda: phase_T2b(0)),
            ("E1_0", lambda: phase_E1(0)), ("E2_0", lambda: phase_E2(0)), ("E3_0", lambda: phase_E3(0)),
            ("T3_0", lambda: phase_T3(0)),
            ("T1_1", lambda: phase_T1(1)), ("H1_1", lambda: phase_H1(1)), ("DA", phase_DA),
            ("T2a_1", lambda: phase_T2a(1)), ("T2b_1", lambda: phase_T2b(1)),
            ("E1_1", lambda: phase_E1(1)), ("E2_1", lambda: phase_E2(1)), ("E3_1", lambda: phase_E3(1)),
            ("T3_1", lambda: phase_T3(1))]
    for name, fn in plan:
        if phases is not None and name not in phases:
            continue
        if fn():
            break

    es.close()
    nc._declared_inputs = list(declared.keys())
    return nc, list(declared.keys())


def _host_constants():
    ident = np.eye(128, dtype=np.float32)
    lstrict = np.triu(np.ones((128, 128), np.float32), 1)
    qc = np.arange(GRID_W)
    col_start = np.clip(qc - 8, 0, GRID_W - 16)
    col_mask = (qc[None, :] >= col_start[:, None]) & (qc[None, :] < col_start[:, None] + 16)
    nmask = np.where(col_mask, 0.0, -1e30).astype(np.float32)
    col_idx = np.clip(qc[None, :] - qc[:, None] + 15, 0, 30)
    t = np.arange(N)
    row = (t // GRID_W).astype(np.float32)
    col = (t % GRID_W).astype(np.float32)
    freq = (10000.0 ** (-np.arange(32, dtype=np.float32) / 32)).astype(np.float32)
    ang = np.concatenate([row[:, None] * freq, col[:, None] * freq], axis=-1).astype(np.float32)
    return dict(ident=ident, lstrict=lstrict, nmask=nmask, col_idx=col_idx,
                rope_cos=np.cos(ang).astype(np.float32), rope_sin=np.sin(ang).astype(np.float32))


def make_in_maps(inp):
    cst = _host_constants()
    f = lambda a: np.ascontiguousarray(np.asarray(a, dtype=np.float32))
    rpb = f(inp["na_rpb"])[0]
    nb = rpb[:, :, cst["col_idx"]]
    nb = np.ascontiguousarray(nb.transpose(0, 2, 1, 3)).reshape(32, 64, 15 * 64)
    lam = np.concatenate([f(inp[k]) for k in ("da_lambda_q1", "da_lambda_k1", "da_lambda_q2", "da_lambda_k2")], 0)
    shared = dict(
        ada_w=f(inp["ada_w"]), ada_b=f(inp["ada_b"]), norm1_g=f(inp["norm1_g"]), norm2_g=f(inp["norm2_g"]),
        final_g=f(inp["final_g"]).reshape(1, D), na_w_qkv=f(inp["na_w_qkv"])[0], da_w_qkv=f(inp["da_w_qkv"])[0],
        na_w_o=f(inp["na_w_o"])[0], da_w_o=f(inp["da_w_o"])[0], nbias=nb, nmask=cst["nmask"], lam=lam,
        subln_g=f(inp["da_subln_g"]).reshape(1, 256), w_router=f(inp["moe_w_router"]), w_gate=f(inp["moe_w_gate"]),
        w_up=f(inp["moe_w_up"]), w_down=f(inp["moe_w_down"]), ident=cst["ident"], lstrict=cst["lstrict"],
        rope_cos=cst["rope_cos"], rope_sin=cst["rope_sin"])
    maps = []
    x = f(inp["x"]); c = f(inp["c"]); ctx = f(inp["ctx"]); cc = f(inp["c_ctx"])
    for b in range(2):
        cv = np.stack([c[b], cc], axis=-1)
        cT = np.ascontiguousarray(cv.reshape(KC, 128, 2).transpose(1, 0, 2))
        m = dict(shared)
        m.update(x=x[b], ctx=ctx[b], cT=cT)
        maps.append(m)
    return maps


def kernel(**inputs):
    nc, names = build_program()
    maps = [{k: m[k] for k in names} for m in make_in_maps(inputs)]
    res = run_bass_kernel_spmd(nc, maps, core_ids=[0, 1])
    return np.stack([np.asarray(res.results[b]["out"], dtype=np.float32) for b in range(2)], 0)
```

```python
import numpy as np
from contextlib import ExitStack
import concourse.bass as bass
import concourse.mybir as mybir
from concourse.bass_utils import run_bass_kernel_spmd

F32 = mybir.dt.float32
BF16 = mybir.dt.bfloat16
I32 = mybir.dt.int32
AF = mybir.ActivationFunctionType
ALU = mybir.AluOpType
AX = mybir.AxisListType

D = 4096
KC = D // 128
N = 8192
NCX = 256
NT = N + NCX
NTL = N // 128
NTC = NCX // 128
NE = 16
FF = 1024
CAP = 1024
CAPC = 32
SLOTS = CAP + CAPC
GRID_W = 64
NROWS = N // GRID_W
EPS = 1e-6
SUBLN_EPS = 1e-5
BIG = 1.0e6


class Res:
    __slots__ = ("name", "w", "r")

    def __init__(self, name):
        self.name = name
        self.w = None
        self.r = {}


class Sched:
    CE = ("pe", "act", "dve", "pool")

    def __init__(self, nc, es, ndma=12):
        self.nc = nc
        self.semobj = {}
        self.cnt = {}
        for e in self.CE:
            self.semobj[e] = es.enter_context(nc.semaphore("s_" + e))
            self.cnt[e] = 0
        self.queues = ("sp", "pool")
        self.dkeys = {}
        self.dnext = {}
        for q in self.queues:
            ks = []
            for k in range(ndma):
                key = (q, k)
                self.semobj[key] = es.enter_context(nc.semaphore("d_%s%d" % (q, k)))
                self.cnt[key] = 0
                ks.append(key)
            self.dkeys[q] = ks
            self.dnext[q] = 0
        self.issuers = ("pe", "act", "dve", "pool", "sp")
        self.waited = {i: {} for i in self.issuers}
        self.thunks = {i: [] for i in self.issuers}
        self.ninst = 0

    def _wait(self, issuer, ev):
        if ev is None:
            return
        key, val = ev
        if self.waited[issuer].get(key, 0) >= val:
            return
        self.waited[issuer][key] = val
        sem = self.semobj[key]
        self.thunks[issuer].append(lambda e, sem=sem, val=val: e.wait_ge(sem, val))

    def _deps(self, issuer, reads, writes):
        for r in reads:
            if r.w is not None and not (issuer == "pe" and r.w[0] == "pe"):
                self._wait(issuer, r.w)
        for w in writes:
            if w.w is not None and not (issuer == "pe" and w.w[0] == "pe"):
                self._wait(issuer, w.w)
            for key, val in w.r.items():
                if not (issuer == "pe" and key == "pe"):
                    self._wait(issuer, (key, val))

    def _mark(self, ev, reads, writes):
        key, val = ev
        for r in reads:
            if r.r.get(key, 0) < val:
                r.r[key] = val
        for w in writes:
            w.w = ev
            w.r = {}

    def op(self, eng, fn, reads=(), writes=()):
        self._deps(eng, reads, writes)
        self.cnt[eng] += 1
        val = self.cnt[eng]
        sem = self.semobj[eng]
        self.thunks[eng].append(lambda e, fn=fn, sem=sem: fn(e).then_inc(sem, 1))
        ev = (eng, val)
        self._mark(ev, reads, writes)
        self.ninst += 1
        return ev

    def raw(self, issuer, fn):
        self.thunks[issuer].append(lambda e, fn=fn: fn(e))

    def pe_quiet(self, fn):
        self.thunks["pe"].append(lambda e, fn=fn: fn(e))
        self.ninst += 1

    def dma(self, q, fn, reads=(), writes=()):
        self._deps(q, reads, writes)
        ks = self.dkeys[q]
        key = ks[self.dnext[q] % len(ks)]
        self.dnext[q] += 1
        if self.cnt[key] > 0:
            self._wait(q, (key, self.cnt[key]))
        self.cnt[key] += 16
        val = self.cnt[key]
        sem = self.semobj[key]
        self.thunks[q].append(lambda e, fn=fn, sem=sem: fn(e).then_inc(sem, 16))
        ev = (key, val)
        self._mark(ev, reads, writes)
        self.ninst += 1
        return ev

    def flush(self, name):
        nc = self.nc
        for q in self.queues:
            for key in self.dkeys[q]:
                if self.cnt[key] > 0:
                    self._wait(q, (key, self.cnt[key]))
        th = self.thunks
        with nc.Block(name) as block:
            if th["sp"]:
                @block.sync
                def _(e):
                    for t in th["sp"]:
                        t(e)
            if th["pe"]:
                @block.tensor
                def _(e):
                    for t in th["pe"]:
                        t(e)
            if th["act"]:
                @block.scalar
                def _(e):
                    for t in th["act"]:
                        t(e)
            if th["dve"]:
                @block.vector
                def _(e):
                    for t in th["dve"]:
                        t(e)
            if th["pool"]:
                @block.gpsimd
                def _(e):
                    for t in th["pool"]:
                        t(e)
        self.thunks = {i: [] for i in self.issuers}
        for i in self.issuers:
            for key, val in self.cnt.items():
                self.waited[i][key] = val


def R(*names):
    return [Res(n) for n in names]


def build_program(stop_after=None, debug=(), inject=(), phases=None):
    nc = bass.Bass("TRN2", target_bir_lowering=False)
    es = ExitStack()
    S = Sched(nc, es)

    declared = {}

    class _Lazy:
        def __init__(self, name, shape):
            self.name, self.shape, self.t = name, list(shape), None

        def _get(self):
            if self.t is None:
                self.t = nc.dram_tensor(self.name, self.shape, F32, kind="ExternalInput")
                declared[self.name] = self.t
            return self.t

        def __getitem__(self, key):
            return self._get()[key]

    def din(name, shape):
        return _Lazy(name, shape)

    def dscr(name, shape, dt):
        kind = "ExternalOutput" if name in debug else ("ExternalInput" if name in inject else "Internal")
        return nc.dram_tensor(name, list(shape), dt, kind=kind)

    x_in = din("x", [N, D])
    ctx_in = din("ctx", [NCX, D])
    cT_in = din("cT", [128, KC, 2])
    ada_w = din("ada_w", [2, D, 6 * D])
    ada_b = din("ada_b", [2, 6 * D])
    n1g = din("norm1_g", [2, D])
    n2g = din("norm2_g", [2, D])
    fing = din("final_g", [1, D])
    wqkv = [din("na_w_qkv", [D, 3 * D]), din("da_w_qkv", [D, 3 * D])]
    wo = [din("na_w_o", [D, D]), din("da_w_o", [D, D])]
    nbias = din("nbias", [32, 64, 15 * 64])
    nmask = din("nmask", [64, 64])
    lamp = din("lam", [4, 128])
    sublng = din("subln_g", [1, 256])
    wr = din("w_router", [2, D, NE])
    wg = din("w_gate", [2, NE, D, FF])
    wu = din("w_up", [2, NE, D, FF])
    wd = din("w_down", [2, NE, FF, D])
    ident_in = din("ident", [128, 128])
    lstrict_in = din("lstrict", [128, 128])
    rope_cos = din("rope_cos", [N, 64])
    rope_sin = din("rope_sin", [N, 64])
    out_d = nc.dram_tensor("out", [N, D], F32, kind="ExternalOutput")

    mod_d = dscr("mod_d", [2, 2, 6 * D], F32)
    hT_d = dscr("hT_d", [128, KC, NT], BF16)
    qkv_d = dscr("qkv_d", [NT, 3 * D], BF16)
    oT_d = dscr("oT_d", [128, KC, NT], BF16)
    x1_d = dscr("x1_d", [NT, D], F32)
    x2_d = dscr("x2_d", [NT, D], F32)
    h2_d = dscr("h2_d", [NT, D], BF16)
    aff_d = dscr("aff_d", [NT, NE], F32)
    xg_d = dscr("xg_d", [NE * SLOTS, D], BF16)
    y_d = dscr("y_d", [NE * SLOTS, D], BF16)

    state = {"done": False, "uid": 0}

    def U(n):
        return "%s_u%d" % (n, state["uid"])

    def phase_end(name):
        S.flush(name)
        state["uid"] += 1
        if stop_after == name:
            state["done"] = True
        return state["done"]

    def phase_A():
        with ExitStack() as ps:
            sb = lambda n, s, d: ps.enter_context(nc.sbuf_tensor(U(n), s, d))
            cT = sb("a_cT", [128, KC, 2], F32)
            scT = sb("a_scT", [128, KC, 2], F32)
            wbuf = [sb("a_w%d" % i, [128, KC, 512], F32) for i in range(2)]
            bt = [sb("a_b%d" % i, [2, 512], F32) for i in range(2)]
            ot = [sb("a_o%d" % i, [2, 512], F32) for i in range(2)]
            pacc = [ps.enter_context(nc.psum_tensor(U("a_p%d" % i), [128, 512], F32)) for i in range(2)]
            r_cT, r_scT = R("cT", "scT")
            r_w = R("w0", "w1"); r_b = R("b0", "b1"); r_o = R("o0", "o1"); r_p = R("p0", "p1")
            S.dma("sp", lambda e: e.dma_start(out=cT[:, :, :], in_=cT_in[:, :, :]), [], [r_cT])
            S.op("act", lambda e: e.activation(out=scT[:, :, :], in_=cT[:, :, :], func=AF.Silu), [r_cT], [r_scT])
            it = 0
            for i in range(2):
                for cb in range(6 * D // 512):
                    k = it % 2
                    it += 1
                    c0 = cb * 512
                    S.dma("sp", lambda e, i=i, c0=c0, k=k: e.dma_start(
                        out=wbuf[k][:, :, :],
                        in_=ada_w[i, :, c0:c0 + 512].rearrange("(kc p) n -> p kc n", p=128)), [], [r_w[k]])
                    S.dma("sp", lambda e, i=i, c0=c0, k=k: e.dma_start(
                        out=bt[k][:, :], in_=ada_b[i:i + 1, c0:c0 + 512].partition_broadcast(2)), [], [r_b[k]])
                    for kc in range(KC):
                        fn = lambda e, k=k, kc=kc: e.matmul(pacc[k][0:2, :], scT[:, kc, :], wbuf[k][:, kc, :],
                                                            start=(kc == 0), stop=(kc == KC - 1))
                        if kc < KC - 1:
                            if kc == 0:
                                S.op("pe", fn, [r_scT, r_w[k]], [r_p[k]])
                            else:
                                S.pe_quiet(fn)
                        else:
                            S.op("pe", fn, [r_scT, r_w[k]], [r_p[k]])
                    S.op("dve", lambda e, k=k: e.tensor_tensor(out=ot[k][:, :], in0=pacc[k][0:2, :], in1=bt[k][:, :],
                                                               op=ALU.add), [r_p[k], r_b[k]], [r_o[k]])
                    S.dma("pool", lambda e, i=i, c0=c0, k=k: e.dma_start(out=mod_d[i, :, c0:c0 + 512], in_=ot[k][:, :]),
                          [r_o[k]], [])
        return phase_end("A")

    def src_rows(layer, t0, n):
        if layer == 0:
            if t0 < N:
                return x_in[t0:t0 + n, :]
            return ctx_in[t0 - N:t0 - N + n, :]
        return x2_d[t0:t0 + n, :]

    def load_rows_bcast(q, dst, src_ap, res):
        S.dma(q, lambda e: e.dma_start(out=dst, in_=src_ap.partition_broadcast(128)), [], [res])

    def phase_T1(layer):
        with ExitStack() as ps:
            sb = lambda n, s, d: ps.enter_context(nc.sbuf_tensor(U(n), s, d))
            xt = [sb("t1_x%d" % i, [128, D], F32) for i in range(2)]
            hb = [sb("t1_h%d" % i, [128, D], BF16) for i in range(2)]
            hT4 = [sb("t1_hT%d" % i, [128, KC, 512], BF16) for i in range(2)]
            Arow = sb("t1_A", [128, D], F32)
            Brow = sb("t1_B", [128, D], F32)
            tmp = sb("t1_tmp", [128, D], F32)
            st = [sb("t1_st%d" % i, [128, 2], F32) for i in range(2)]
            ident = sb("t1_id", [128, 128], BF16)
            ptr = [ps.enter_context(nc.psum_tensor(U("t1_p%d" % i), [128, 1024], BF16)) for i in range(4)]
            r_x = R("x0", "x1"); r_h = R("h0", "h1"); r_hT = R("hT0", "hT1"); r_st = R("st0", "st1")
            r_A, r_B, r_tmp, r_id = R("A", "B", "tmp", "id")
            r_p = R("p0", "p1", "p2", "p3")
            S.dma("pool", lambda e: e.dma_start(out=ident[:, :], in_=ident_in[:, :]), [], [r_id])
            it = 0
            si = 0
            for grp, (tok0, ntile, row) in enumerate(((0, NTL, 0), (N, NTC, 1))):
                load_rows_bcast("sp", Arow[:, :], mod_d[layer, row:row + 1, D:2 * D], r_A)
                load_rows_bcast("sp", tmp[:, :], n1g[layer:layer + 1, :], r_tmp)
                load_rows_bcast("sp", Brow[:, :], mod_d[layer, row:row + 1, 0:D], r_B)
                S.op("dve", lambda e: e.scalar_tensor_tensor(out=Arow[:, :], in0=Arow[:, :], scalar=1.0, in1=tmp[:, :],
                                                             op0=ALU.add, op1=ALU.mult), [r_A, r_tmp], [r_A])
                for tt in range(ntile):
                    k = it % 2
                    sti = si % 2
                    sub = tt % 4
                    t0 = tok0 + tt * 128
                    S.dma("sp", lambda e, k=k, t0=t0: e.dma_start(out=xt[k][:, :], in_=src_rows(layer, t0, 128)),
                          [], [r_x[k]])
                    S.op("dve", lambda e, k=k: e.memset(st[k][:, :], 0.0), [], [r_st[k]])
                    S.op("act", lambda e, k=k: e.activation(out=hb[k][:, :], in_=xt[k][:, :], func=AF.Square,
                                                            accum_out=st[k][:, 0:1]), [r_x[k], r_st[k]], [r_h[k], r_st[k]])
                    S.op("dve", lambda e, k=k: e.tensor_scalar(out=st[k][:, 1:2], in0=st[k][:, 0:1], scalar1=1.0 / D,
                                                               scalar2=EPS, op0=ALU.mult, op1=ALU.add), [r_st[k]], [r_st[k]])
                    S.op("act", lambda e, k=k: e.activation(out=st[k][:, 1:2], in_=st[k][:, 1:2], func=AF.Sqrt),
                         [r_st[k]], [r_st[k]])
                    S.op("dve", lambda e, k=k: e.reciprocal(out=st[k][:, 1:2], in_=st[k][:, 1:2]), [r_st[k]], [r_st[k]])
                    S.op("dve", lambda e, k=k: e.scalar_tensor_tensor(out=xt[k][:, :], in0=xt[k][:, :],
                                                                      scalar=st[k][:, 1:2], in1=Arow[:, :],
                                                                      op0=ALU.mult, op1=ALU.mult),
                         [r_x[k], r_st[k], r_A], [r_x[k]])
                    S.op("pool", lambda e, k=k: e.tensor_tensor(out=hb[k][:, :], in0=xt[k][:, :], in1=Brow[:, :],
                                                                op=ALU.add), [r_x[k], r_B], [r_h[k]])
                    for g in range(4):
                        for j in range(8):
                            kc = g * 8 + j
                            fn = lambda e, k=k, g=g, j=j, kc=kc: e.transpose(ptr[g][:, j * 128:(j + 1) * 128],
                                                                             hb[k][:, kc * 128:(kc + 1) * 128], ident[:, :])
                            if j == 0 or j == 7:
                                S.op("pe", fn, [r_h[k], r_id], [r_p[g]])
                            else:
                                S.pe_quiet(fn)
                        eng = "act" if g % 2 == 0 else "dve"
                        if eng == "act":
                            S.op("act", lambda e, g=g, sti=sti, sub=sub: e.activation(
                                out=hT4[sti][:, g * 8:(g + 1) * 8, sub * 128:(sub + 1) * 128],
                                in_=ptr[g][:, :].rearrange("p (j t) -> p j t", j=8), func=AF.Copy), [r_p[g]], [r_hT[sti]])
                        else:
                            S.op("dve", lambda e, g=g, sti=sti, sub=sub: e.tensor_copy(
                                out=hT4[sti][:, g * 8:(g + 1) * 8, sub * 128:(sub + 1) * 128],
                                in_=ptr[g][:, :].rearrange("p (j t) -> p j t", j=8)), [r_p[g]], [r_hT[sti]])
                    it += 1
                    if sub == 3 or tt == ntile - 1:
                        nn = (sub + 1) * 128
                        s0 = t0 - sub * 128
                        S.dma("pool", lambda e, sti=sti, s0=s0, nn=nn: e.dma_start(out=hT_d[:, :, s0:s0 + nn],
                                                                                 in_=hT4[sti][:, :, 0:nn]), [r_hT[sti]], [])
                        si += 1
        return phase_end("T1_%d" % layer)


    SCALE = 128.0 ** -0.5

    def mm_group(out_ap, pairs, reads, wres):
        n = len(pairs)
        for i, (l, r) in enumerate(pairs):
            fn = lambda e, l=l, r=r, i=i: e.matmul(out_ap, l, r, start=(i == 0), stop=(i == n - 1))
            if i == 0 or i == n - 1:
                S.op("pe", fn, reads, [wres])
            else:
                S.pe_quiet(fn)

    def phase_H1(layer):
        W = wqkv[layer]
        with ExitStack() as ps:
            sb = lambda n, s, d: ps.enter_context(nc.sbuf_tensor(U(n), s, d))
            wb = sb("h1_w", [128, KC, 1024], BF16)
            hT4 = [sb("h1_hT%d" % i, [128, KC, 512], BF16) for i in range(2)]
            stage = [sb("h1_st%d" % i, [128, 1024], BF16) for i in range(2)]
            pacc = [ps.enter_context(nc.psum_tensor(U("h1_p%d" % i), [128, 512], F32)) for i in range(4)]
            r_w, = R("w"); r_hT = R("hT0", "hT1"); r_st = R("st0", "st1"); r_p = R("p0", "p1", "p2", "p3")
            if layer == 1:
                cs = [sb("h1_cos%d" % i, [128, 64], F32) for i in range(2)]
                sn = [sb("h1_sin%d" % i, [128, 64], F32) for i in range(2)]
                tm = [sb("h1_tm%d" % i, [128, 4, 64], F32) for i in range(4)]
                r_cs = R("cs0", "cs1"); r_tm = R("tm0", "tm1", "tm2", "tm3")
            hi = 0; si = 0; pi = 0; ci = 0
            for cb in range(12):
                S.dma("pool", lambda e, cb=cb: e.dma_start(
                    out=wb[:, :, :], in_=W[:, cb * 1024:(cb + 1) * 1024].rearrange("(kc p) n -> p kc n", p=128)),
                    [], [r_w])
                def load_hT(st_, k_):
                    nt_ = 4 if st_ < 16 else 2
                    S.dma("sp", lambda e, k_=k_, st_=st_, nt_=nt_: e.dma_start(
                        out=hT4[k_][:, :, 0:nt_ * 128], in_=hT_d[:, :, st_ * 512:st_ * 512 + nt_ * 128]), [], [r_hT[k_]])
                if cb == 0:
                    load_hT(0, hi % 2)
                for st in range(17):
                    ntile = 4 if st < 16 else 2
                    k = hi % 2; hi += 1
                    if st + 1 < 17:
                        load_hT(st + 1, hi % 2)
                    elif cb + 1 < 12:
                        load_hT(0, hi % 2)
                    for ts in range(ntile):
                        t0 = st * 512 + ts * 128
                        sk = si % 2; si += 1
                        rope = (layer == 1 and cb < 8 and st < 16)
                        if rope:
                            ck = ci % 2; ci += 1
                            S.dma("sp", lambda e, ck=ck, t0=t0: e.dma_start(out=cs[ck][:, :], in_=rope_cos[t0:t0 + 128, :]),
                                  [], [r_cs[ck]])
                            S.dma("sp", lambda e, ck=ck, t0=t0: e.dma_start(out=sn[ck][:, :], in_=rope_sin[t0:t0 + 128, :]),
                                  [], [r_cs[ck]])
                        for half in range(2):
                            p = pi % 4; pi += 1
                            mm_group(pacc[p][:, :],
                                     [(hT4[k][:, kc, ts * 128:(ts + 1) * 128], wb[:, kc, half * 512:(half + 1) * 512])
                                      for kc in range(KC)], [r_hT[k], r_w], r_p[p])
                            dst = stage[sk][:, half * 512:(half + 1) * 512]
                            if not rope:
                                if half == 0:
                                    S.op("act", lambda e, dst=dst, p=p: e.activation(out=dst, in_=pacc[p][:, :], func=AF.Copy),
                                         [r_p[p]], [r_st[sk]])
                                else:
                                    S.op("dve", lambda e, dst=dst, p=p: e.tensor_copy(out=dst, in_=pacc[p][:, :]),
                                         [r_p[p]], [r_st[sk]])
                            else:
                                pv = pacc[p][:, :].rearrange("p (g i two) -> p g i two", g=4, i=64, two=2)
                                dv = dst.rearrange("p (g i two) -> p g i two", g=4, i=64, two=2)
                                cb_ = cs[ck][:, :].unsqueeze(1).to_broadcast([128, 4, 64])
                                sb_ = sn[ck][:, :].unsqueeze(1).to_broadcast([128, 4, 64])
                                xe, xo = pv[:, :, :, 0], pv[:, :, :, 1]
                                S.op("dve", lambda e, xe=xe, cb_=cb_: e.tensor_tensor(out=tm[0][:, :, :], in0=xe, in1=cb_, op=ALU.mult),
                                     [r_p[p], r_cs[ck]], [r_tm[0]])
                                S.op("dve", lambda e, xo=xo, sb_=sb_: e.tensor_tensor(out=tm[1][:, :, :], in0=xo, in1=sb_, op=ALU.mult),
                                     [r_p[p], r_cs[ck]], [r_tm[1]])
                                S.op("dve", lambda e, xe=xe, sb_=sb_: e.tensor_tensor(out=tm[2][:, :, :], in0=xe, in1=sb_, op=ALU.mult),
                                     [r_p[p], r_cs[ck]], [r_tm[2]])
                                S.op("dve", lambda e, xo=xo, cb_=cb_: e.tensor_tensor(out=tm[3][:, :, :], in0=xo, in1=cb_, op=ALU.mult),
                                     [r_p[p], r_cs[ck]], [r_tm[3]])
                                S.op("pool", lambda e, dv=dv: e.tensor_tensor(out=dv[:, :, :, 0], in0=tm[0][:, :, :], in1=tm[1][:, :, :],
                                                                              op=ALU.subtract), [r_tm[0], r_tm[1]], [r_st[sk]])
                                S.op("pool", lambda e, dv=dv: e.tensor_tensor(out=dv[:, :, :, 1], in0=tm[2][:, :, :], in1=tm[3][:, :, :],
                                                                              op=ALU.add), [r_tm[2], r_tm[3]], [r_st[sk]])
                        S.dma("pool", lambda e, sk=sk, t0=t0, cb=cb: e.dma_start(
                            out=qkv_d[t0:t0 + 128, cb * 1024:(cb + 1) * 1024], in_=stage[sk][:, :]), [r_st[sk]], [])
        return phase_end("H1_%d" % layer)

    def phase_N():
        with ExitStack() as ps:
            sb = lambda n, s, d: ps.enter_context(nc.sbuf_tensor(U(n), s, d))
            qtm = sb("n_qtm", [128, 66, 128], BF16)
            ktm = sb("n_ktm", [128, 66, 128], BF16)
            QT = sb("n_QT", [128, NT], BF16)
            KT = sb("n_KT", [128, NT], BF16)
            Va = [sb("n_va%d" % i, [128, 64, 128], BF16) for i in range(2)]
            Vb = [sb("n_vb%d" % i, [128, 63, 128], BF16) for i in range(2)]
            Vc = [sb("n_vc%d" % i, [128, 2, 128], BF16) for i in range(2)]
            bias = [sb("n_bias%d" % i, [64, 960], F32) for i in range(2)]
            oTh = sb("n_oT", [128, NT], BF16)
            sbs = [sb("n_s%d" % i, [128, 768], F32) for i in range(2)]
            pbf = [sb("n_p%d" % i, [128, 768], BF16) for i in range(2)]
            pT = [sb("n_pT%d" % i, [128, 384], BF16) for i in range(2)]
            stt = [sb("n_stt%d" % i, [128, 4], F32) for i in range(2)]
            stt2 = [sb("n_stt2%d" % i, [128, 4], F32) for i in range(2)]
            obf = [sb("n_o%d" % i, [128, 128], BF16) for i in range(2)]
            ident = sb("n_id", [128, 128], BF16)
            msk = sb("n_msk", [64, 64], F32)
            ptq0 = ps.enter_context(nc.psum_tensor(U("n_ptq0"), [128, 1024], BF16))
            ptq1 = ps.enter_context(nc.psum_tensor(U("n_ptq1"), [128, 1024], BF16))
            ps_a = [ps.enter_context(nc.psum_tensor(U("n_pa%d" % i), [128, 512], F32)) for i in range(2)]
            ps_b = [ps.enter_context(nc.psum_tensor(U("n_pb%d" % i), [128, 512], F32)) for i in range(2)]
            ps_o = [ps.enter_context(nc.psum_tensor(U("n_po%d" % i), [128, 512], F32)) for i in range(2)]
            r_qtm, r_ktm, r_QT, r_KT, r_oTh, r_id, r_msk, r_q0, r_q1a, r_q1b = R(
                "qtm", "ktm", "QT", "KT", "oTh", "id", "msk", "q0", "q1a", "q1b")
            r_V = R("V0", "V1"); r_bias = R("b0", "b1")
            r_s = R("s0", "s1"); r_p = R("p0", "p1"); r_pT = R("pT0", "pT1"); r_stt = R("t0", "t1"); r_o = R("o0", "o1")
            r_pa = R("pa0", "pa1"); r_pb = R("pb0", "pb1"); r_po = R("po0", "po1"); r_stt2 = R("u0", "u1")
            S.dma("pool", lambda e: e.dma_start(out=ident[:, :], in_=ident_in[:, :]), [], [r_id])
            S.dma("sp", lambda e: e.dma_start(out=msk[:, :], in_=nmask[:, :]), [], [r_msk])
            wi = 0
            for hh in range(32):
                hb = hh % 2
                qs = lambda c0: qkv_d[:, c0:c0 + 128]
                S.dma("sp", lambda e, hh=hh: e.dma_start(
                    out=qtm[:, :, :], in_=qkv_d[:, hh * 128:(hh + 1) * 128].rearrange("(c p) d -> p c d", p=128)), [], [r_qtm])
                S.dma("sp", lambda e, hh=hh: e.dma_start(
                    out=ktm[:, :, :], in_=qkv_d[:, D + hh * 128:D + (hh + 1) * 128].rearrange("(c p) d -> p c d", p=128)),
                    [], [r_ktm])
                vcol = 2 * D + hh * 128
                S.dma("sp", lambda e, hb=hb, vcol=vcol: e.dma_start(
                    out=Va[hb][:, :, :], in_=qkv_d[0:N, vcol:vcol + 128].rearrange("(c p) d -> p c d", p=128)), [], [r_V[hb]])
                S.dma("sp", lambda e, hb=hb, vcol=vcol: e.dma_start(
                    out=Vb[hb][:, :, :], in_=qkv_d[64:64 + 63 * 128, vcol:vcol + 128].rearrange("(c p) d -> p c d", p=128)),
                    [], [r_V[hb]])
                S.dma("sp", lambda e, hb=hb, vcol=vcol: e.dma_start(
                    out=Vc[hb][:, :, :], in_=qkv_d[N:NT, vcol:vcol + 128].rearrange("(c p) d -> p c d", p=128)), [], [r_V[hb]])
                S.dma("sp", lambda e, hb=hb, hh=hh: e.dma_start(out=bias[hb][:, :], in_=nbias[hh, :, :]), [], [r_bias[hb]])
                S.op("dve", lambda e, hb=hb: e.tensor_tensor(
                    out=bias[hb][:, :].rearrange("p (j k) -> p j k", j=15),
                    in0=bias[hb][:, :].rearrange("p (j k) -> p j k", j=15),
                    in1=msk[:, :].unsqueeze(1).to_broadcast([64, 15, 64]), op=ALU.add), [r_bias[hb], r_msk], [r_bias[hb]])
                for (src, r_src, dstT, r_dst) in ((qtm, r_qtm, QT, r_QT), (ktm, r_ktm, KT, r_KT)):
                    for c0 in range(0, 66, 8):
                        nb = min(8, 66 - c0)
                        for j in range(nb):
                            fn = lambda e, src=src, c=c0 + j, j=j: e.transpose(ptq0[:, j * 128:(j + 1) * 128], src[:, c, :], ident[:, :])
                            if j == 0 or j == nb - 1:
                                S.op("pe", fn, [r_src, r_id], [r_q0])
                            else:
                                S.pe_quiet(fn)
                        S.op("dve", lambda e, dstT=dstT, c0=c0, nb=nb: e.tensor_copy(
                            out=dstT[:, c0 * 128:(c0 + nb) * 128], in_=ptq0[:, 0:nb * 128]), [r_q0], [r_dst])
                def row_info(r):
                    is_ctx = r >= NROWS
                    if not is_ctx:
                        rs = min(max(r - 4, 0), NROWS - 8)
                        return dict(is_ctx=False, P=64, rs=rs, j0=rs - r + 7, q0=r * 64, nk=768)
                    return dict(is_ctx=True, P=128, rs=0, j0=0, q0=N + (r - NROWS) * 128, nk=256)

                def emit_S(r, k):
                    ri = row_info(r)
                    q0 = ri["q0"]
                    if not ri["is_ctx"]:
                        rs, j0 = ri["rs"], ri["j0"]
                        S.op("pe", lambda e, k=k, q0=q0, rs=rs: e.matmul(ps_a[k][0:64, 0:512], QT[:, q0:q0 + 64],
                                                                       KT[:, rs * 64:rs * 64 + 512], start=True, stop=True),
                             [r_QT, r_KT], [r_pa[k]])
                        S.op("pe", lambda e, k=k, q0=q0: e.matmul(ps_b[k][0:64, 0:256], QT[:, q0:q0 + 64], KT[:, N:NT],
                                                                start=True, stop=True), [r_QT, r_KT], [r_pb[k]])
                    else:
                        S.op("pe", lambda e, k=k, q0=q0: e.matmul(ps_a[k][:, 0:256], QT[:, q0:q0 + 128], KT[:, N:NT],
                                                                start=True, stop=True), [r_QT, r_KT], [r_pa[k]])

                def emit_softmax(r, k):
                    ri = row_info(r)
                    P_, nk = ri["P"], ri["nk"]
                    if not ri["is_ctx"]:
                        j0 = ri["j0"]
                        S.op("dve", lambda e, k=k, hb=hb, j0=j0: e.scalar_tensor_tensor(
                            out=sbs[k][0:64, 0:512], in0=ps_a[k][0:64, 0:512], scalar=SCALE,
                            in1=bias[hb][:, j0 * 64:j0 * 64 + 512], op0=ALU.mult, op1=ALU.add),
                            [r_pa[k], r_bias[hb]], [r_s[k]])
                        S.op("act", lambda e, k=k: e.activation(out=sbs[k][0:64, 512:768], in_=ps_b[k][0:64, 0:256],
                                                                func=AF.Copy, scale=SCALE), [r_pb[k]], [r_s[k]])
                    else:
                        S.op("act", lambda e, k=k: e.activation(out=sbs[k][:, 0:256], in_=ps_a[k][:, 0:256],
                                                                func=AF.Copy, scale=SCALE), [r_pa[k]], [r_s[k]])
                    S.op("pool", lambda e, k=k, P_=P_: e.memset(stt2[k][0:P_, 0:1], 0.0), [], [r_stt2[k]])
                    S.op("act", lambda e, k=k, P_=P_, nk=nk: e.activation(
                        out=pbf[k][0:P_, 0:nk], in_=sbs[k][0:P_, 0:nk], func=AF.Exp,
                        accum_out=stt2[k][0:P_, 0:1]), [r_s[k], r_stt2[k]], [r_p[k], r_stt2[k]])
                    S.op("dve", lambda e, k=k, P_=P_: e.reciprocal(out=stt2[k][0:P_, 1:2], in_=stt2[k][0:P_, 0:1]),
                         [r_stt2[k]], [r_stt2[k]])

                def emit_PV(r, k):
                    ri = row_info(r)
                    P_, nk, rs, q0, is_ctx = ri["P"], ri["nk"], ri["rs"], ri["q0"], ri["is_ctx"]
                    nch = nk // 128
                    for c in range(nch):
                        fn = lambda e, k=k, c=c, P_=P_: e.transpose(ptq1[:, c * P_:(c + 1) * P_],
                                                                    pbf[k][0:P_, c * 128:(c + 1) * 128], ident[0:P_, 0:P_])
                        if c == 0 or c == nch - 1:
                            S.op("pe", fn, [r_p[k], r_id], [r_q1a])
                        else:
                            S.pe_quiet(fn)
                    S.op("act", lambda e, k=k, w_=nch * P_: e.activation(out=pT[k][:, 0:w_], in_=ptq1[:, 0:w_], func=AF.Copy),
                         [r_q1a], [r_pT[k]])
                    pairs = []
                    for c in range(nch):
                        if is_ctx:
                            vch = Vc[hb][:, c, :]
                        elif c < 4:
                            vch = Va[hb][:, rs // 2 + c, :] if rs % 2 == 0 else Vb[hb][:, (rs - 1) // 2 + c, :]
                        else:
                            vch = Vc[hb][:, c - 4, :]
                        pairs.append((pT[k][:, c * P_:(c + 1) * P_], vch))
                    mm_group(ps_o[k][0:P_, 0:128], pairs, [r_pT[k], r_V[hb]], r_po[k])
                    S.op("act", lambda e, k=k, P_=P_: e.activation(out=obf[k][0:P_, :], in_=ps_o[k][0:P_, 0:128], func=AF.Copy,
                                                                   scale=stt2[k][0:P_, 1:2]), [r_po[k], r_stt2[k]], [r_o[k]])
                    S.op("pe", lambda e, k=k, P_=P_: e.transpose(ptq1[:, 512:512 + P_], obf[k][0:P_, :], ident[0:P_, 0:P_]),
                         [r_o[k], r_id], [r_q1b])
                    S.op("dve", lambda e, q0=q0, P_=P_: e.tensor_copy(out=oTh[:, q0:q0 + P_], in_=ptq1[:, 512:512 + P_]),
                         [r_q1b], [r_oTh])

                NR = NROWS + 2
                emit_S(0, wi % 2)
                for r in range(NR):
                    k = wi % 2
                    emit_softmax(r, k)
                    if r + 1 < NR:
                        emit_S(r + 1, (wi + 1) % 2)
                    emit_PV(r, k)
                    wi += 1
                S.dma("sp", lambda e, hh=hh: e.dma_start(out=oT_d[:, hh, :], in_=oTh[:, :]), [r_oTh], [])
        return phase_end("N")

    def phase_T2a(layer):
        W = wo[layer]
        with ExitStack() as ps:
            sb = lambda n, s, d: ps.enter_context(nc.sbuf_tensor(U(n), s, d))
            wob = [sb("t2_w%d" % i, [128, KC, 512], BF16) for i in range(2)]
            oT4 = [sb("t2_oT%d" % i, [128, KC, 512], BF16) for i in range(2)]
            xb = [sb("t2_x%d" % i, [128, 512], F32) for i in range(2)]
            yb = [sb("t2_y%d" % i, [128, 512], F32) for i in range(2)]
            g2 = [sb("t2_g%d" % i, [128, D], F32) for i in range(2)]
            pacc = [ps.enter_context(nc.psum_tensor(U("t2_p%d" % i), [128, 512], F32)) for i in range(4)]
            r_w = R("w0", "w1"); r_oT = R("o0", "o1"); r_x = R("x0", "x1"); r_y = R("y0", "y1"); r_g = R("g0", "g1")
            r_p = R("p0", "p1", "p2", "p3")
            for row in range(2):
                load_rows_bcast("sp", g2[row][:, :], mod_d[layer, row:row + 1, 2 * D:3 * D], r_g[row])
            oi = 0; xi = 0; pi = 0
            for nb in range(8):
                wk = nb % 2
                S.dma("pool", lambda e, wk=wk, nb=nb: e.dma_start(
                    out=wob[wk][:, :, :], in_=W[:, nb * 512:(nb + 1) * 512].rearrange("(kc p) n -> p kc n", p=128)),
                    [], [r_w[wk]])
                NST = 17 if layer == 0 else 16

                def load_oT(st_, k_):
                    nt_ = 4 if st_ < 16 else 2
                    S.dma("sp", lambda e, k_=k_, st_=st_, nt_=nt_: e.dma_start(
                        out=oT4[k_][:, :, 0:nt_ * 128], in_=oT_d[:, :, st_ * 512:st_ * 512 + nt_ * 128]), [], [r_oT[k_]])
                if nb == 0:
                    load_oT(0, oi % 2)
                for st in range(NST):
                    ntile = 4 if st < 16 else 2
                    row = 0 if st < 16 else 1
                    k = oi % 2; oi += 1
                    if st + 1 < NST:
                        load_oT(st + 1, oi % 2)
                    elif nb + 1 < 8:
                        load_oT(0, oi % 2)
                    for ts in range(ntile):
                        t0 = st * 512 + ts * 128
                        j = xi % 2; xi += 1
                        p = pi % 4; pi += 1
                        S.dma("sp", lambda e, j=j, t0=t0, nb=nb: e.dma_start(
                            out=xb[j][:, :], in_=src_rows(layer, t0, 128)[:, nb * 512:(nb + 1) * 512]), [], [r_x[j]])
                        mm_group(pacc[p][:, :], [(oT4[k][:, kc, ts * 128:(ts + 1) * 128], wob[wk][:, kc, :]) for kc in range(KC)],
                                 [r_oT[k], r_w[wk]], r_p[p])
                        S.op("dve", lambda e, j=j, p=p, row=row, nb=nb: e.tensor_tensor(
                            out=yb[j][:, :], in0=pacc[p][:, :], in1=g2[row][:, nb * 512:(nb + 1) * 512], op=ALU.mult),
                            [r_p[p], r_g[row]], [r_y[j]])
                        S.op("pool", lambda e, j=j: e.tensor_tensor(out=yb[j][:, :], in0=yb[j][:, :], in1=xb[j][:, :], op=ALU.add),
                             [r_y[j], r_x[j]], [r_y[j]])
                        S.dma("pool", lambda e, j=j, t0=t0, nb=nb: e.dma_start(
                            out=x1_d[t0:t0 + 128, nb * 512:(nb + 1) * 512], in_=yb[j][:, :]), [r_y[j]], [])
        return phase_end("T2a_%d" % layer)

    def phase_T2b(layer):
        with ExitStack() as ps:
            sb = lambda n, s, d: ps.enter_context(nc.sbuf_tensor(U(n), s, d))
            xt = [sb("tb_x%d" % i, [128, D], F32) for i in range(2)]
            hb = [sb("tb_h%d" % i, [128, D], BF16) for i in range(2)]
            Arow = sb("tb_A", [128, D], F32)
            Brow = sb("tb_B", [128, D], F32)
            tmp = sb("tb_tmp", [128, D], F32)
            st = [sb("tb_st%d" % i, [128, 8], F32) for i in range(2)]
            hfT = sb("tb_hfT", [128, KC, 128], F32)
            identf = sb("tb_id", [128, 128], F32)
            wrb = sb("tb_wr", [128, KC, NE], F32)
            lg = [sb("tb_lg%d" % i, [128, NE], F32) for i in range(2)]
            ptr = [ps.enter_context(nc.psum_tensor(U("tb_p%d" % i), [128, 512], F32)) for i in range(4)]
            pl = ps.enter_context(nc.psum_tensor(U("tb_pl"), [128, 512], F32))
            r_x = R("x0", "x1"); r_h = R("h0", "h1"); r_st = R("st0", "st1"); r_lg = R("lg0", "lg1")
            r_A, r_B, r_tmp, r_id, r_wr, r_hfT, r_pl = R("A", "B", "tmp", "id", "wr", "hfT", "pl")
            r_p = R("p0", "p1", "p2", "p3")
            S.dma("sp", lambda e: e.dma_start(out=identf[:, :], in_=ident_in[:, :]), [], [r_id])
            S.dma("sp", lambda e: e.dma_start(out=wrb[:, :, :], in_=wr[layer, :, :].rearrange("(kc p) n -> p kc n", p=128)),
                  [], [r_wr])
            it = 0
            for (tok0, ntile, row) in (((0, NTL, 0), (N, NTC, 1)) if layer == 0 else ((0, NTL, 0),)):
                load_rows_bcast("sp", Arow[:, :], mod_d[layer, row:row + 1, 4 * D:5 * D], r_A)
                load_rows_bcast("sp", tmp[:, :], n2g[layer:layer + 1, :], r_tmp)
                load_rows_bcast("sp", Brow[:, :], mod_d[layer, row:row + 1, 3 * D:4 * D], r_B)
                S.op("dve", lambda e: e.scalar_tensor_tensor(out=Arow[:, :], in0=Arow[:, :], scalar=1.0, in1=tmp[:, :],
                                                             op0=ALU.add, op1=ALU.mult), [r_A, r_tmp], [r_A])
                for tt in range(ntile):
                    k = it % 2; it += 1
                    t0 = tok0 + tt * 128
                    S.dma("sp", lambda e, k=k, t0=t0: e.dma_start(out=xt[k][:, :], in_=x1_d[t0:t0 + 128, :]), [], [r_x[k]])
                    S.op("dve", lambda e, k=k: e.memset(st[k][:, 0:1], 0.0), [], [r_st[k]])
                    S.op("act", lambda e, k=k: e.activation(out=hb[k][:, :], in_=xt[k][:, :], func=AF.Square,
                                                            accum_out=st[k][:, 0:1]), [r_x[k], r_st[k]], [r_h[k], r_st[k]])
                    S.op("dve", lambda e, k=k: e.tensor_scalar(out=st[k][:, 1:2], in0=st[k][:, 0:1], scalar1=1.0 / D,
                                                               scalar2=EPS, op0=ALU.mult, op1=ALU.add), [r_st[k]], [r_st[k]])
                    S.op("act", lambda e, k=k: e.activation(out=st[k][:, 1:2], in_=st[k][:, 1:2], func=AF.Sqrt),
                         [r_st[k]], [r_st[k]])
                    S.op("dve", lambda e, k=k: e.reciprocal(out=st[k][:, 1:2], in_=st[k][:, 1:2]), [r_st[k]], [r_st[k]])
                    S.op("dve", lambda e, k=k: e.scalar_tensor_tensor(out=xt[k][:, :], in0=xt[k][:, :], scalar=st[k][:, 1:2],
                                                                      in1=Arow[:, :], op0=ALU.mult, op1=ALU.mult),
                         [r_x[k], r_st[k], r_A], [r_x[k]])
                    S.op("pool", lambda e, k=k: e.tensor_tensor(out=xt[k][:, :], in0=xt[k][:, :], in1=Brow[:, :], op=ALU.add),
                         [r_x[k], r_B], [r_x[k]])
                    S.op("act", lambda e, k=k: e.activation(out=hb[k][:, :], in_=xt[k][:, :], func=AF.Copy), [r_x[k]], [r_h[k]])
                    S.dma("pool", lambda e, k=k, t0=t0: e.dma_start(out=h2_d[t0:t0 + 128, :], in_=hb[k][:, :]), [r_h[k]], [])
                    for g in range(8):
                        pg = g % 4
                        for j in range(4):
                            kc = g * 4 + j
                            fn = lambda e, k=k, pg=pg, j=j, kc=kc: e.transpose(ptr[pg][:, j * 128:(j + 1) * 128],
                                                                               xt[k][:, kc * 128:(kc + 1) * 128], identf[:, :])
                            if j == 0 or j == 3:
                                S.op("pe", fn, [r_x[k], r_id], [r_p[pg]])
                            else:
                                S.pe_quiet(fn)
                        if g % 2 == 0:
                            S.op("act", lambda e, g=g, pg=pg: e.activation(
                                out=hfT[:, g * 4:(g + 1) * 4, :], in_=ptr[pg][:, :].rearrange("p (j t) -> p j t", j=4),
                                func=AF.Copy), [r_p[pg]], [r_hfT])
                        else:
                            S.op("dve", lambda e, g=g, pg=pg: e.tensor_copy(
                                out=hfT[:, g * 4:(g + 1) * 4, :], in_=ptr[pg][:, :].rearrange("p (j t) -> p j t", j=4)),
                                [r_p[pg]], [r_hfT])
                    mm_group(pl[:, 0:NE], [(hfT[:, kc, :], wrb[:, kc, :]) for kc in range(KC)], [r_hfT, r_wr], r_pl)
                    S.op("dve", lambda e, k=k: e.tensor_reduce(out=st[k][:, 2:3], in_=pl[:, 0:NE], axis=AX.X, op=ALU.max),
                         [r_pl], [r_st[k]])
                    S.op("dve", lambda e, k=k: e.tensor_scalar(out=st[k][:, 3:4], in0=st[k][:, 2:3], scalar1=-1.0, scalar2=None,
                                                               op0=ALU.mult), [r_st[k]], [r_st[k]])
                    S.op("dve", lambda e, k=k: e.memset(st[k][:, 4:5], 0.0), [], [r_st[k]])
                    S.op("act", lambda e, k=k: e.activation(out=lg[k][:, :], in_=pl[:, 0:NE], func=AF.Exp, bias=st[k][:, 3:4],
                                                            scale=1.0, accum_out=st[k][:, 4:5]), [r_pl, r_st[k]], [r_lg[k], r_st[k]])
                    S.op("dve", lambda e, k=k: e.reciprocal(out=st[k][:, 5:6], in_=st[k][:, 4:5]), [r_st[k]], [r_st[k]])
                    S.op("dve", lambda e, k=k: e.tensor_scalar(out=lg[k][:, :], in0=lg[k][:, :], scalar1=st[k][:, 5:6],
                                                               scalar2=None, op0=ALU.mult), [r_lg[k], r_st[k]], [r_lg[k]])
                    S.dma("pool", lambda e, k=k, t0=t0: e.dma_start(out=aff_d[t0:t0 + 128, :], in_=lg[k][:, :]), [r_lg[k]], [])
        return phase_end("T2b_%d" % layer)

    def phase_DA():
        import math
        LAM_INIT = 0.8 - 0.6 * math.exp(-0.3 * 1)
        with ExitStack() as ps:
            sb = lambda n, s, d: ps.enter_context(nc.sbuf_tensor(U(n), s, d))
            tm = sb("da_tm", [128, 66, 256], BF16)
            QT = sb("da_QT", [128, 2, N], BF16)
            KT = sb("da_KT", [128, 2, NT], BF16)
            Vaug = sb("da_V", [128, 66, 257], BF16)
            PT = [sb("da_PT%d" % i, [128, 512], BF16) for i in range(3)]
            ident = sb("da_id", [128, 128], BF16)
            lrow = sb("da_lrow", [128, 4, 128], F32)
            ltmp = sb("da_ltmp", [128, 128], F32)
            lamt = sb("da_lam", [128, 8], F32)
            gsub = sb("da_gsub", [128, 256], F32)
            o32 = [sb("da_o32%d" % i, [128, 256], F32) for i in range(2)]
            obf = [sb("da_obf%d" % i, [128, 256], BF16) for i in range(2)]
            osb = [sb("da_osb%d" % i, [128, 2, 128], BF16) for i in range(2)]
            stt = [sb("da_stt%d" % i, [128, 8], F32) for i in range(2)]
            junk = sb("da_junk", [128, 256], BF16)
            ptq = ps.enter_context(nc.psum_tensor(U("da_ptq"), [128, 1024], BF16))
            pso = ptq
            ps_s = [ps.enter_context(nc.psum_tensor(U("da_ps%d" % i), [128, 512], F32)) for i in range(3)]
            po = [ps.enter_context(nc.psum_tensor(U("da_po%d" % i), [128, 512], F32)) for i in range(4)]
            r_tm, r_QT, r_KT, r_V, r_id, r_lrow, r_ltmp, r_lam, r_gsub, r_junk, r_ptq, r_pso = R(
                "tm", "QT", "KT", "V", "id", "lrow", "ltmp", "lam", "gsub", "junk", "ptq", "pso")
            r_PT = R("PT0", "PT1", "PT2"); r_o32 = R("o0", "o1"); r_obf = R("ob0", "ob1"); r_osb = R("os0", "os1")
            r_stt = R("st0", "st1"); r_ps = R("ps0", "ps1", "ps2"); r_po = R("po0", "po1", "po2", "po3")
            r_pso = r_ptq
            S.dma("pool", lambda e: e.dma_start(out=ident[:, :], in_=ident_in[:, :]), [], [r_id])
            for i in range(4):
                S.dma("sp", lambda e, i=i: e.dma_start(out=lrow[:, i, :], in_=lamp[i:i + 1, :].partition_broadcast(128)),
                      [], [r_lrow])
            for j in range(2):
                S.op("dve", lambda e, j=j: e.tensor_tensor(out=ltmp[:, :], in0=lrow[:, 2 * j, :], in1=lrow[:, 2 * j + 1, :],
                                                           op=ALU.mult), [r_lrow], [r_ltmp])
                S.op("dve", lambda e, j=j: e.tensor_reduce(out=lamt[:, j:j + 1], in_=ltmp[:, :], axis=AX.X, op=ALU.add),
                     [r_ltmp], [r_lam])
            S.op("act", lambda e: e.activation(out=lamt[:, 2:4], in_=lamt[:, 0:2], func=AF.Exp), [r_lam], [r_lam])
            S.op("dve", lambda e: e.tensor_tensor(out=lamt[:, 4:5], in0=lamt[:, 3:4], in1=lamt[:, 2:3], op=ALU.subtract),
                 [r_lam], [r_lam])
            S.op("dve", lambda e: e.tensor_scalar(out=lamt[:, 5:6], in0=lamt[:, 4:5], scalar1=-LAM_INIT, scalar2=None, op0=ALU.add),
                 [r_lam], [r_lam])
            load_rows_bcast("sp", gsub[:, :], sublng[0:1, :], r_gsub)
            S.op("dve", lambda e: e.tensor_scalar(out=gsub[:, :], in0=gsub[:, :], scalar1=1.0 - LAM_INIT, scalar2=None, op0=ALU.mult),
                 [r_gsub], [r_gsub])
            S.op("pool", lambda e: e.memset(Vaug[:, :, 256:257], 1.0), [], [r_V])
            step = 0; ei = 0
            for h in range(16):
                for (c_off, nchunk, dstT, r_dst) in ((h * 256, NTL, QT, r_QT), (D + h * 256, NTL + NTC, KT, r_KT)):
                    S.dma("sp", lambda e, c_off=c_off, nchunk=nchunk: e.dma_start(
                        out=tm[:, 0:nchunk, :],
                        in_=qkv_d[0:nchunk * 128, c_off:c_off + 256].rearrange("(c p) d -> p c d", p=128)), [], [r_tm])
                    for comp in range(2):
                        for c0 in range(0, nchunk, 8):
                            nb = min(8, nchunk - c0)
                            for j in range(nb):
                                fn = lambda e, c=c0 + j, j=j, comp=comp: e.transpose(
                                    ptq[:, j * 128:(j + 1) * 128], tm[:, c, comp * 128:(comp + 1) * 128], ident[:, :])
                                if j == 0 or j == nb - 1:
                                    S.op("pe", fn, [r_tm, r_id], [r_ptq])
                                else:
                                    S.pe_quiet(fn)
                            if (c0 // 8) % 2 == 0:
                                S.op("dve", lambda e, dstT=dstT, comp=comp, c0=c0, nb=nb: e.tensor_copy(
                                    out=dstT[:, comp, c0 * 128:(c0 + nb) * 128], in_=ptq[:, 0:nb * 128]), [r_ptq], [r_dst])
                            else:
                                S.op("act", lambda e, dstT=dstT, comp=comp, c0=c0, nb=nb: e.activation(
                                    out=dstT[:, comp, c0 * 128:(c0 + nb) * 128], in_=ptq[:, 0:nb * 128], func=AF.Copy),
                                    [r_ptq], [r_dst])
                vcol = 2 * D + h * 256
                S.dma("sp", lambda e, vcol=vcol: e.dma_start(
                    out=Vaug[:, :, 0:256], in_=qkv_d[:, vcol:vcol + 256].rearrange("(c p) d -> p c d", p=128)), [], [r_V])
                NKC = NTL + NTC

                def emit_scores(qb_, kc_, gi_):
                    sk_ = gi_ % 3
                    for comp in range(2):
                        S.op("pe", lambda e, sk_=sk_, comp=comp, kc_=kc_, q0_=qb_ * 256: e.matmul(
                            ps_s[sk_][:, comp * 256:(comp + 1) * 256], KT[:, comp, kc_ * 128:(kc_ + 1) * 128],
                            QT[:, comp, q0_:q0_ + 256], start=True, stop=True), [r_KT, r_QT], [r_ps[sk_]])

                def emit_exp_pv(kc_, gi_):
                    sk_ = gi_ % 3
                    S.op("act", lambda e, sk_=sk_: e.activation(out=PT[sk_][:, :], in_=ps_s[sk_][:, :], func=AF.Exp, scale=SCALE),
                         [r_ps[sk_]], [r_PT[sk_]])
                    for comp in range(2):
                        for qs in range(2):
                            pi = comp * 2 + qs
                            S.op("pe", lambda e, sk_=sk_, comp=comp, qs=qs, pi=pi, kc_=kc_: e.matmul(
                                po[pi][:, 0:257], PT[sk_][:, comp * 256 + qs * 128:comp * 256 + (qs + 1) * 128],
                                Vaug[:, kc_, :], start=(kc_ == 0), stop=(kc_ == NKC - 1)), [r_PT[sk_], r_V], [r_po[pi]])

                NQB = N // 256
                flat = [(qb_, kc_) for qb_ in range(NQB) for kc_ in range(NKC)]
                emit_scores(flat[0][0], flat[0][1], step)
                emit_scores(flat[1][0], flat[1][1], step + 1)
                for fi, (qb, kc) in enumerate(flat):
                    q0 = qb * 256
                    if fi + 2 < len(flat):
                        emit_scores(flat[fi + 2][0], flat[fi + 2][1], step + 2)
                    emit_exp_pv(kc, step)
                    step += 1
                    if kc != NKC - 1:
                        continue
                    for qs in range(2):
                        k2 = ei % 2; ei += 1
                        S.op("dve", lambda e, k2=k2, qs=qs: e.reciprocal(out=stt[k2][:, 0:1], in_=po[qs][:, 256:257]),
                             [r_po[qs]], [r_stt[k2]])
                        S.op("dve", lambda e, k2=k2, qs=qs: e.reciprocal(out=stt[k2][:, 1:2], in_=po[2 + qs][:, 256:257]),
                             [r_po[2 + qs]], [r_stt[k2]])
                        S.op("dve", lambda e, k2=k2: e.tensor_tensor(out=stt[k2][:, 1:2], in0=stt[k2][:, 1:2], in1=lamt[:, 5:6],
                                                                     op=ALU.mult), [r_stt[k2], r_lam], [r_stt[k2]])
                        S.op("dve", lambda e, k2=k2, qs=qs: e.tensor_scalar(out=o32[k2][:, :], in0=po[qs][:, 0:256],
                                                                            scalar1=stt[k2][:, 0:1], scalar2=None, op0=ALU.mult),
                             [r_po[qs], r_stt[k2]], [r_o32[k2]])
                        S.op("dve", lambda e, k2=k2, qs=qs: e.scalar_tensor_tensor(
                            out=o32[k2][:, :], in0=po[2 + qs][:, 0:256], scalar=stt[k2][:, 1:2], in1=o32[k2][:, :],
                            op0=ALU.mult, op1=ALU.add), [r_po[2 + qs], r_stt[k2], r_o32[k2]], [r_o32[k2]])
                        S.op("dve", lambda e, k2=k2: e.memset(stt[k2][:, 2:3], 0.0), [], [r_stt[k2]])
                        S.op("act", lambda e, k2=k2: e.activation(out=junk[:, :], in_=o32[k2][:, :], func=AF.Square,
                                                                  accum_out=stt[k2][:, 2:3]), [r_o32[k2], r_stt[k2]],
                             [r_junk, r_stt[k2]])
                        S.op("dve", lambda e, k2=k2: e.tensor_scalar(out=stt[k2][:, 3:4], in0=stt[k2][:, 2:3], scalar1=1.0 / 256,
                                                                     scalar2=SUBLN_EPS, op0=ALU.mult, op1=ALU.add),
                             [r_stt[k2]], [r_stt[k2]])
                        S.op("act", lambda e, k2=k2: e.activation(out=stt[k2][:, 3:4], in_=stt[k2][:, 3:4], func=AF.Sqrt),
                             [r_stt[k2]], [r_stt[k2]])
                        S.op("dve", lambda e, k2=k2: e.reciprocal(out=stt[k2][:, 3:4], in_=stt[k2][:, 3:4]), [r_stt[k2]], [r_stt[k2]])
                        S.op("dve", lambda e, k2=k2: e.scalar_tensor_tensor(out=obf[k2][:, :], in0=o32[k2][:, :],
                                                                            scalar=stt[k2][:, 3:4], in1=gsub[:, :],
                                                                            op0=ALU.mult, op1=ALU.mult),
                             [r_o32[k2], r_stt[k2], r_gsub], [r_obf[k2]])
                        for j in range(2):
                            S.op("pe", lambda e, k2=k2, j=j: e.transpose(pso[:, j * 128:(j + 1) * 128],
                                                                         obf[k2][:, j * 128:(j + 1) * 128], ident[:, :]),
                                 [r_obf[k2], r_id], [r_pso])
                        S.op("dve", lambda e, k2=k2: e.tensor_copy(out=osb[k2][:, :, :],
                                                                   in_=pso[:, 0:256].rearrange("p (j t) -> p j t", j=2)),
                             [r_pso], [r_osb[k2]])
                        t0 = q0 + qs * 128
                        S.dma("sp", lambda e, k2=k2, h=h, t0=t0: e.dma_start(out=oT_d[:, 2 * h:2 * h + 2, t0:t0 + 128],
                                                                             in_=osb[k2][:, :, :]), [r_osb[k2]], [])
        return phase_end("DA")


    offs_l = es.enter_context(nc.sbuf_tensor("offs_l", [128, NTL, NE], I32))
    offs_c = es.enter_context(nc.sbuf_tensor("offs_c", [128, NTC, NE], I32))
    gsel_l = es.enter_context(nc.sbuf_tensor("gsel_l", [128, NTL, NE], F32))
    gsel_c = es.enter_context(nc.sbuf_tensor("gsel_c", [128, NTC, NE], F32))
    r_offs, r_gsel = R("offs", "gsel")
    NROW_XG = NE * SLOTS

    def phase_E1(layer):
        sets = [(0, NTL, CAP, 0, offs_l, gsel_l)]
        if layer == 0:
            sets.append((N, NTC, CAPC, CAP, offs_c, gsel_c))
        with ExitStack() as ps:
            sb = lambda n, s, d: ps.enter_context(nc.sbuf_tensor(U(n), s, d))
            A = sb("e1_A", [128, NTL, NE], F32)
            cmp = sb("e1_cmp", [128, NTL, NE], F32)
            M = sb("e1_M", [128, NTL, NE], F32)
            s0 = sb("e1_s0", [128, NTL, NE], F32)
            s1 = sb("e1_s1", [128, NTL, NE], F32)
            cc = sb("e1_cc", [128, NTL, NE], F32)
            sm = {n: sb("e1_" + n, [128, NE], F32) for n in ("lo", "hi", "mid", "d1", "d2", "pred", "cnt", "erow")}
            ones = sb("e1_ones", [128, 128], F32)
            lst = sb("e1_lst", [128, 128], F32)
            ptot = ps.enter_context(nc.psum_tensor(U("e1_pt"), [128, 512], F32))
            pcc = [ps.enter_context(nc.psum_tensor(U("e1_pc%d" % i), [128, 512], F32)) for i in range(2)]
            pwi = [ps.enter_context(nc.psum_tensor(U("e1_pw%d" % i), [128, 512], F32)) for i in range(2)]
            r_A, r_cmp, r_M, r_s0, r_s1, r_cc, r_ones, r_lst, r_pt = R("A", "cmp", "M", "s0", "s1", "cc", "ones", "lst", "pt")
            r_pc = R("pc0", "pc1"); r_pw = R("pw0", "pw1")
            rs = {n: Res(n) for n in sm}
            S.op("dve", lambda e: e.memset(ones[:, :], 1.0), [], [r_ones])
            S.dma("sp", lambda e: e.dma_start(out=lst[:, :], in_=lstrict_in[:, :]), [], [r_lst])
            D_ = lambda fn, rd, wr_: S.op("dve", fn, rd, wr_)
            for (tok0, nt, cap, sbase, offs, gsel) in sets:
                Av = A[:, 0:nt, :]
                S.dma("sp", lambda e, Av=Av, tok0=tok0, nt=nt: e.dma_start(
                    out=Av, in_=aff_d[tok0:tok0 + nt * 128, :].rearrange("(c p) e -> p c e", p=128)), [], [r_A])
                D_(lambda e: e.memset(sm["lo"][:, :], 0.0), [], [rs["lo"]])
                D_(lambda e: e.memset(sm["hi"][:, :], 2.0), [], [rs["hi"]])
                for e_ in range(NE):
                    D_(lambda e, e_=e_, sbase=sbase: e.memset(sm["erow"][:, e_:e_ + 1], float(e_ * SLOTS + sbase)), [], [rs["erow"]])
                bc = lambda t, nt=nt: t[:, :].unsqueeze(1).to_broadcast([128, nt, NE])
                for itr in range(36):
                    D_(lambda e: e.tensor_tensor(out=sm["mid"][:, :], in0=sm["lo"][:, :], in1=sm["hi"][:, :], op=ALU.add),
                       [rs["lo"], rs["hi"]], [rs["mid"]])
                    D_(lambda e: e.tensor_scalar(out=sm["mid"][:, :], in0=sm["mid"][:, :], scalar1=0.5, scalar2=None, op0=ALU.mult),
                       [rs["mid"]], [rs["mid"]])
                    D_(lambda e, Av=Av, nt=nt, bc=bc: e.tensor_tensor(out=cmp[:, 0:nt, :], in0=Av, in1=bc(sm["mid"]), op=ALU.is_ge),
                       [r_A, rs["mid"]], [r_cmp])
                    D_(lambda e, nt=nt: e.tensor_reduce(out=sm["cnt"][:, :], in_=cmp[:, 0:nt, :].rearrange("p c e -> p e c"),
                                                        axis=AX.X, op=ALU.add), [r_cmp], [rs["cnt"]])
                    S.op("pe", lambda e: e.matmul(ptot[:, 0:NE], ones[:, :], sm["cnt"][:, :], start=True, stop=True),
                         [r_ones, rs["cnt"]], [r_pt])
                    D_(lambda e, cap=cap: e.tensor_scalar(out=sm["pred"][:, :], in0=ptot[:, 0:NE], scalar1=float(cap) - 0.5,
                                                          scalar2=None, op0=ALU.is_ge), [r_pt], [rs["pred"]])
                    D_(lambda e: e.tensor_tensor(out=sm["d1"][:, :], in0=sm["mid"][:, :], in1=sm["lo"][:, :], op=ALU.subtract),
                       [rs["mid"], rs["lo"]], [rs["d1"]])
                    D_(lambda e: e.tensor_tensor(out=sm["d1"][:, :], in0=sm["d1"][:, :], in1=sm["pred"][:, :], op=ALU.mult),
                       [rs["d1"], rs["pred"]], [rs["d1"]])
                    D_(lambda e: e.tensor_tensor(out=sm["d2"][:, :], in0=sm["hi"][:, :], in1=sm["mid"][:, :], op=ALU.subtract),
                       [rs["mid"], rs["hi"]], [rs["d2"]])
                    D_(lambda e: e.tensor_tensor(out=sm["d2"][:, :], in0=sm["d2"][:, :], in1=sm["pred"][:, :], op=ALU.mult),
                       [rs["d2"], rs["pred"]], [rs["d2"]])
                    D_(lambda e: e.tensor_tensor(out=sm["lo"][:, :], in0=sm["lo"][:, :], in1=sm["d1"][:, :], op=ALU.add),
                       [rs["lo"], rs["d1"]], [rs["lo"]])
                    D_(lambda e: e.tensor_tensor(out=sm["hi"][:, :], in0=sm["mid"][:, :], in1=sm["d2"][:, :], op=ALU.add),
                       [rs["mid"], rs["d2"]], [rs["hi"]])
                D_(lambda e, Av=Av, nt=nt, bc=bc: e.tensor_tensor(out=M[:, 0:nt, :], in0=Av, in1=bc(sm["lo"]), op=ALU.is_ge),
                   [r_A, rs["lo"]], [r_M])
                ncol = nt * NE
                Mf = M[:, :, :].rearrange("p c e -> p (c e)")
                nh = (ncol + 511) // 512
                for h in range(nh):
                    w_ = min(512, ncol - h * 512)
                    S.op("pe", lambda e, h=h, w_=w_: e.matmul(pcc[h][:, 0:w_], ones[:, :], Mf[:, h * 512:h * 512 + w_],
                                                             start=True, stop=True), [r_ones, r_M], [r_pc[h]])
                    S.op("pe", lambda e, h=h, w_=w_: e.matmul(pwi[h][:, 0:w_], lst[:, :], Mf[:, h * 512:h * 512 + w_],
                                                             start=True, stop=True), [r_lst, r_M], [r_pw[h]])
                    D_(lambda e, h=h, w_=w_: e.tensor_copy(out=cc[:, :, :].rearrange("p c e -> p (c e)")[:, h * 512:h * 512 + w_],
                                                          in_=pcc[h][:, 0:w_]), [r_pc[h]], [r_cc])
                D_(lambda e, nt=nt: e.tensor_copy(out=s0[:, 0:nt, :], in_=cc[:, 0:nt, :]), [r_cc], [r_s0])
                src, dst, r_src, r_dst = s0, s1, r_s0, r_s1
                d = 1
                while d < nt:
                    D_(lambda e, src=src, dst=dst, d=d, nt=nt: e.tensor_tensor(out=dst[:, d:nt, :], in0=src[:, d:nt, :],
                                                                             in1=src[:, 0:nt - d, :], op=ALU.add), [r_src], [r_dst])
                    D_(lambda e, src=src, dst=dst, d=d: e.tensor_copy(out=dst[:, 0:d, :], in_=src[:, 0:d, :]), [r_src], [r_dst])
                    src, dst, r_src, r_dst = dst, src, r_dst, r_src
                    d *= 2
                D_(lambda e, src=src, nt=nt: e.tensor_tensor(out=src[:, 0:nt, :], in0=src[:, 0:nt, :], in1=cc[:, 0:nt, :],
                                                            op=ALU.subtract), [r_src, r_cc], [r_src])
                srcf = src[:, :, :].rearrange("p c e -> p (c e)")
                for h in range(nh):
                    w_ = min(512, ncol - h * 512)
                    D_(lambda e, h=h, w_=w_, srcf=srcf: e.tensor_tensor(out=srcf[:, h * 512:h * 512 + w_],
                                                                       in0=srcf[:, h * 512:h * 512 + w_], in1=pwi[h][:, 0:w_],
                                                                       op=ALU.add), [r_src, r_pw[h]], [r_src])
                D_(lambda e, src=src, nt=nt, cap=cap: e.tensor_scalar(out=cmp[:, 0:nt, :], in0=src[:, 0:nt, :],
                                                                     scalar1=float(cap) - 0.5, scalar2=None, op0=ALU.is_lt),
                   [r_src], [r_cmp])
                D_(lambda e, nt=nt: e.tensor_tensor(out=M[:, 0:nt, :], in0=M[:, 0:nt, :], in1=cmp[:, 0:nt, :], op=ALU.mult),
                   [r_M, r_cmp], [r_M])
                D_(lambda e, gsel=gsel, Av=Av, nt=nt: e.tensor_tensor(out=gsel[:, :, :], in0=Av, in1=M[:, 0:nt, :], op=ALU.mult),
                   [r_A, r_M], [r_gsel])
                D_(lambda e, src=src, nt=nt, bc=bc: e.tensor_tensor(out=src[:, 0:nt, :], in0=src[:, 0:nt, :], in1=bc(sm["erow"]),
                                                                   op=ALU.add), [r_src, rs["erow"]], [r_src])
                D_(lambda e, src=src, nt=nt: e.tensor_scalar(out=src[:, 0:nt, :], in0=src[:, 0:nt, :], scalar1=-BIG, scalar2=None,
                                                            op0=ALU.add), [r_src], [r_src])
                D_(lambda e, src=src, nt=nt: e.tensor_tensor(out=src[:, 0:nt, :], in0=src[:, 0:nt, :], in1=M[:, 0:nt, :],
                                                            op=ALU.mult), [r_src, r_M], [r_src])
                D_(lambda e, src=src, nt=nt: e.tensor_scalar(out=src[:, 0:nt, :], in0=src[:, 0:nt, :], scalar1=BIG, scalar2=None,
                                                            op0=ALU.add), [r_src], [r_src])
                D_(lambda e, src=src, nt=nt, offs=offs: e.tensor_copy(out=offs[:, :, :], in_=src[:, 0:nt, :]), [r_src], [r_offs])
        return phase_end("E1_%d" % layer)

    def phase_E2(layer):
        sets = [(0, NTL, offs_l)]
        if layer == 0:
            sets.append((N, NTC, offs_c))
        with ExitStack() as ps:
            sb = lambda n, s, d: ps.enter_context(nc.sbuf_tensor(U(n), s, d))
            h2t = [sb("e2_h%d" % i, [128, D], BF16) for i in range(3)]
            r_h = R("h0", "h1", "h2")
            bcr = {}

            def mkreg(e):
                bcr["r"] = e.alloc_register(U("e2_bc"))
                return e.reg_mov(bcr["r"], NROW_XG - 1)
            S.raw("pool", mkreg)
            it = 0
            for (tok0, nt, offs) in sets:
                for c in range(nt):
                    k = it % 3; it += 1
                    t0 = tok0 + c * 128
                    S.dma("sp", lambda e, k=k, t0=t0: e.dma_start(out=h2t[k][:, :], in_=h2_d[t0:t0 + 128, :]), [], [r_h[k]])
                    for e_ in range(NE):
                        S.dma("pool", lambda e, k=k, c=c, e_=e_, offs=offs: e.indirect_dma_start(
                            out=xg_d[:, :], out_offset=bass.IndirectOffsetOnAxis(ap=offs[:, c, e_:e_ + 1], axis=0),
                            in_=h2t[k][:, :], in_offset=None, bounds_check=bcr["r"], oob_is_err=False),
                            [r_h[k], r_offs], [])
            S.raw("pool", lambda e: (e.free_register(bcr["r"]), None)[1])
        return phase_end("E2_%d" % layer)

    def phase_E3(layer):
        SL = SLOTS if layer == 0 else CAP
        stiles = [(i * 128, 128) for i in range(8)] + ([(CAP, CAPC)] if layer == 0 else [])
        chunks = [(0, 512), (512, 512)] + ([(CAP, CAPC)] if layer == 0 else [])
        with ExitStack() as ps:
            sb = lambda n, s, d: ps.enter_context(nc.sbuf_tensor(U(n), s, d))
            xgt = [sb("e3_x%d" % i, [128, D], BF16) for i in range(2)]
            xgT = sb("e3_xT", [128, KC, SLOTS], BF16)
            wgb = [sb("e3_wg%d" % i, [128, KC, 256], BF16) for i in range(2)]
            wub = [sb("e3_wu%d" % i, [128, KC, 256], BF16) for i in range(2)]
            aT = sb("e3_aT", [128, 8, SLOTS], BF16)
            wdb = [sb("e3_wd%d" % i, [128, 8, 512], BF16) for i in range(2)]
            sg = [sb("e3_sg%d" % i, [128, 512], F32) for i in range(2)]
            ysb = [sb("e3_y%d" % i, [128, 512], BF16) for i in range(2)]
            ident = sb("e3_id", [128, 128], BF16)
            ptr = [ps.enter_context(nc.psum_tensor(U("e3_pt%d" % i), [128, 1024], BF16)) for i in range(2)]
            pg = [ps.enter_context(nc.psum_tensor(U("e3_pg%d" % i), [128, 512], F32)) for i in range(2)]
            pu = [ps.enter_context(nc.psum_tensor(U("e3_pu%d" % i), [128, 512], F32)) for i in range(2)]
            py = [ps.enter_context(nc.psum_tensor(U("e3_py%d" % i), [128, 512], F32)) for i in range(2)]
            r_x = R("x0", "x1"); r_wg = R("wg0", "wg1"); r_wu = R("wu0", "wu1"); r_wd = R("wd0", "wd1")
            r_sg = R("sg0", "sg1"); r_y = R("y0", "y1"); r_pt = R("pt0", "pt1"); r_pg = R("pg0", "pg1")
            r_pu = R("pu0", "pu1"); r_py = R("py0", "py1")
            r_xT, r_aT, r_id = R("xT", "aT", "id")
            S.dma("pool", lambda e: e.dma_start(out=ident[:, :], in_=ident_in[:, :]), [], [r_id])
            xi = 0; ti = 0; wi = 0; gi = 0; di = 0; yi = 0
            for ex in range(NE):
                row0 = ex * SLOTS
                for (s0_, P_) in stiles:
                    k = xi % 2; xi += 1
                    S.dma("sp", lambda e, k=k, row0=row0, s0_=s0_, P_=P_: e.dma_start(
                        out=xgt[k][0:P_, :], in_=xg_d[row0 + s0_:row0 + s0_ + P_, :]), [], [r_x[k]])
                    for g in range(4):
                        tk = ti % 2; ti += 1
                        for j in range(8):
                            kc = g * 8 + j
                            fn = lambda e, k=k, tk=tk, j=j, kc=kc, P_=P_: e.transpose(
                                ptr[tk][:, j * P_:(j + 1) * P_], xgt[k][0:P_, kc * 128:(kc + 1) * 128], ident[0:P_, 0:P_])
                            if j == 0 or j == 7:
                                S.op("pe", fn, [r_x[k], r_id], [r_pt[tk]])
                            else:
                                S.pe_quiet(fn)
                        eng = "act" if g % 2 == 0 else "dve"
                        o_ = xgT[:, g * 8:(g + 1) * 8, s0_:s0_ + P_]
                        i_ = ptr[tk][:, 0:8 * P_].rearrange("p (j t) -> p j t", j=8)
                        if eng == "act":
                            S.op("act", lambda e, o_=o_, i_=i_: e.activation(out=o_, in_=i_, func=AF.Copy), [r_pt[tk]], [r_xT])
                        else:
                            S.op("dve", lambda e, o_=o_, i_=i_: e.tensor_copy(out=o_, in_=i_), [r_pt[tk]], [r_xT])
                for fb in range(4):
                    wk = wi % 2; wi += 1
                    S.dma("pool", lambda e, wk=wk, ex=ex, fb=fb: e.dma_start(
                        out=wgb[wk][:, :, :], in_=wg[layer, ex, :, fb * 256:(fb + 1) * 256].rearrange("(kc p) n -> p kc n", p=128)),
                        [], [r_wg[wk]])
                    S.dma("pool", lambda e, wk=wk, ex=ex, fb=fb: e.dma_start(
                        out=wub[wk][:, :, :], in_=wu[layer, ex, :, fb * 256:(fb + 1) * 256].rearrange("(kc p) n -> p kc n", p=128)),
                        [], [r_wu[wk]])
                    for sub in range(2):
                        f8 = fb * 2 + sub
                        for (c0, cn) in chunks:
                            g_ = gi % 2; gi += 1
                            mm_group(pg[g_][:, 0:cn], [(wgb[wk][:, kc, sub * 128:(sub + 1) * 128], xgT[:, kc, c0:c0 + cn])
                                                       for kc in range(KC)], [r_wg[wk], r_xT], r_pg[g_])
                            mm_group(pu[g_][:, 0:cn], [(wub[wk][:, kc, sub * 128:(sub + 1) * 128], xgT[:, kc, c0:c0 + cn])
                                                       for kc in range(KC)], [r_wu[wk], r_xT], r_pu[g_])
                            S.op("act", lambda e, g_=g_, cn=cn: e.activation(out=sg[g_][:, 0:cn], in_=pg[g_][:, 0:cn], func=AF.Silu),
                                 [r_pg[g_]], [r_sg[g_]])
                            S.op("dve", lambda e, g_=g_, cn=cn, c0=c0, f8=f8: e.tensor_tensor(
                                out=aT[:, f8, c0:c0 + cn], in0=sg[g_][:, 0:cn], in1=pu[g_][:, 0:cn], op=ALU.mult),
                                [r_sg[g_], r_pu[g_]], [r_aT])
                for nb in range(8):
                    dk = di % 2; di += 1
                    S.dma("pool", lambda e, dk=dk, ex=ex, nb=nb: e.dma_start(
                        out=wdb[dk][:, :, :], in_=wd[layer, ex, :, nb * 512:(nb + 1) * 512].rearrange("(fc p) n -> p fc n", p=128)),
                        [], [r_wd[dk]])
                    for (s0_, P_) in stiles:
                        yk = yi % 2; yi += 1
                        mm_group(py[yk][0:P_, :], [(aT[:, fc, s0_:s0_ + P_], wdb[dk][:, fc, :]) for fc in range(8)],
                                 [r_aT, r_wd[dk]], r_py[yk])
                        if yk == 0:
                            S.op("act", lambda e, yk=yk, P_=P_: e.activation(out=ysb[yk][0:P_, :], in_=py[yk][0:P_, :], func=AF.Copy),
                                 [r_py[yk]], [r_y[yk]])
                        else:
                            S.op("dve", lambda e, yk=yk, P_=P_: e.tensor_copy(out=ysb[yk][0:P_, :], in_=py[yk][0:P_, :]),
                                 [r_py[yk]], [r_y[yk]])
                        S.dma("sp", lambda e, yk=yk, row0=row0, s0_=s0_, P_=P_, nb=nb: e.dma_start(
                            out=y_d[row0 + s0_:row0 + s0_ + P_, nb * 512:(nb + 1) * 512], in_=ysb[yk][0:P_, :]), [r_y[yk]], [])
        return phase_end("E3_%d" % layer)

    def phase_T3(layer):
        last = layer == 1
        sets = [(0, NTL, 0, offs_l, gsel_l)]
        if not last:
            sets.append((N, NTC, 1, offs_c, gsel_c))
        with ExitStack() as ps:
            sb = lambda n, s, d: ps.enter_context(nc.sbuf_tensor(U(n), s, d))
            x1t = [sb("t3_x%d" % i, [128, D], F32) for i in range(2)]
            macc = [sb("t3_m%d" % i, [128, D], F32) for i in range(2)]
            G = [sb("t3_g%d" % i, [128, D], BF16) for i in range(4)]
            g5 = sb("t3_g5", [128, D], F32)
            r_x = R("x0", "x1"); r_m = R("m0", "m1"); r_G = R("G0", "G1", "G2", "G3"); r_g5, = R("g5")
            if last:
                fg = sb("t3_fg", [128, D], F32)
                junk = sb("t3_junk", [128, D], BF16)
                st = [sb("t3_st%d" % i, [128, 2], F32) for i in range(2)]
                r_fg, r_junk = R("fg", "junk"); r_st = R("st0", "st1")
                load_rows_bcast("sp", fg[:, :], fing[0:1, :], r_fg)
            for i in range(4):
                S.op("pool", lambda e, i=i: e.memset(G[i][:, :], 0.0), [], [r_G[i]])
            bcr = {}

            def mkreg(e):
                bcr["r"] = e.alloc_register(U("t3_bc"))
                return e.reg_mov(bcr["r"], NROW_XG - 1)
            S.raw("pool", mkreg)
            it = 0; gi = 0
            for (tok0, nt, row, offs, gsel) in sets:
                load_rows_bcast("sp", g5[:, :], mod_d[layer, row:row + 1, 5 * D:6 * D], r_g5)
                for c in range(nt):
                    k = it % 2; it += 1
                    t0 = tok0 + c * 128
                    S.dma("sp", lambda e, k=k, t0=t0: e.dma_start(out=x1t[k][:, :], in_=x1_d[t0:t0 + 128, :]), [], [r_x[k]])
                    for e_ in range(NE):
                        g = gi % 4; gi += 1
                        S.dma("pool", lambda e, g=g, c=c, e_=e_, offs=offs: e.indirect_dma_start(
                            out=G[g][:, :], out_offset=None, in_=y_d[:, :],
                            in_offset=bass.IndirectOffsetOnAxis(ap=offs[:, c, e_:e_ + 1], axis=0),
                            bounds_check=bcr["r"], oob_is_err=False), [r_offs], [r_G[g]])
                        if e_ == 0:
                            S.op("dve", lambda e, g=g, k=k, c=c, gsel=gsel: e.tensor_scalar(
                                out=macc[k][:, :], in0=G[g][:, :], scalar1=gsel[:, c, 0:1], scalar2=None, op0=ALU.mult),
                                [r_G[g], r_gsel], [r_m[k]])
                        else:
                            S.op("dve", lambda e, g=g, k=k, c=c, e_=e_, gsel=gsel: e.scalar_tensor_tensor(
                                out=macc[k][:, :], in0=G[g][:, :], scalar=gsel[:, c, e_:e_ + 1], in1=macc[k][:, :],
                                op0=ALU.mult, op1=ALU.add), [r_G[g], r_gsel, r_m[k]], [r_m[k]])
                    S.op("dve", lambda e, k=k: e.tensor_tensor(out=macc[k][:, :], in0=macc[k][:, :], in1=g5[:, :], op=ALU.mult),
                         [r_m[k], r_g5], [r_m[k]])
                    S.op("pool", lambda e, k=k: e.tensor_tensor(out=x1t[k][:, :], in0=x1t[k][:, :], in1=macc[k][:, :], op=ALU.add),
                         [r_x[k], r_m[k]], [r_x[k]])
                    if not last:
                        S.dma("sp", lambda e, k=k, t0=t0: e.dma_start(out=x2_d[t0:t0 + 128, :], in_=x1t[k][:, :]), [r_x[k]], [])
                    else:
                        S.op("dve", lambda e, k=k: e.memset(st[k][:, 0:1], 0.0), [], [r_st[k]])
                        S.op("act", lambda e, k=k: e.activation(out=junk[:, :], in_=x1t[k][:, :], func=AF.Square,
                                                                accum_out=st[k][:, 0:1]), [r_x[k], r_st[k]], [r_junk, r_st[k]])
                        S.op("dve", lambda e, k=k: e.tensor_scalar(out=st[k][:, 1:2], in0=st[k][:, 0:1], scalar1=1.0 / D,
                                                                   scalar2=EPS, op0=ALU.mult, op1=ALU.add), [r_st[k]], [r_st[k]])
                        S.op("act", lambda e, k=k: e.activation(out=st[k][:, 1:2], in_=st[k][:, 1:2], func=AF.Sqrt),
                             [r_st[k]], [r_st[k]])
                        S.op("dve", lambda e, k=k: e.reciprocal(out=st[k][:, 1:2], in_=st[k][:, 1:2]), [r_st[k]], [r_st[k]])
                        S.op("dve", lambda e, k=k: e.scalar_tensor_tensor(out=x1t[k][:, :], in0=x1t[k][:, :], scalar=st[k][:, 1:2],
                                                                          in1=fg[:, :], op0=ALU.mult, op1=ALU.mult),
                             [r_x[k], r_st[k], r_fg], [r_x[k]])
                        S.dma("sp", lambda e, k=k, t0=t0: e.dma_start(out=out_d[t0:t0 + 128, :], in_=x1t[k][:, :]), [r_x[k]], [])
            S.raw("pool", lambda e: (e.free_register(bcr["r"]), None)[1])
        return phase_end("T3_%d" % layer)

    plan = [("A", phase_A), ("T1_0", lambda: phase_T1(0)), ("H1_0", lambda: phase_H1(0)), ("N", phase_N),
            ("T2a_0", lambda: phase_T2a(0)), ("T2b_0", lambda: phase_T2b(0)),
            ("E1_0", lambda: phase_E1(0)), ("E2_0", lambda: phase_E2(0)), ("E3_0", lambda: phase_E3(0)),
            ("T3_0", lambda: phase_T3(0)),
            ("T1_1", lambda: phase_T1(1)), ("H1_1", lambda: phase_H1(1)), ("DA", phase_DA),
            ("T2a_1", lambda: phase_T2a(1)), ("T2b_1", lambda: phase_T2b(1)),
            ("E1_1", lambda: phase_E1(1)), ("E2_1", lambda: phase_E2(1)), ("E3_1", lambda: phase_E3(1)),
            ("T3_1", lambda: phase_T3(1))]
    for name, fn in plan:
        if phases is not None and name not in phases:
            continue
        if fn():
            break

    es.close()
    nc._declared_inputs = list(declared.keys())
    return nc, list(declared.keys())


def _host_constants():
    ident = np.eye(128, dtype=np.float32)
    lstrict = np.triu(np.ones((128, 128), np.float32), 1)
    qc = np.arange(GRID_W)
    col_start = np.clip(qc - 8, 0, GRID_W - 16)
    col_mask = (qc[None, :] >= col_start[:, None]) & (qc[None, :] < col_start[:, None] + 16)
    nmask = np.where(col_mask, 0.0, -1e30).astype(np.float32)
    col_idx = np.clip(qc[None, :] - qc[:, None] + 15, 0, 30)
    t = np.arange(N)
    row = (t // GRID_W).astype(np.float32)
    col = (t % GRID_W).astype(np.float32)
    freq = (10000.0 ** (-np.arange(32, dtype=np.float32) / 32)).astype(np.float32)
    ang = np.concatenate([row[:, None] * freq, col[:, None] * freq], axis=-1).astype(np.float32)
    return dict(ident=ident, lstrict=lstrict, nmask=nmask, col_idx=col_idx,
                rope_cos=np.cos(ang).astype(np.float32), rope_sin=np.sin(ang).astype(np.float32))


def make_in_maps(inp):
    cst = _host_constants()
    f = lambda a: np.ascontiguousarray(np.asarray(a, dtype=np.float32))
    rpb = f(inp["na_rpb"])[0]
    nb = rpb[:, :, cst["col_idx"]]
    nb = np.ascontiguousarray(nb.transpose(0, 2, 1, 3)).reshape(32, 64, 15 * 64)
    lam = np.concatenate([f(inp[k]) for k in ("da_lambda_q1", "da_lambda_k1", "da_lambda_q2", "da_lambda_k2")], 0)
    shared = dict(
        ada_w=f(inp["ada_w"]), ada_b=f(inp["ada_b"]), norm1_g=f(inp["norm1_g"]), norm2_g=f(inp["norm2_g"]),
        final_g=f(inp["final_g"]).reshape(1, D), na_w_qkv=f(inp["na_w_qkv"])[0], da_w_qkv=f(inp["da_w_qkv"])[0],
        na_w_o=f(inp["na_w_o"])[0], da_w_o=f(inp["da_w_o"])[0], nbias=nb, nmask=cst["nmask"], lam=lam,
        subln_g=f(inp["da_subln_g"]).reshape(1, 256), w_router=f(inp["moe_w_router"]), w_gate=f(inp["moe_w_gate"]),
        w_up=f(inp["moe_w_up"]), w_down=f(inp["moe_w_down"]), ident=cst["ident"], lstrict=cst["lstrict"],
        rope_cos=cst["rope_cos"], rope_sin=cst["rope_sin"])
    maps = []
    x = f(inp["x"]); c = f(inp["c"]); ctx = f(inp["ctx"]); cc = f(inp["c_ctx"])
    for b in range(2):
        cv = np.stack([c[b], cc], axis=-1)
        cT = np.ascontiguousarray(cv.reshape(KC, 128, 2).transpose(1, 0, 2))
        m = dict(shared)
        m.update(x=x[b], ctx=ctx[b], cT=cT)
        maps.append(m)
    return maps


def kernel(**inputs):
    nc, names = build_program()
    maps = [{k: m[k] for k in names} for m in make_in_maps(inputs)]
    res = run_bass_kernel_spmd(nc, maps, core_ids=[0, 1])
    return np.stack([np.asarray(res.results[b]["out"], dtype=np.float32) for b in range(2)], 0)
```

```python
import numpy as np
from contextlib import ExitStack
import concourse.bass as bass
import concourse.mybir as mybir
from concourse.bass_utils import run_bass_kernel_spmd

F32 = mybir.dt.float32
BF16 = mybir.dt.bfloat16
I32 = mybir.dt.int32
AF = mybir.ActivationFunctionType
ALU = mybir.AluOpType
AX = mybir.AxisListType

D = 4096
KC = D // 128
N = 8192
NCX = 256
NT = N + NCX
NTL = N // 128
NTC = NCX // 128
NE = 16
FF = 1024
CAP = 1024
CAPC = 32
SLOTS = CAP + CAPC
GRID_W = 64
NROWS = N // GRID_W
EPS = 1e-6
SUBLN_EPS = 1e-5
BIG = 1.0e6


class Res:
    __slots__ = ("name", "w", "r")

    def __init__(self, name):
        self.name = name
        self.w = None
        self.r = {}


class Sched:
    CE = ("pe", "act", "dve", "pool")

    def __init__(self, nc, es, ndma=12):
        self.nc = nc
        self.semobj = {}
        self.cnt = {}
        for e in self.CE:
            self.semobj[e] = es.enter_context(nc.semaphore("s_" + e))
            self.cnt[e] = 0
        self.queues = ("sp", "pool")
        self.dkeys = {}
        self.dnext = {}
        for q in self.queues:
            ks = []
            for k in range(ndma):
                key = (q, k)
                self.semobj[key] = es.enter_context(nc.semaphore("d_%s%d" % (q, k)))
                self.cnt[key] = 0
                ks.append(key)
            self.dkeys[q] = ks
            self.dnext[q] = 0
        self.issuers = ("pe", "act", "dve", "pool", "sp")
        self.waited = {i: {} for i in self.issuers}
        self.thunks = {i: [] for i in self.issuers}
        self.ninst = 0

    def _wait(self, issuer, ev):
        if ev is None:
            return
        key, val = ev
        if self.waited[issuer].get(key, 0) >= val:
            return
        self.waited[issuer][key] = val
        sem = self.semobj[key]
        self.thunks[issuer].append(lambda e, sem=sem, val=val: e.wait_ge(sem, val))

    def _deps(self, issuer, reads, writes):
        for r in reads:
            if r.w is not None and not (issuer == "pe" and r.w[0] == "pe"):
                self._wait(issuer, r.w)
        for w in writes:
            if w.w is not None and not (issuer == "pe" and w.w[0] == "pe"):
                self._wait(issuer, w.w)
            for key, val in w.r.items():
                if not (issuer == "pe" and key == "pe"):
                    self._wait(issuer, (key, val))

    def _mark(self, ev, reads, writes):
        key, val = ev
        for r in reads:
            if r.r.get(key, 0) < val:
                r.r[key] = val
        for w in writes:
            w.w = ev
            w.r = {}

    def op(self, eng, fn, reads=(), writes=()):
        self._deps(eng, reads, writes)
        self.cnt[eng] += 1
        val = self.cnt[eng]
        sem = self.semobj[eng]
        self.thunks[eng].append(lambda e, fn=fn, sem=sem: fn(e).then_inc(sem, 1))
        ev = (eng, val)
        self._mark(ev, reads, writes)
        self.ninst += 1
        return ev

    def raw(self, issuer, fn):
        self.thunks[issuer].append(lambda e, fn=fn: fn(e))

    def pe_quiet(self, fn):
        self.thunks["pe"].append(lambda e, fn=fn: fn(e))
        self.ninst += 1

    def dma(self, q, fn, reads=(), writes=()):
        self._deps(q, reads, writes)
        ks = self.dkeys[q]
        key = ks[self.dnext[q] % len(ks)]
        self.dnext[q] += 1
        if self.cnt[key] > 0:
            self._wait(q, (key, self.cnt[key]))
        self.cnt[key] += 16
        val = self.cnt[key]
        sem = self.semobj[key]
        self.thunks[q].append(lambda e, fn=fn, sem=sem: fn(e).then_inc(sem, 16))
        ev = (key, val)
        self._mark(ev, reads, writes)
        self.ninst += 1
        return ev

    def flush(self, name):
        nc = self.nc
        for q in self.queues:
            for key in self.dkeys[q]:
                if self.cnt[key] > 0:
                    self._wait(q, (key, self.cnt[key]))
        th = self.thunks
        with nc.Block(name) as block:
            if th["sp"]:
                @block.sync
                def _(e):
                    for t in th["sp"]:
                        t(e)
            if th["pe"]:
                @block.tensor
                def _(e):
                    for t in th["pe"]:
                        t(e)
            if th["act"]:
                @block.scalar
                def _(e):
                    for t in th["act"]:
                        t(e)
            if th["dve"]:
                @block.vector
                def _(e):
                    for t in th["dve"]:
                        t(e)
            if th["pool"]:
                @block.gpsimd
                def _(e):
                    for t in th["pool"]:
                        t(e)
        self.thunks = {i: [] for i in self.issuers}
        for i in self.issuers:
            for key, val in self.cnt.items():
                self.waited[i][key] = val


def R(*names):
    return [Res(n) for n in names]


def build_program(stop_after=None, debug=(), inject=(), phases=None):
    nc = bass.Bass("TRN2", target_bir_lowering=False)
    es = ExitStack()
    S = Sched(nc, es)

    declared = {}

    class _Lazy:
        def __init__(self, name, shape):
            self.name, self.shape, self.t = name, list(shape), None

        def _get(self):
            if self.t is None:
                self.t = nc.dram_tensor(self.name, self.shape, F32, kind="ExternalInput")
                declared[self.name] = self.t
            return self.t

        def __getitem__(self, key):
            return self._get()[key]

    def din(name, shape):
        return _Lazy(name, shape)

    def dscr(name, shape, dt):
        kind = "ExternalOutput" if name in debug else ("ExternalInput" if name in inject else "Internal")
        return nc.dram_tensor(name, list(shape), dt, kind=kind)

    x_in = din("x", [N, D])
    ctx_in = din("ctx", [NCX, D])
    cT_in = din("cT", [128, KC, 2])
    ada_w = din("ada_w", [2, D, 6 * D])
    ada_b = din("ada_b", [2, 6 * D])
    n1g = din("norm1_g", [2, D])
    n2g = din("norm2_g", [2, D])
    fing = din("final_g", [1, D])
    wqkv = [din("na_w_qkv", [D, 3 * D]), din("da_w_qkv", [D, 3 * D])]
    wo = [din("na_w_o", [D, D]), din("da_w_o", [D, D])]
    nbias = din("nbias", [32, 64, 15 * 64])
    nmask = din("nmask", [64, 64])
    lamp = din("lam", [4, 128])
    sublng = din("subln_g", [1, 256])
    wr = din("w_router", [2, D, NE])
    wg = din("w_gate", [2, NE, D, FF])
    wu = din("w_up", [2, NE, D, FF])
    wd = din("w_down", [2, NE, FF, D])
    ident_in = din("ident", [128, 128])
    lstrict_in = din("lstrict", [128, 128])
    rope_cos = din("rope_cos", [N, 64])
    rope_sin = din("rope_sin", [N, 64])
    out_d = nc.dram_tensor("out", [N, D], F32, kind="ExternalOutput")

    mod_d = dscr("mod_d", [2, 2, 6 * D], F32)
    hT_d = dscr("hT_d", [128, KC, NT], BF16)
    qkv_d = dscr("qkv_d", [NT, 3 * D], BF16)
    oT_d = dscr("oT_d", [128, KC, NT], BF16)
    x1_d = dscr("x1_d", [NT, D], F32)
    x2_d = dscr("x2_d", [NT, D], F32)
    h2_d = dscr("h2_d", [NT, D], BF16)
    aff_d = dscr("aff_d", [NT, NE], F32)
    xg_d = dscr("xg_d", [NE * SLOTS, D], BF16)
    y_d = dscr("y_d", [NE * SLOTS, D], BF16)

    state = {"done": False, "uid": 0}

    def U(n):
        return "%s_u%d" % (n, state["uid"])

    def phase_end(name):
        S.flush(name)
        state["uid"] += 1
        if stop_after == name:
            state["done"] = True
        return state["done"]

    def phase_A():
        with ExitStack() as ps:
            sb = lambda n, s, d: ps.enter_context(nc.sbuf_tensor(U(n), s, d))
            cT = sb("a_cT", [128, KC, 2], F32)
            scT = sb("a_scT", [128, KC, 2], F32)
            wbuf = [sb("a_w%d" % i, [128, KC, 512], F32) for i in range(2)]
            bt = [sb("a_b%d" % i, [2, 512], F32) for i in range(2)]
            ot = [sb("a_o%d" % i, [2, 512], F32) for i in range(2)]
            pacc = [ps.enter_context(nc.psum_tensor(U("a_p%d" % i), [128, 512], F32)) for i in range(2)]
            r_cT, r_scT = R("cT", "scT")
            r_w = R("w0", "w1"); r_b = R("b0", "b1"); r_o = R("o0", "o1"); r_p = R("p0", "p1")
            S.dma("sp", lambda e: e.dma_start(out=cT[:, :, :], in_=cT_in[:, :, :]), [], [r_cT])
            S.op("act", lambda e: e.activation(out=scT[:, :, :], in_=cT[:, :, :], func=AF.Silu), [r_cT], [r_scT])
            it = 0
            for i in range(2):
                for cb in range(6 * D // 512):
                    k = it % 2
                    it += 1
                    c0 = cb * 512
                    S.dma("sp", lambda e, i=i, c0=c0, k=k: e.dma_start(
                        out=wbuf[k][:, :, :],
                        in_=ada_w[i, :, c0:c0 + 512].rearrange("(kc p) n -> p kc n", p=128)), [], [r_w[k]])
                    S.dma("sp", lambda e, i=i, c0=c0, k=k: e.dma_start(
                        out=bt[k][:, :], in_=ada_b[i:i + 1, c0:c0 + 512].partition_broadcast(2)), [], [r_b[k]])
                    for kc in range(KC):
                        fn = lambda e, k=k, kc=kc: e.matmul(pacc[k][0:2, :], scT[:, kc, :], wbuf[k][:, kc, :],
                                                            start=(kc == 0), stop=(kc == KC - 1))
                        if kc < KC - 1:
                            if kc == 0:
                                S.op("pe", fn, [r_scT, r_w[k]], [r_p[k]])
                            else:
                                S.pe_quiet(fn)
                        else:
                            S.op("pe", fn, [r_scT, r_w[k]], [r_p[k]])
                    S.op("dve", lambda e, k=k: e.tensor_tensor(out=ot[k][:, :], in0=pacc[k][0:2, :], in1=bt[k][:, :],
                                                               op=ALU.add), [r_p[k], r_b[k]], [r_o[k]])
                    S.dma("pool", lambda e, i=i, c0=c0, k=k: e.dma_start(out=mod_d[i, :, c0:c0 + 512], in_=ot[k][:, :]),
                          [r_o[k]], [])
        return phase_end("A")

    def src_rows(layer, t0, n):
        if layer == 0:
            if t0 < N:
                return x_in[t0:t0 + n, :]
            return ctx_in[t0 - N:t0 - N + n, :]
        return x2_d[t0:t0 + n, :]

    def load_rows_bcast(q, dst, src_ap, res):
        S.dma(q, lambda e: e.dma_start(out=dst, in_=src_ap.partition_broadcast(128)), [], [res])

    def phase_T1(layer):
        with ExitStack() as ps:
            sb = lambda n, s, d: ps.enter_context(nc.sbuf_tensor(U(n), s, d))
            xt = [sb("t1_x%d" % i, [128, D], F32) for i in range(2)]
            hb = [sb("t1_h%d" % i, [128, D], BF16) for i in range(2)]
            hT4 = [sb("t1_hT%d" % i, [128, KC, 512], BF16) for i in range(2)]
            Arow = sb("t1_A", [128, D], F32)
            Brow = sb("t1_B", [128, D], F32)
            tmp = sb("t1_tmp", [128, D], F32)
            st = [sb("t1_st%d" % i, [128, 2], F32) for i in range(2)]
            ident = sb("t1_id", [128, 128], BF16)
            ptr = [ps.enter_context(nc.psum_tensor(U("t1_p%d" % i), [128, 1024], BF16)) for i in range(4)]
            r_x = R("x0", "x1"); r_h = R("h0", "h1"); r_hT = R("hT0", "hT1"); r_st = R("st0", "st1")
            r_A, r_B, r_tmp, r_id = R("A", "B", "tmp", "id")
            r_p = R("p0", "p1", "p2", "p3")
            S.dma("pool", lambda e: e.dma_start(out=ident[:, :], in_=ident_in[:, :]), [], [r_id])
            it = 0
            si = 0
            for grp, (tok0, ntile, row) in enumerate(((0, NTL, 0), (N, NTC, 1))):
                load_rows_bcast("sp", Arow[:, :], mod_d[layer, row:row + 1, D:2 * D], r_A)
                load_rows_bcast("sp", tmp[:, :], n1g[layer:layer + 1, :], r_tmp)
                load_rows_bcast("sp", Brow[:, :], mod_d[layer, row:row + 1, 0:D], r_B)
                S.op("dve", lambda e: e.scalar_tensor_tensor(out=Arow[:, :], in0=Arow[:, :], scalar=1.0, in1=tmp[:, :],
                                                             op0=ALU.add, op1=ALU.mult), [r_A, r_tmp], [r_A])
                for tt in range(ntile):
                    k = it % 2
                    sti = si % 2
                    sub = tt % 4
                    t0 = tok0 + tt * 128
                    S.dma("sp", lambda e, k=k, t0=t0: e.dma_start(out=xt[k][:, :], in_=src_rows(layer, t0, 128)),
                          [], [r_x[k]])
                    S.op("dve", lambda e, k=k: e.memset(st[k][:, :], 0.0), [], [r_st[k]])
                    S.op("act", lambda e, k=k: e.activation(out=hb[k][:, :], in_=xt[k][:, :], func=AF.Square,
                                                            accum_out=st[k][:, 0:1]), [r_x[k], r_st[k]], [r_h[k], r_st[k]])
                    S.op("dve", lambda e, k=k: e.tensor_scalar(out=st[k][:, 1:2], in0=st[k][:, 0:1], scalar1=1.0 / D,
                                                               scalar2=EPS, op0=ALU.mult, op1=ALU.add), [r_st[k]], [r_st[k]])
                    S.op("act", lambda e, k=k: e.activation(out=st[k][:, 1:2], in_=st[k][:, 1:2], func=AF.Sqrt),
                         [r_st[k]], [r_st[k]])
                    S.op("dve", lambda e, k=k: e.reciprocal(out=st[k][:, 1:2], in_=st[k][:, 1:2]), [r_st[k]], [r_st[k]])
                    S.op("dve", lambda e, k=k: e.scalar_tensor_tensor(out=xt[k][:, :], in0=xt[k][:, :],
                                                                      scalar=st[k][:, 1:2], in1=Arow[:, :],
                                                                      op0=ALU.mult, op1=ALU.mult),
                         [r_x[k], r_st[k], r_A], [r_x[k]])
                    S.op("pool", lambda e, k=k: e.tensor_tensor(out=hb[k][:, :], in0=xt[k][:, :], in1=Brow[:, :],
                                                                op=ALU.add), [r_x[k], r_B], [r_h[k]])
                    for g in range(4):
                        for j in range(8):
                            kc = g * 8 + j
                            fn = lambda e, k=k, g=g, j=j, kc=kc: e.transpose(ptr[g][:, j * 128:(j + 1) * 128],
                                                                             hb[k][:, kc * 128:(kc + 1) * 128], ident[:, :])
                            if j == 0 or j == 7:
                                S.op("pe", fn, [r_h[k], r_id], [r_p[g]])
                            else:
                                S.pe_quiet(fn)
                        eng = "act" if g % 2 == 0 else "dve"
                        if eng == "act":
                            S.op("act", lambda e, g=g, sti=sti, sub=sub: e.activation(
                                out=hT4[sti][:, g * 8:(g + 1) * 8, sub * 128:(sub + 1) * 128],
                                in_=ptr[g][:, :].rearrange("p (j t) -> p j t", j=8), func=AF.Copy), [r_p[g]], [r_hT[sti]])
                        else:
                            S.op("dve", lambda e, g=g, sti=sti, sub=sub: e.tensor_copy(
                                out=hT4[sti][:, g * 8:(g + 1) * 8, sub * 128:(sub + 1) * 128],
                                in_=ptr[g][:, :].rearrange("p (j t) -> p j t", j=8)), [r_p[g]], [r_hT[sti]])
                    it += 1
                    if sub == 3 or tt == ntile - 1:
                        nn = (sub + 1) * 128
                        s0 = t0 - sub * 128
                        S.dma("pool", lambda e, sti=sti, s0=s0, nn=nn: e.dma_start(out=hT_d[:, :, s0:s0 + nn],
                                                                                 in_=hT4[sti][:, :, 0:nn]), [r_hT[sti]], [])
                        si += 1
        return phase_end("T1_%d" % layer)


    SCALE = 128.0 ** -0.5

    def mm_group(out_ap, pairs, reads, wres):
        n = len(pairs)
        for i, (l, r) in enumerate(pairs):
            fn = lambda e, l=l, r=r, i=i: e.matmul(out_ap, l, r, start=(i == 0), stop=(i == n - 1))
            if i == 0 or i == n - 1:
                S.op("pe", fn, reads, [wres])
            else:
                S.pe_quiet(fn)

    def phase_H1(layer):
        W = wqkv[layer]
        with ExitStack() as ps:
            sb = lambda n, s, d: ps.enter_context(nc.sbuf_tensor(U(n), s, d))
            wb = sb("h1_w", [128, KC, 1024], BF16)
            hT4 = [sb("h1_hT%d" % i, [128, KC, 512], BF16) for i in range(2)]
            stage = [sb("h1_st%d" % i, [128, 1024], BF16) for i in range(2)]
            pacc = [ps.enter_context(nc.psum_tensor(U("h1_p%d" % i), [128, 512], F32)) for i in range(4)]
            r_w, = R("w"); r_hT = R("hT0", "hT1"); r_st = R("st0", "st1"); r_p = R("p0", "p1", "p2", "p3")
            if layer == 1:
                cs = [sb("h1_cos%d" % i, [128, 64], F32) for i in range(2)]
                sn = [sb("h1_sin%d" % i, [128, 64], F32) for i in range(2)]
                tm = [sb("h1_tm%d" % i, [128, 4, 64], F32) for i in range(4)]
                r_cs = R("cs0", "cs1"); r_tm = R("tm0", "tm1", "tm2", "tm3")
            hi = 0; si = 0; pi = 0; ci = 0
            for cb in range(12):
                S.dma("pool", lambda e, cb=cb: e.dma_start(
                    out=wb[:, :, :], in_=W[:, cb * 1024:(cb + 1) * 1024].rearrange("(kc p) n -> p kc n", p=128)),
                    [], [r_w])
                def load_hT(st_, k_):
                    nt_ = 4 if st_ < 16 else 2
                    S.dma("sp", lambda e, k_=k_, st_=st_, nt_=nt_: e.dma_start(
                        out=hT4[k_][:, :, 0:nt_ * 128], in_=hT_d[:, :, st_ * 512:st_ * 512 + nt_ * 128]), [], [r_hT[k_]])
                if cb == 0:
                    load_hT(0, hi % 2)
                for st in range(17):
                    ntile = 4 if st < 16 else 2
                    k = hi % 2; hi += 1
                    if st + 1 < 17:
                        load_hT(st + 1, hi % 2)
                    elif cb + 1 < 12:
                        load_hT(0, hi % 2)
                    for ts in range(ntile):
                        t0 = st * 512 + ts * 128
                        sk = si % 2; si += 1
                        rope = (layer == 1 and cb < 8 and st < 16)
                        if rope:
                            ck = ci % 2; ci += 1
                            S.dma("sp", lambda e, ck=ck, t0=t0: e.dma_start(out=cs[ck][:, :], in_=rope_cos[t0:t0 + 128, :]),
                                  [], [r_cs[ck]])
                            S.dma("sp", lambda e, ck=ck, t0=t0: e.dma_start(out=sn[ck][:, :], in_=rope_sin[t0:t0 + 128, :]),
                                  [], [r_cs[ck]])
                        for half in range(2):
                            p = pi % 4; pi += 1
                            mm_group(pacc[p][:, :],
                                     [(hT4[k][:, kc, ts * 128:(ts + 1) * 128], wb[:, kc, half * 512:(half + 1) * 512])
                                      for kc in range(KC)], [r_hT[k], r_w], r_p[p])
                            dst = stage[sk][:, half * 512:(half + 1) * 512]
                            if not rope:
                                if half == 0:
                                    S.op("act", lambda e, dst=dst, p=p: e.activation(out=dst, in_=pacc[p][:, :], func=AF.Copy),
                                         [r_p[p]], [r_st[sk]])
                                else:
                                    S.op("dve", lambda e, dst=dst, p=p: e.tensor_copy(out=dst, in_=pacc[p][:, :]),
                                         [r_p[p]], [r_st[sk]])
                            else:
                                pv = pacc[p][:, :].rearrange("p (g i two) -> p g i two", g=4, i=64, two=2)
                                dv = dst.rearrange("p (g i two) -> p g i two", g=4, i=64, two=2)
                                cb_ = cs[ck][:, :].unsqueeze(1).to_broadcast([128, 4, 64])
                                sb_ = sn[ck][:, :].unsqueeze(1).to_broadcast([128, 4, 64])
                                xe, xo = pv[:, :, :, 0], pv[:, :, :, 1]
                                S.op("dve", lambda e, xe=xe, cb_=cb_: e.tensor_tensor(out=tm[0][:, :, :], in0=xe, in1=cb_, op=ALU.mult),
                                     [r_p[p], r_cs[ck]], [r_tm[0]])
                                S.op("dve", lambda e, xo=xo, sb_=sb_: e.tensor_tensor(out=tm[1][:, :, :], in0=xo, in1=sb_, op=ALU.mult),
                                     [r_p[p], r_cs[ck]], [r_tm[1]])
                                S.op("dve", lambda e, xe=xe, sb_=sb_: e.tensor_tensor(out=tm[2][:, :, :], in0=xe, in1=sb_, op=ALU.mult),
                                     [r_p[p], r_cs[ck]], [r_tm[2]])
                                S.op("dve", lambda e, xo=xo, cb_=cb_: e.tensor_tensor(out=tm[3][:, :, :], in0=xo, in1=cb_, op=ALU.mult),
                                     [r_p[p], r_cs[ck]], [r_tm[3]])
                                S.op("pool", lambda e, dv=dv: e.tensor_tensor(out=dv[:, :, :, 0], in0=tm[0][:, :, :], in1=tm[1][:, :, :],
                                                                              op=ALU.subtract), [r_tm[0], r_tm[1]], [r_st[sk]])
                                S.op("pool", lambda e, dv=dv: e.tensor_tensor(out=dv[:, :, :, 1], in0=tm[2][:, :, :], in1=tm[3][:, :, :],
                                                                              op=ALU.add), [r_tm[2], r_tm[3]], [r_st[sk]])
                        S.dma("pool", lambda e, sk=sk, t0=t0, cb=cb: e.dma_start(
                            out=qkv_d[t0:t0 + 128, cb * 1024:(cb + 1) * 1024], in_=stage[sk][:, :]), [r_st[sk]], [])
        return phase_end("H1_%d" % layer)

    def phase_N():
        with ExitStack() as ps:
            sb = lambda n, s, d: ps.enter_context(nc.sbuf_tensor(U(n), s, d))
            qtm = sb("n_qtm", [128, 66, 128], BF16)
            ktm = sb("n_ktm", [128, 66, 128], BF16)
            QT = sb("n_QT", [128, NT], BF16)
            KT = sb("n_KT", [128, NT], BF16)
            Va = [sb("n_va%d" % i, [128, 64, 128], BF16) for i in range(2)]
            Vb = [sb("n_vb%d" % i, [128, 63, 128], BF16) for i in range(2)]
            Vc = [sb("n_vc%d" % i, [128, 2, 128], BF16) for i in range(2)]
            bias = [sb("n_bias%d" % i, [64, 960], F32) for i in range(2)]
            oTh = sb("n_oT", [128, NT], BF16)
            sbs = [sb("n_s%d" % i, [128, 768], F32) for i in range(2)]
            pbf = [sb("n_p%d" % i, [128, 768], BF16) for i in range(2)]
            pT = [sb("n_pT%d" % i, [128, 384], BF16) for i in range(2)]
            stt = [sb("n_stt%d" % i, [128, 4], F32) for i in range(2)]
            stt2 = [sb("n_stt2%d" % i, [128, 4], F32) for i in range(2)]
            obf = [sb("n_o%d" % i, [128, 128], BF16) for i in range(2)]
            ident = sb("n_id", [128, 128], BF16)
            msk = sb("n_msk", [64, 64], F32)
            ptq0 = ps.enter_context(nc.psum_tensor(U("n_ptq0"), [128, 1024], BF16))
            ptq1 = ps.enter_context(nc.psum_tensor(U("n_ptq1"), [128, 1024], BF16))
            ps_a = [ps.enter_context(nc.psum_tensor(U("n_pa%d" % i), [128, 512], F32)) for i in range(2)]
            ps_b = [ps.enter_context(nc.psum_tensor(U("n_pb%d" % i), [128, 512], F32)) for i in range(2)]
            ps_o = [ps.enter_context(nc.psum_tensor(U("n_po%d" % i), [128, 512], F32)) for i in range(2)]
            r_qtm, r_ktm, r_QT, r_KT, r_oTh, r_id, r_msk, r_q0, r_q1a, r_q1b = R(
                "qtm", "ktm", "QT", "KT", "oTh", "id", "msk", "q0", "q1a", "q1b")
            r_V = R("V0", "V1"); r_bias = R("b0", "b1")
            r_s = R("s0", "s1"); r_p = R("p0", "p1"); r_pT = R("pT0", "pT1"); r_stt = R("t0", "t1"); r_o = R("o0", "o1")
            r_pa = R("pa0", "pa1"); r_pb = R("pb0", "pb1"); r_po = R("po0", "po1"); r_stt2 = R("u0", "u1")
            S.dma("pool", lambda e: e.dma_start(out=ident[:, :], in_=ident_in[:, :]), [], [r_id])
            S.dma("sp", lambda e: e.dma_start(out=msk[:, :], in_=nmask[:, :]), [], [r_msk])
            wi = 0
            for hh in range(32):
                hb = hh % 2
                qs = lambda c0: qkv_d[:, c0:c0 + 128]
                S.dma("sp", lambda e, hh=hh: e.dma_start(
                    out=qtm[:, :, :], in_=qkv_d[:, hh * 128:(hh + 1) * 128].rearrange("(c p) d -> p c d", p=128)), [], [r_qtm])
                S.dma("sp", lambda e, hh=hh: e.dma_start(
                    out=ktm[:, :, :], in_=qkv_d[:, D + hh * 128:D + (hh + 1) * 128].rearrange("(c p) d -> p c d", p=128)),
                    [], [r_ktm])
                vcol = 2 * D + hh * 128
                S.dma("sp", lambda e, hb=hb, vcol=vcol: e.dma_start(
                    out=Va[hb][:, :, :], in_=qkv_d[0:N, vcol:vcol + 128].rearrange("(c p) d -> p c d", p=128)), [], [r_V[hb]])
                S.dma("sp", lambda e, hb=hb, vcol=vcol: e.dma_start(
                    out=Vb[hb][:, :, :], in_=qkv_d[64:64 + 63 * 128, vcol:vcol + 128].rearrange("(c p) d -> p c d", p=128)),
                    [], [r_V[hb]])
                S.dma("sp", lambda e, hb=hb, vcol=vcol: e.dma_start(
                    out=Vc[hb][:, :, :], in_=qkv_d[N:NT, vcol:vcol + 128].rearrange("(c p) d -> p c d", p=128)), [], [r_V[hb]])
                S.dma("sp", lambda e, hb=hb, hh=hh: e.dma_start(out=bias[hb][:, :], in_=nbias[hh, :, :]), [], [r_bias[hb]])
                S.op("dve", lambda e, hb=hb: e.tensor_tensor(
                    out=bias[hb][:, :].rearrange("p (j k) -> p j k", j=15),
                    in0=bias[hb][:, :].rearrange("p (j k) -> p j k", j=15),
                    in1=msk[:, :].unsqueeze(1).to_broadcast([64, 15, 64]), op=ALU.add), [r_bias[hb], r_msk], [r_bias[hb]])
                for (src, r_src, dstT, r_dst) in ((qtm, r_qtm, QT, r_QT), (ktm, r_ktm, KT, r_KT)):
                    for c0 in range(0, 66, 8):
                        nb = min(8, 66 - c0)
                        for j in range(nb):
                            fn = lambda e, src=src, c=c0 + j, j=j: e.transpose(ptq0[:, j * 128:(j + 1) * 128], src[:, c, :], ident[:, :])
                            if j == 0 or j == nb - 1:
                                S.op("pe", fn, [r_src, r_id], [r_q0])
                            else:
                                S.pe_quiet(fn)
                        S.op("dve", lambda e, dstT=dstT, c0=c0, nb=nb: e.tensor_copy(
                            out=dstT[:, c0 * 128:(c0 + nb) * 128], in_=ptq0[:, 0:nb * 128]), [r_q0], [r_dst])
                def row_info(r):
                    is_ctx = r >= NROWS
                    if not is_ctx:
                        rs = min(max(r - 4, 0), NROWS - 8)
                        return dict(is_ctx=False, P=64, rs=rs, j0=rs - r + 7, q0=r * 64, nk=768)
                    return dict(is_ctx=True, P=128, rs=0, j0=0, q0=N + (r - NROWS) * 128, nk=256)

                def emit_S(r, k):
                    ri = row_info(r)
                    q0 = ri["q0"]
                    if not ri["is_ctx"]:
                        rs, j0 = ri["rs"], ri["j0"]
                        S.op("pe", lambda e, k=k, q0=q0, rs=rs: e.matmul(ps_a[k][0:64, 0:512], QT[:, q0:q0 + 64],
                                                                       KT[:, rs * 64:rs * 64 + 512], start=True, stop=True),
                             [r_QT, r_KT], [r_pa[k]])
                        S.op("pe", lambda e, k=k, q0=q0: e.matmul(ps_b[k][0:64, 0:256], QT[:, q0:q0 + 64], KT[:, N:NT],
                                                                start=True, stop=True), [r_QT, r_KT], [r_pb[k]])
                    else:
                        S.op("pe", lambda e, k=k, q0=q0: e.matmul(ps_a[k][:, 0:256], QT[:, q0:q0 + 128], KT[:, N:NT],
                                                                start=True, stop=True), [r_QT, r_KT], [r_pa[k]])

                def emit_softmax(r, k):
                    ri = row_info(r)
                    P_, nk = ri["P"], ri["nk"]
                    if not ri["is_ctx"]:
                        j0 = ri["j0"]
                        S.op("dve", lambda e, k=k, hb=hb, j0=j0: e.scalar_tensor_tensor(
                            out=sbs[k][0:64, 0:512], in0=ps_a[k][0:64, 0:512], scalar=SCALE,
                            in1=bias[hb][:, j0 * 64:j0 * 64 + 512], op0=ALU.mult, op1=ALU.add),
                            [r_pa[k], r_bias[hb]], [r_s[k]])
                        S.op("act", lambda e, k=k: e.activation(out=sbs[k][0:64, 512:768], in_=ps_b[k][0:64, 0:256],
                                                                func=AF.Copy, scale=SCALE), [r_pb[k]], [r_s[k]])
                    else:
                        S.op("act", lambda e, k=k: e.activation(out=sbs[k][:, 0:256], in_=ps_a[k][:, 0:256],
                                                                func=AF.Copy, scale=SCALE), [r_pa[k]], [r_s[k]])
                    S.op("pool", lambda e, k=k, P_=P_: e.memset(stt2[k][0:P_, 0:1], 0.0), [], [r_stt2[k]])
                    S.op("act", lambda e, k=k, P_=P_, nk=nk: e.activation(
                        out=pbf[k][0:P_, 0:nk], in_=sbs[k][0:P_, 0:nk], func=AF.Exp,
                        accum_out=stt2[k][0:P_, 0:1]), [r_s[k], r_stt2[k]], [r_p[k], r_stt2[k]])
                    S.op("dve", lambda e, k=k, P_=P_: e.reciprocal(out=stt2[k][0:P_, 1:2], in_=stt2[k][0:P_, 0:1]),
                         [r_stt2[k]], [r_stt2[k]])

                def emit_PV(r, k):
                    ri = row_info(r)
                    P_, nk, rs, q0, is_ctx = ri["P"], ri["nk"], ri["rs"], ri["q0"], ri["is_ctx"]
                    nch = nk // 128
                    for c in range(nch):
                        fn = lambda e, k=k, c=c, P_=P_: e.transpose(ptq1[:, c * P_:(c + 1) * P_],
                                                                    pbf[k][0:P_, c * 128:(c + 1) * 128], ident[0:P_, 0:P_])
                        if c == 0 or c == nch - 1:
                            S.op("pe", fn, [r_p[k], r_id], [r_q1a])
                        else:
                            S.pe_quiet(fn)
                    S.op("dve", lambda e, k=k, w_=nch * P_: e.tensor_copy(out=pT[k][:, 0:w_], in_=ptq1[:, 0:w_]),
                         [r_q1a], [r_pT[k]])
                    pairs = []
                    for c in range(nch):
                        if is_ctx:
                            vch = Vc[hb][:, c, :]
                        elif c < 4:
                            vch = Va[hb][:, rs // 2 + c, :] if rs % 2 == 0 else Vb[hb][:, (rs - 1) // 2 + c, :]
                        else:
                            vch = Vc[hb][:, c - 4, :]
                        pairs.append((pT[k][:, c * P_:(c + 1) * P_], vch))
                    mm_group(ps_o[k][0:P_, 0:128], pairs, [r_pT[k], r_V[hb]], r_po[k])
                    S.op("act", lambda e, k=k, P_=P_: e.activation(out=obf[k][0:P_, :], in_=ps_o[k][0:P_, 0:128], func=AF.Copy,
                                                                   scale=stt2[k][0:P_, 1:2]), [r_po[k], r_stt2[k]], [r_o[k]])
                    S.op("pe", lambda e, k=k, P_=P_: e.transpose(ptq1[:, 512:512 + P_], obf[k][0:P_, :], ident[0:P_, 0:P_]),
                         [r_o[k], r_id], [r_q1b])
                    S.op("dve", lambda e, q0=q0, P_=P_: e.tensor_copy(out=oTh[:, q0:q0 + P_], in_=ptq1[:, 512:512 + P_]),
                         [r_q1b], [r_oTh])

                NR = NROWS + 2
                emit_S(0, wi % 2)
                for r in range(NR):
                    k = wi % 2
                    emit_softmax(r, k)
                    if r + 1 < NR:
                        emit_S(r + 1, (wi + 1) % 2)
                    emit_PV(r, k)
                    wi += 1
                S.dma("sp", lambda e, hh=hh: e.dma_start(out=oT_d[:, hh, :], in_=oTh[:, :]), [r_oTh], [])
        return phase_end("N")

    def phase_T2a(layer):
        W = wo[layer]
        with ExitStack() as ps:
            sb = lambda n, s, d: ps.enter_context(nc.sbuf_tensor(U(n), s, d))
            wob = [sb("t2_w%d" % i, [128, KC, 512], BF16) for i in range(2)]
            oT4 = [sb("t2_oT%d" % i, [128, KC, 512], BF16) for i in range(2)]
            xb = [sb("t2_x%d" % i, [128, 512], F32) for i in range(2)]
            yb = [sb("t2_y%d" % i, [128, 512], F32) for i in range(2)]
            g2 = [sb("t2_g%d" % i, [128, D], F32) for i in range(2)]
            pacc = [ps.enter_context(nc.psum_tensor(U("t2_p%d" % i), [128, 512], F32)) for i in range(4)]
            r_w = R("w0", "w1"); r_oT = R("o0", "o1"); r_x = R("x0", "x1"); r_y = R("y0", "y1"); r_g = R("g0", "g1")
            r_p = R("p0", "p1", "p2", "p3")
            for row in range(2):
                load_rows_bcast("sp", g2[row][:, :], mod_d[layer, row:row + 1, 2 * D:3 * D], r_g[row])
            oi = 0; xi = 0; pi = 0
            for nb in range(8):
                wk = nb % 2
                S.dma("pool", lambda e, wk=wk, nb=nb: e.dma_start(
                    out=wob[wk][:, :, :], in_=W[:, nb * 512:(nb + 1) * 512].rearrange("(kc p) n -> p kc n", p=128)),
                    [], [r_w[wk]])
                NST = 17 if layer == 0 else 16

                def load_oT(st_, k_):
                    nt_ = 4 if st_ < 16 else 2
                    S.dma("sp", lambda e, k_=k_, st_=st_, nt_=nt_: e.dma_start(
                        out=oT4[k_][:, :, 0:nt_ * 128], in_=oT_d[:, :, st_ * 512:st_ * 512 + nt_ * 128]), [], [r_oT[k_]])
                if nb == 0:
                    load_oT(0, oi % 2)
                for st in range(NST):
                    ntile = 4 if st < 16 else 2
                    row = 0 if st < 16 else 1
                    k = oi % 2; oi += 1
                    if st + 1 < NST:
                        load_oT(st + 1, oi % 2)
                    elif nb + 1 < 8:
                        load_oT(0, oi % 2)
                    for ts in range(ntile):
                        t0 = st * 512 + ts * 128
                        j = xi % 2; xi += 1
                        p = pi % 4; pi += 1
                        S.dma("sp", lambda e, j=j, t0=t0, nb=nb: e.dma_start(
                            out=xb[j][:, :], in_=src_rows(layer, t0, 128)[:, nb * 512:(nb + 1) * 512]), [], [r_x[j]])
                        mm_group(pacc[p][:, :], [(oT4[k][:, kc, ts * 128:(ts + 1) * 128], wob[wk][:, kc, :]) for kc in range(KC)],
                                 [r_oT[k], r_w[wk]], r_p[p])
                        S.op("dve", lambda e, j=j, p=p, row=row, nb=nb: e.tensor_tensor(
                            out=yb[j][:, :], in0=pacc[p][:, :], in1=g2[row][:, nb * 512:(nb + 1) * 512], op=ALU.mult),
                            [r_p[p], r_g[row]], [r_y[j]])
                        S.op("pool", lambda e, j=j: e.tensor_tensor(out=yb[j][:, :], in0=yb[j][:, :], in1=xb[j][:, :], op=ALU.add),
                             [r_y[j], r_x[j]], [r_y[j]])
                        S.dma("pool", lambda e, j=j, t0=t0, nb=nb: e.dma_start(
                            out=x1_d[t0:t0 + 128, nb * 512:(nb + 1) * 512], in_=yb[j][:, :]), [r_y[j]], [])
        return phase_end("T2a_%d" % layer)

    def phase_T2b(layer):
        with ExitStack() as ps:
            sb = lambda n, s, d: ps.enter_context(nc.sbuf_tensor(U(n), s, d))
            xt = [sb("tb_x%d" % i, [128, D], F32) for i in range(2)]
            hb = [sb("tb_h%d" % i, [128, D], BF16) for i in range(2)]
            Arow = sb("tb_A", [128, D], F32)
            Brow = sb("tb_B", [128, D], F32)
            tmp = sb("tb_tmp", [128, D], F32)
            st = [sb("tb_st%d" % i, [128, 8], F32) for i in range(2)]
            hfT = sb("tb_hfT", [128, KC, 128], F32)
            identf = sb("tb_id", [128, 128], F32)
            wrb = sb("tb_wr", [128, KC, NE], F32)
            lg = [sb("tb_lg%d" % i, [128, NE], F32) for i in range(2)]
            ptr = [ps.enter_context(nc.psum_tensor(U("tb_p%d" % i), [128, 512], F32)) for i in range(4)]
            pl = ps.enter_context(nc.psum_tensor(U("tb_pl"), [128, 512], F32))
            r_x = R("x0", "x1"); r_h = R("h0", "h1"); r_st = R("st0", "st1"); r_lg = R("lg0", "lg1")
            r_A, r_B, r_tmp, r_id, r_wr, r_hfT, r_pl = R("A", "B", "tmp", "id", "wr", "hfT", "pl")
            r_p = R("p0", "p1", "p2", "p3")
            S.dma("sp", lambda e: e.dma_start(out=identf[:, :], in_=ident_in[:, :]), [], [r_id])
            S.dma("sp", lambda e: e.dma_start(out=wrb[:, :, :], in_=wr[layer, :, :].rearrange("(kc p) n -> p kc n", p=128)),
                  [], [r_wr])
            it = 0
            for (tok0, ntile, row) in (((0, NTL, 0), (N, NTC, 1)) if layer == 0 else ((0, NTL, 0),)):
                load_rows_bcast("sp", Arow[:, :], mod_d[layer, row:row + 1, 4 * D:5 * D], r_A)
                load_rows_bcast("sp", tmp[:, :], n2g[layer:layer + 1, :], r_tmp)
                load_rows_bcast("sp", Brow[:, :], mod_d[layer, row:row + 1, 3 * D:4 * D], r_B)
                S.op("dve", lambda e: e.scalar_tensor_tensor(out=Arow[:, :], in0=Arow[:, :], scalar=1.0, in1=tmp[:, :],
                                                             op0=ALU.add, op1=ALU.mult), [r_A, r_tmp], [r_A])
                for tt in range(ntile):
                    k = it % 2; it += 1
                    t0 = tok0 + tt * 128
                    S.dma("sp", lambda e, k=k, t0=t0: e.dma_start(out=xt[k][:, :], in_=x1_d[t0:t0 + 128, :]), [], [r_x[k]])
                    S.op("dve", lambda e, k=k: e.memset(st[k][:, 0:1], 0.0), [], [r_st[k]])
                    S.op("act", lambda e, k=k: e.activation(out=hb[k][:, :], in_=xt[k][:, :], func=AF.Square,
                                                            accum_out=st[k][:, 0:1]), [r_x[k], r_st[k]], [r_h[k], r_st[k]])
                    S.op("dve", lambda e, k=k: e.tensor_scalar(out=st[k][:, 1:2], in0=st[k][:, 0:1], scalar1=1.0 / D,
                                                               scalar2=EPS, op0=ALU.mult, op1=ALU.add), [r_st[k]], [r_st[k]])
                    S.op("act", lambda e, k=k: e.activation(out=st[k][:, 1:2], in_=st[k][:, 1:2], func=AF.Sqrt),
                         [r_st[k]], [r_st[k]])
                    S.op("dve", lambda e, k=k: e.reciprocal(out=st[k][:, 1:2], in_=st[k][:, 1:2]), [r_st[k]], [r_st[k]])
                    S.op("dve", lambda e, k=k: e.scalar_tensor_tensor(out=xt[k][:, :], in0=xt[k][:, :], scalar=st[k][:, 1:2],
                                                                      in1=Arow[:, :], op0=ALU.mult, op1=ALU.mult),
                         [r_x[k], r_st[k], r_A], [r_x[k]])
                    S.op("pool", lambda e, k=k: e.tensor_tensor(out=xt[k][:, :], in0=xt[k][:, :], in1=Brow[:, :], op=ALU.add),
                         [r_x[k], r_B], [r_x[k]])
                    S.op("act", lambda e, k=k: e.activation(out=hb[k][:, :], in_=xt[k][:, :], func=AF.Copy), [r_x[k]], [r_h[k]])
                    S.dma("pool", lambda e, k=k, t0=t0: e.dma_start(out=h2_d[t0:t0 + 128, :], in_=hb[k][:, :]), [r_h[k]], [])
                    for g in range(8):
                        pg = g % 4
                        for j in range(4):
                            kc = g * 4 + j
                            fn = lambda e, k=k, pg=pg, j=j, kc=kc: e.transpose(ptr[pg][:, j * 128:(j + 1) * 128],
                                                                               xt[k][:, kc * 128:(kc + 1) * 128], identf[:, :])
                            if j == 0 or j == 3:
                                S.op("pe", fn, [r_x[k], r_id], [r_p[pg]])
                            else:
                                S.pe_quiet(fn)
                        if g % 2 == 0:
                            S.op("act", lambda e, g=g, pg=pg: e.activation(
                                out=hfT[:, g * 4:(g + 1) * 4, :], in_=ptr[pg][:, :].rearrange("p (j t) -> p j t", j=4),
                                func=AF.Copy), [r_p[pg]], [r_hfT])
                        else:
                            S.op("dve", lambda e, g=g, pg=pg: e.tensor_copy(
                                out=hfT[:, g * 4:(g + 1) * 4, :], in_=ptr[pg][:, :].rearrange("p (j t) -> p j t", j=4)),
                                [r_p[pg]], [r_hfT])
                    mm_group(pl[:, 0:NE], [(hfT[:, kc, :], wrb[:, kc, :]) for kc in range(KC)], [r_hfT, r_wr], r_pl)
                    S.op("dve", lambda e, k=k: e.tensor_reduce(out=st[k][:, 2:3], in_=pl[:, 0:NE], axis=AX.X, op=ALU.max),
                         [r_pl], [r_st[k]])
                    S.op("dve", lambda e, k=k: e.tensor_scalar(out=st[k][:, 3:4], in0=st[k][:, 2:3], scalar1=-1.0, scalar2=None,
                                                               op0=ALU.mult), [r_st[k]], [r_st[k]])
                    S.op("dve", lambda e, k=k: e.memset(st[k][:, 4:5], 0.0), [], [r_st[k]])
                    S.op("act", lambda e, k=k: e.activation(out=lg[k][:, :], in_=pl[:, 0:NE], func=AF.Exp, bias=st[k][:, 3:4],
                                                            scale=1.0, accum_out=st[k][:, 4:5]), [r_pl, r_st[k]], [r_lg[k], r_st[k]])
                    S.op("dve", lambda e, k=k: e.reciprocal(out=st[k][:, 5:6], in_=st[k][:, 4:5]), [r_st[k]], [r_st[k]])
                    S.op("dve", lambda e, k=k: e.tensor_scalar(out=lg[k][:, :], in0=lg[k][:, :], scalar1=st[k][:, 5:6],
                                                               scalar2=None, op0=ALU.mult), [r_lg[k], r_st[k]], [r_lg[k]])
                    S.dma("pool", lambda e, k=k, t0=t0: e.dma_start(out=aff_d[t0:t0 + 128, :], in_=lg[k][:, :]), [r_lg[k]], [])
        return phase_end("T2b_%d" % layer)

    def phase_DA():
        import math
        LAM_INIT = 0.8 - 0.6 * math.exp(-0.3 * 1)
        with ExitStack() as ps:
            sb = lambda n, s, d: ps.enter_context(nc.sbuf_tensor(U(n), s, d))
            tm = sb("da_tm", [128, 66, 256], BF16)
            QT = sb("da_QT", [128, 2, N], BF16)
            KT = sb("da_KT", [128, 2, NT], BF16)
            Vaug = sb("da_V", [128, 66, 257], BF16)
            PT = [sb("da_PT%d" % i, [128, 512], BF16) for i in range(3)]
            ident = sb("da_id", [128, 128], BF16)
            lrow = sb("da_lrow", [128, 4, 128], F32)
            ltmp = sb("da_ltmp", [128, 128], F32)
            lamt = sb("da_lam", [128, 8], F32)
            gsub = sb("da_gsub", [128, 256], F32)
            o32 = [sb("da_o32%d" % i, [128, 256], F32) for i in range(2)]
            obf = [sb("da_obf%d" % i, [128, 256], BF16) for i in range(2)]
            osb = [sb("da_osb%d" % i, [128, 2, 128], BF16) for i in range(2)]
            stt = [sb("da_stt%d" % i, [128, 8], F32) for i in range(2)]
            junk = sb("da_junk", [128, 256], BF16)
            ptq = ps.enter_context(nc.psum_tensor(U("da_ptq"), [128, 1024], BF16))
            pso = ptq
            ps_s = [ps.enter_context(nc.psum_tensor(U("da_ps%d" % i), [128, 512], F32)) for i in range(3)]
            po = [ps.enter_context(nc.psum_tensor(U("da_po%d" % i), [128, 512], F32)) for i in range(4)]
            r_tm, r_QT, r_KT, r_V, r_id, r_lrow, r_ltmp, r_lam, r_gsub, r_junk, r_ptq, r_pso = R(
                "tm", "QT", "KT", "V", "id", "lrow", "ltmp", "lam", "gsub", "junk", "ptq", "pso")
            r_PT = R("PT0", "PT1", "PT2"); r_o32 = R("o0", "o1"); r_obf = R("ob0", "ob1"); r_osb = R("os0", "os1")
            r_stt = R("st0", "st1"); r_ps = R("ps0", "ps1", "ps2"); r_po = R("po0", "po1", "po2", "po3")
            r_pso = r_ptq
            S.dma("pool", lambda e: e.dma_start(out=ident[:, :], in_=ident_in[:, :]), [], [r_id])
            for i in range(4):
                S.dma("sp", lambda e, i=i: e.dma_start(out=lrow[:, i, :], in_=lamp[i:i + 1, :].partition_broadcast(128)),
                      [], [r_lrow])
            for j in range(2):
                S.op("dve", lambda e, j=j: e.tensor_tensor(out=ltmp[:, :], in0=lrow[:, 2 * j, :], in1=lrow[:, 2 * j + 1, :],
                                                           op=ALU.mult), [r_lrow], [r_ltmp])
                S.op("dve", lambda e, j=j: e.tensor_reduce(out=lamt[:, j:j + 1], in_=ltmp[:, :], axis=AX.X, op=ALU.add),
                     [r_ltmp], [r_lam])
            S.op("act", lambda e: e.activation(out=lamt[:, 2:4], in_=lamt[:, 0:2], func=AF.Exp), [r_lam], [r_lam])
            S.op("dve", lambda e: e.tensor_tensor(out=lamt[:, 4:5], in0=lamt[:, 3:4], in1=lamt[:, 2:3], op=ALU.subtract),
                 [r_lam], [r_lam])
            S.op("dve", lambda e: e.tensor_scalar(out=lamt[:, 5:6], in0=lamt[:, 4:5], scalar1=-LAM_INIT, scalar2=None, op0=ALU.add),
                 [r_lam], [r_lam])
            load_rows_bcast("sp", gsub[:, :], sublng[0:1, :], r_gsub)
            S.op("dve", lambda e: e.tensor_scalar(out=gsub[:, :], in0=gsub[:, :], scalar1=1.0 - LAM_INIT, scalar2=None, op0=ALU.mult),
                 [r_gsub], [r_gsub])
            S.op("pool", lambda e: e.memset(Vaug[:, :, 256:257], 1.0), [], [r_V])
            step = 0; ei = 0
            for h in range(16):
                for (c_off, nchunk, dstT, r_dst) in ((h * 256, NTL, QT, r_QT), (D + h * 256, NTL + NTC, KT, r_KT)):
                    S.dma("sp", lambda e, c_off=c_off, nchunk=nchunk: e.dma_start(
                        out=tm[:, 0:nchunk, :],
                        in_=qkv_d[0:nchunk * 128, c_off:c_off + 256].rearrange("(c p) d -> p c d", p=128)), [], [r_tm])
                    for comp in range(2):
                        for c0 in range(0, nchunk, 8):
                            nb = min(8, nchunk - c0)
                            for j in range(nb):
                                fn = lambda e, c=c0 + j, j=j, comp=comp: e.transpose(
                                    ptq[:, j * 128:(j + 1) * 128], tm[:, c, comp * 128:(comp + 1) * 128], ident[:, :])
                                if j == 0 or j == nb - 1:
                                    S.op("pe", fn, [r_tm, r_id], [r_ptq])
                                else:
                                    S.pe_quiet(fn)
                            if (c0 // 8) % 2 == 0:
                                S.op("dve", lambda e, dstT=dstT, comp=comp, c0=c0, nb=nb: e.tensor_copy(
                                    out=dstT[:, comp, c0 * 128:(c0 + nb) * 128], in_=ptq[:, 0:nb * 128]), [r_ptq], [r_dst])
                            else:
                                S.op("act", lambda e, dstT=dstT, comp=comp, c0=c0, nb=nb: e.activation(
                                    out=dstT[:, comp, c0 * 128:(c0 + nb) * 128], in_=ptq[:, 0:nb * 128], func=AF.Copy),
                                    [r_ptq], [r_dst])
                vcol = 2 * D + h * 256
                S.dma("sp", lambda e, vcol=vcol: e.dma_start(
                    out=Vaug[:, :, 0:256], in_=qkv_d[:, vcol:vcol + 256].rearrange("(c p) d -> p c d", p=128)), [], [r_V])
                NKC = NTL + NTC

                def emit_scores(qb_, kc_, gi_):
                    sk_ = gi_ % 3
                    for comp in range(2):
                        S.op("pe", lambda e, sk_=sk_, comp=comp, kc_=kc_, q0_=qb_ * 256: e.matmul(
                            ps_s[sk_][:, comp * 256:(comp + 1) * 256], KT[:, comp, kc_ * 128:(kc_ + 1) * 128],
                            QT[:, comp, q0_:q0_ + 256], start=True, stop=True), [r_KT, r_QT], [r_ps[sk_]])

                def emit_exp_pv(kc_, gi_):
                    sk_ = gi_ % 3
                    S.op("act", lambda e, sk_=sk_: e.activation(out=PT[sk_][:, :], in_=ps_s[sk_][:, :], func=AF.Exp, scale=SCALE),
                         [r_ps[sk_]], [r_PT[sk_]])
                    for comp in range(2):
                        for qs in range(2):
                            pi = comp * 2 + qs
                            S.op("pe", lambda e, sk_=sk_, comp=comp, qs=qs, pi=pi, kc_=kc_: e.matmul(
                                po[pi][:, 0:257], PT[sk_][:, comp * 256 + qs * 128:comp * 256 + (qs + 1) * 128],
                                Vaug[:, kc_, :], start=(kc_ == 0), stop=(kc_ == NKC - 1)), [r_PT[sk_], r_V], [r_po[pi]])

                NQB = N // 256
                flat = [(qb_, kc_) for qb_ in range(NQB) for kc_ in range(NKC)]
                emit_scores(flat[0][0], flat[0][1], step)
                emit_scores(flat[1][0], flat[1][1], step + 1)
                for fi, (qb, kc) in enumerate(flat):
                    q0 = qb * 256
                    if fi + 2 < len(flat):
                        emit_scores(flat[fi + 2][0], flat[fi + 2][1], step + 2)
                    emit_exp_pv(kc, step)
                    step += 1
                    if kc != NKC - 1:
                        continue
                    for qs in range(2):
                        k2 = ei % 2; ei += 1
                        S.op("dve", lambda e, k2=k2, qs=qs: e.reciprocal(out=stt[k2][:, 0:1], in_=po[qs][:, 256:257]),
                             [r_po[qs]], [r_stt[k2]])
                        S.op("dve", lambda e, k2=k2, qs=qs: e.reciprocal(out=stt[k2][:, 1:2], in_=po[2 + qs][:, 256:257]),
                             [r_po[2 + qs]], [r_stt[k2]])
                        S.op("dve", lambda e, k2=k2: e.tensor_tensor(out=stt[k2][:, 1:2], in0=stt[k2][:, 1:2], in1=lamt[:, 5:6],
                                                                     op=ALU.mult), [r_stt[k2], r_lam], [r_stt[k2]])
                        S.op("dve", lambda e, k2=k2, qs=qs: e.tensor_scalar(out=o32[k2][:, :], in0=po[qs][:, 0:256],
                                                                            scalar1=stt[k2][:, 0:1], scalar2=None, op0=ALU.mult),
                             [r_po[qs], r_stt[k2]], [r_o32[k2]])
                        S.op("dve", lambda e, k2=k2, qs=qs: e.scalar_tensor_tensor(
                            out=o32[k2][:, :], in0=po[2 + qs][:, 0:256], scalar=stt[k2][:, 1:2], in1=o32[k2][:, :],
                            op0=ALU.mult, op1=ALU.add), [r_po[2 + qs], r_stt[k2], r_o32[k2]], [r_o32[k2]])
                        S.op("dve", lambda e, k2=k2: e.memset(stt[k2][:, 2:3], 0.0), [], [r_stt[k2]])
                        S.op("act", lambda e, k2=k2: e.activation(out=junk[:, :], in_=o32[k2][:, :], func=AF.Square,
                                                                  accum_out=stt[k2][:, 2:3]), [r_o32[k2], r_stt[k2]],
                             [r_junk, r_stt[k2]])
                        S.op("dve", lambda e, k2=k2: e.tensor_scalar(out=stt[k2][:, 3:4], in0=stt[k2][:, 2:3], scalar1=1.0 / 256,
                                                                     scalar2=SUBLN_EPS, op0=ALU.mult, op1=ALU.add),
                             [r_stt[k2]], [r_stt[k2]])
                        S.op("act", lambda e, k2=k2: e.activation(out=stt[k2][:, 3:4], in_=stt[k2][:, 3:4], func=AF.Sqrt),
                             [r_stt[k2]], [r_stt[k2]])
                        S.op("dve", lambda e, k2=k2: e.reciprocal(out=stt[k2][:, 3:4], in_=stt[k2][:, 3:4]), [r_stt[k2]], [r_stt[k2]])
                        S.op("dve", lambda e, k2=k2: e.scalar_tensor_tensor(out=obf[k2][:, :], in0=o32[k2][:, :],
                                                                            scalar=stt[k2][:, 3:4], in1=gsub[:, :],
                                                                            op0=ALU.mult, op1=ALU.mult),
                             [r_o32[k2], r_stt[k2], r_gsub], [r_obf[k2]])
                        for j in range(2):
                            S.op("pe", lambda e, k2=k2, j=j: e.transpose(pso[:, j * 128:(j + 1) * 128],
                                                                         obf[k2][:, j * 128:(j + 1) * 128], ident[:, :]),
                                 [r_obf[k2], r_id], [r_pso])
                        S.op("dve", lambda e, k2=k2: e.tensor_copy(out=osb[k2][:, :, :],
                                                                   in_=pso[:, 0:256].rearrange("p (j t) -> p j t", j=2)),
                             [r_pso], [r_osb[k2]])
                        t0 = q0 + qs * 128
                        S.dma("sp", lambda e, k2=k2, h=h, t0=t0: e.dma_start(out=oT_d[:, 2 * h:2 * h + 2, t0:t0 + 128],
                                                                             in_=osb[k2][:, :, :]), [r_osb[k2]], [])
        return phase_end("DA")


    offs_l = es.enter_context(nc.sbuf_tensor("offs_l", [128, NTL, NE], I32))
    offs_c = es.enter_context(nc.sbuf_tensor("offs_c", [128, NTC, NE], I32))
    gsel_l = es.enter_context(nc.sbuf_tensor("gsel_l", [128, NTL, NE], F32))
    gsel_c = es.enter_context(nc.sbuf_tensor("gsel_c", [128, NTC, NE], F32))
    r_offs, r_gsel = R("offs", "gsel")
    NROW_XG = NE * SLOTS

    def phase_E1(layer):
        sets = [(0, NTL, CAP, 0, offs_l, gsel_l)]
        if layer == 0:
            sets.append((N, NTC, CAPC, CAP, offs_c, gsel_c))
        with ExitStack() as ps:
            sb = lambda n, s, d: ps.enter_context(nc.sbuf_tensor(U(n), s, d))
            A = sb("e1_A", [128, NTL, NE], F32)
            cmp = sb("e1_cmp", [128, NTL, NE], F32)
            M = sb("e1_M", [128, NTL, NE], F32)
            s0 = sb("e1_s0", [128, NTL, NE], F32)
            s1 = sb("e1_s1", [128, NTL, NE], F32)
            cc = sb("e1_cc", [128, NTL, NE], F32)
            sm = {n: sb("e1_" + n, [128, NE], F32) for n in ("lo", "hi", "mid", "d1", "d2", "pred", "cnt", "erow")}
            ones = sb("e1_ones", [128, 128], F32)
            lst = sb("e1_lst", [128, 128], F32)
            ptot = ps.enter_context(nc.psum_tensor(U("e1_pt"), [128, 512], F32))
            pcc = [ps.enter_context(nc.psum_tensor(U("e1_pc%d" % i), [128, 512], F32)) for i in range(2)]
            pwi = [ps.enter_context(nc.psum_tensor(U("e1_pw%d" % i), [128, 512], F32)) for i in range(2)]
            r_A, r_cmp, r_M, r_s0, r_s1, r_cc, r_ones, r_lst, r_pt = R("A", "cmp", "M", "s0", "s1", "cc", "ones", "lst", "pt")
            r_pc = R("pc0", "pc1"); r_pw = R("pw0", "pw1")
            rs = {n: Res(n) for n in sm}
            S.op("dve", lambda e: e.memset(ones[:, :], 1.0), [], [r_ones])
            S.dma("sp", lambda e: e.dma_start(out=lst[:, :], in_=lstrict_in[:, :]), [], [r_lst])
            D_ = lambda fn, rd, wr_: S.op("dve", fn, rd, wr_)
            for (tok0, nt, cap, sbase, offs, gsel) in sets:
                Av = A[:, 0:nt, :]
                S.dma("sp", lambda e, Av=Av, tok0=tok0, nt=nt: e.dma_start(
                    out=Av, in_=aff_d[tok0:tok0 + nt * 128, :].rearrange("(c p) e -> p c e", p=128)), [], [r_A])
                D_(lambda e: e.memset(sm["lo"][:, :], 0.0), [], [rs["lo"]])
                D_(lambda e: e.memset(sm["hi"][:, :], 2.0), [], [rs["hi"]])
                for e_ in range(NE):
                    D_(lambda e, e_=e_, sbase=sbase: e.memset(sm["erow"][:, e_:e_ + 1], float(e_ * SLOTS + sbase)), [], [rs["erow"]])
                bc = lambda t, nt=nt: t[:, :].unsqueeze(1).to_broadcast([128, nt, NE])
                for itr in range(36):
                    D_(lambda e: e.tensor_tensor(out=sm["mid"][:, :], in0=sm["lo"][:, :], in1=sm["hi"][:, :], op=ALU.add),
                       [rs["lo"], rs["hi"]], [rs["mid"]])
                    D_(lambda e: e.tensor_scalar(out=sm["mid"][:, :], in0=sm["mid"][:, :], scalar1=0.5, scalar2=None, op0=ALU.mult),
                       [rs["mid"]], [rs["mid"]])
                    D_(lambda e, Av=Av, nt=nt, bc=bc: e.tensor_tensor(out=cmp[:, 0:nt, :], in0=Av, in1=bc(sm["mid"]), op=ALU.is_ge),
                       [r_A, rs["mid"]], [r_cmp])
                    D_(lambda e, nt=nt: e.tensor_reduce(out=sm["cnt"][:, :], in_=cmp[:, 0:nt, :].rearrange("p c e -> p e c"),
                                                        axis=AX.X, op=ALU.add), [r_cmp], [rs["cnt"]])
                    S.op("pe", lambda e: e.matmul(ptot[:, 0:NE], ones[:, :], sm["cnt"][:, :], start=True, stop=True),
                         [r_ones, rs["cnt"]], [r_pt])
                    D_(lambda e, cap=cap: e.tensor_scalar(out=sm["pred"][:, :], in0=ptot[:, 0:NE], scalar1=float(cap) - 0.5,
                                                          scalar2=None, op0=ALU.is_ge), [r_pt], [rs["pred"]])
                    D_(lambda e: e.tensor_tensor(out=sm["d1"][:, :], in0=sm["mid"][:, :], in1=sm["lo"][:, :], op=ALU.subtract),
                       [rs["mid"], rs["lo"]], [rs["d1"]])
                    D_(lambda e: e.tensor_tensor(out=sm["d1"][:, :], in0=sm["d1"][:, :], in1=sm["pred"][:, :], op=ALU.mult),
                       [rs["d1"], rs["pred"]], [rs["d1"]])
                    D_(lambda e: e.tensor_tensor(out=sm["d2"][:, :], in0=sm["hi"][:, :], in1=sm["mid"][:, :], op=ALU.subtract),
                       [rs["mid"], rs["hi"]], [rs["d2"]])
                    D_(lambda e: e.tensor_tensor(out=sm["d2"][:, :], in0=sm["d2"][:, :], in1=sm["pred"][:, :], op=ALU.mult),
                       [rs["d2"], rs["pred"]], [rs["d2"]])
                    D_(lambda e: e.tensor_tensor(out=sm["lo"][:, :], in0=sm["lo"][:, :], in1=sm["d1"][:, :], op=ALU.add),
                       [rs["lo"], rs["d1"]], [rs["lo"]])
                    D_(lambda e: e.tensor_tensor(out=sm["hi"][:, :], in0=sm["mid"][:, :], in1=sm["d2"][:, :], op=ALU.add),
                       [rs["mid"], rs["d2"]], [rs["hi"]])
                D_(lambda e, Av=Av, nt=nt, bc=bc: e.tensor_tensor(out=M[:, 0:nt, :], in0=Av, in1=bc(sm["lo"]), op=ALU.is_ge),
                   [r_A, rs["lo"]], [r_M])
                ncol = nt * NE
                Mf = M[:, :, :].rearrange("p c e -> p (c e)")
                nh = (ncol + 511) // 512
                for h in range(nh):
                    w_ = min(512, ncol - h * 512)
                    S.op("pe", lambda e, h=h, w_=w_: e.matmul(pcc[h][:, 0:w_], ones[:, :], Mf[:, h * 512:h * 512 + w_],
                                                             start=True, stop=True), [r_ones, r_M], [r_pc[h]])
                    S.op("pe", lambda e, h=h, w_=w_: e.matmul(pwi[h][:, 0:w_], lst[:, :], Mf[:, h * 512:h * 512 + w_],
                                                             start=True, stop=True), [r_lst, r_M], [r_pw[h]])
                    D_(lambda e, h=h, w_=w_: e.tensor_copy(out=cc[:, :, :].rearrange("p c e -> p (c e)")[:, h * 512:h * 512 + w_],
                                                          in_=pcc[h][:, 0:w_]), [r_pc[h]], [r_cc])
                D_(lambda e, nt=nt: e.tensor_copy(out=s0[:, 0:nt, :], in_=cc[:, 0:nt, :]), [r_cc], [r_s0])
                src, dst, r_src, r_dst = s0, s1, r_s0, r_s1
                d = 1
                while d < nt:
                    D_(lambda e, src=src, dst=dst, d=d, nt=nt: e.tensor_tensor(out=dst[:, d:nt, :], in0=src[:, d:nt, :],
                                                                             in1=src[:, 0:nt - d, :], op=ALU.add), [r_src], [r_dst])
                    D_(lambda e, src=src, dst=dst, d=d: e.tensor_copy(out=dst[:, 0:d, :], in_=src[:, 0:d, :]), [r_src], [r_dst])
                    src, dst, r_src, r_dst = dst, src, r_dst, r_src
                    d *= 2
                D_(lambda e, src=src, nt=nt: e.tensor_tensor(out=src[:, 0:nt, :], in0=src[:, 0:nt, :], in1=cc[:, 0:nt, :],
                                                            op=ALU.subtract), [r_src, r_cc], [r_src])
                srcf = src[:, :, :].rearrange("p c e -> p (c e)")
                for h in range(nh):
                    w_ = min(512, ncol - h * 512)
                    D_(lambda e, h=h, w_=w_, srcf=srcf: e.tensor_tensor(out=srcf[:, h * 512:h * 512 + w_],
                                                                       in0=srcf[:, h * 512:h * 512 + w_], in1=pwi[h][:, 0:w_],
                                                                       op=ALU.add), [r_src, r_pw[h]], [r_src])
                D_(lambda e, src=src, nt=nt, cap=cap: e.tensor_scalar(out=cmp[:, 0:nt, :], in0=src[:, 0:nt, :],
                                                                     scalar1=float(cap) - 0.5, scalar2=None, op0=ALU.is_lt),
                   [r_src], [r_cmp])
                D_(lambda e, nt=nt: e.tensor_tensor(out=M[:, 0:nt, :], in0=M[:, 0:nt, :], in1=cmp[:, 0:nt, :], op=ALU.mult),
                   [r_M, r_cmp], [r_M])
                D_(lambda e, gsel=gsel, Av=Av, nt=nt: e.tensor_tensor(out=gsel[:, :, :], in0=Av, in1=M[:, 0:nt, :], op=ALU.mult),
                   [r_A, r_M], [r_gsel])
                D_(lambda e, src=src, nt=nt, bc=bc: e.tensor_tensor(out=src[:, 0:nt, :], in0=src[:, 0:nt, :], in1=bc(sm["erow"]),
                                                                   op=ALU.add), [r_src, rs["erow"]], [r_src])
                D_(lambda e, src=src, nt=nt: e.tensor_scalar(out=src[:, 0:nt, :], in0=src[:, 0:nt, :], scalar1=-BIG, scalar2=None,
                                                            op0=ALU.add), [r_src], [r_src])
                D_(lambda e, src=src, nt=nt: e.tensor_tensor(out=src[:, 0:nt, :], in0=src[:, 0:nt, :], in1=M[:, 0:nt, :],
                                                            op=ALU.mult), [r_src, r_M], [r_src])
                D_(lambda e, src=src, nt=nt: e.tensor_scalar(out=src[:, 0:nt, :], in0=src[:, 0:nt, :], scalar1=BIG, scalar2=None,
                                                            op0=ALU.add), [r_src], [r_src])
                D_(lambda e, src=src, nt=nt, offs=offs: e.tensor_copy(out=offs[:, :, :], in_=src[:, 0:nt, :]), [r_src], [r_offs])
        return phase_end("E1_%d" % layer)

    def phase_E2(layer):
        sets = [(0, NTL, offs_l)]
        if layer == 0:
            sets.append((N, NTC, offs_c))
        with ExitStack() as ps:
            sb = lambda n, s, d: ps.enter_context(nc.sbuf_tensor(U(n), s, d))
            h2t = [sb("e2_h%d" % i, [128, D], BF16) for i in range(3)]
            r_h = R("h0", "h1", "h2")
            bcr = {}

            def mkreg(e):
                bcr["r"] = e.alloc_register(U("e2_bc"))
                return e.reg_mov(bcr["r"], NROW_XG - 1)
            S.raw("pool", mkreg)
            it = 0
            for (tok0, nt, offs) in sets:
                for c in range(nt):
                    k = it % 3; it += 1
                    t0 = tok0 + c * 128
                    S.dma("sp", lambda e, k=k, t0=t0: e.dma_start(out=h2t[k][:, :], in_=h2_d[t0:t0 + 128, :]), [], [r_h[k]])
                    for e_ in range(NE):
                        S.dma("pool", lambda e, k=k, c=c, e_=e_, offs=offs: e.indirect_dma_start(
                            out=xg_d[:, :], out_offset=bass.IndirectOffsetOnAxis(ap=offs[:, c, e_:e_ + 1], axis=0),
                            in_=h2t[k][:, :], in_offset=None, bounds_check=bcr["r"], oob_is_err=False),
                            [r_h[k], r_offs], [])
            S.raw("pool", lambda e: (e.free_register(bcr["r"]), None)[1])
        return phase_end("E2_%d" % layer)

    def phase_E3(layer):
        SL = SLOTS if layer == 0 else CAP
        stiles = [(i * 128, 128) for i in range(8)] + ([(CAP, CAPC)] if layer == 0 else [])
        chunks = [(0, 512), (512, 512)] + ([(CAP, CAPC)] if layer == 0 else [])
        with ExitStack() as ps:
            sb = lambda n, s, d: ps.enter_context(nc.sbuf_tensor(U(n), s, d))
            xgt = [sb("e3_x%d" % i, [128, D], BF16) for i in range(2)]
            xgT = sb("e3_xT", [128, KC, SLOTS], BF16)
            wgb = [sb("e3_wg%d" % i, [128, KC, 256], BF16) for i in range(2)]
            wub = [sb("e3_wu%d" % i, [128, KC, 256], BF16) for i in range(2)]
            aT = sb("e3_aT", [128, 8, SLOTS], BF16)
            wdb = [sb("e3_wd%d" % i, [128, 8, 512], BF16) for i in range(2)]
            sg = [sb("e3_sg%d" % i, [128, 512], F32) for i in range(2)]
            ysb = [sb("e3_y%d" % i, [128, 512], BF16) for i in range(2)]
            ident = sb("e3_id", [128, 128], BF16)
            ptr = [ps.enter_context(nc.psum_tensor(U("e3_pt%d" % i), [128, 1024], BF16)) for i in range(2)]
            pg = [ps.enter_context(nc.psum_tensor(U("e3_pg%d" % i), [128, 512], F32)) for i in range(2)]
            pu = [ps.enter_context(nc.psum_tensor(U("e3_pu%d" % i), [128, 512], F32)) for i in range(2)]
            py = [ps.enter_context(nc.psum_tensor(U("e3_py%d" % i), [128, 512], F32)) for i in range(2)]
            r_x = R("x0", "x1"); r_wg = R("wg0", "wg1"); r_wu = R("wu0", "wu1"); r_wd = R("wd0", "wd1")
            r_sg = R("sg0", "sg1"); r_y = R("y0", "y1"); r_pt = R("pt0", "pt1"); r_pg = R("pg0", "pg1")
            r_pu = R("pu0", "pu1"); r_py = R("py0", "py1")
            r_xT, r_aT, r_id = R("xT", "aT", "id")
            S.dma("pool", lambda e: e.dma_start(out=ident[:, :], in_=ident_in[:, :]), [], [r_id])
            xi = 0; ti = 0; wi = 0; gi = 0; di = 0; yi = 0
            for ex in range(NE):
                row0 = ex * SLOTS
                for (s0_, P_) in stiles:
                    k = xi % 2; xi += 1
                    S.dma("sp", lambda e, k=k, row0=row0, s0_=s0_, P_=P_: e.dma_start(
                        out=xgt[k][0:P_, :], in_=xg_d[row0 + s0_:row0 + s0_ + P_, :]), [], [r_x[k]])
                    for g in range(4):
                        tk = ti % 2; ti += 1
                        for j in range(8):
                            kc = g * 8 + j
                            fn = lambda e, k=k, tk=tk, j=j, kc=kc, P_=P_: e.transpose(
                                ptr[tk][:, j * P_:(j + 1) * P_], xgt[k][0:P_, kc * 128:(kc + 1) * 128], ident[0:P_, 0:P_])
                            if j == 0 or j == 7:
                                S.op("pe", fn, [r_x[k], r_id], [r_pt[tk]])
                            else:
                                S.pe_quiet(fn)
                        eng = "act" if g % 2 == 0 else "dve"
                        o_ = xgT[:, g * 8:(g + 1) * 8, s0_:s0_ + P_]
                        i_ = ptr[tk][:, 0:8 * P_].rearrange("p (j t) -> p j t", j=8)
                        if eng == "act":
                            S.op("act", lambda e, o_=o_, i_=i_: e.activation(out=o_, in_=i_, func=AF.Copy), [r_pt[tk]], [r_xT])
                        else:
                            S.op("dve", lambda e, o_=o_, i_=i_: e.tensor_copy(out=o_, in_=i_), [r_pt[tk]], [r_xT])
                for fb in range(4):
                    wk = wi % 2; wi += 1
                    S.dma("pool", lambda e, wk=wk, ex=ex, fb=fb: e.dma_start(
                        out=wgb[wk][:, :, :], in_=wg[layer, ex, :, fb * 256:(fb + 1) * 256].rearrange("(kc p) n -> p kc n", p=128)),
                        [], [r_wg[wk]])
                    S.dma("pool", lambda e, wk=wk, ex=ex, fb=fb: e.dma_start(
                        out=wub[wk][:, :, :], in_=wu[layer, ex, :, fb * 256:(fb + 1) * 256].rearrange("(kc p) n -> p kc n", p=128)),
                        [], [r_wu[wk]])
                    for sub in range(2):
                        f8 = fb * 2 + sub
                        for (c0, cn) in chunks:
                            g_ = gi % 2; gi += 1
                            mm_group(pg[g_][:, 0:cn], [(wgb[wk][:, kc, sub * 128:(sub + 1) * 128], xgT[:, kc, c0:c0 + cn])
                                                       for kc in range(KC)], [r_wg[wk], r_xT], r_pg[g_])
                            mm_group(pu[g_][:, 0:cn], [(wub[wk][:, kc, sub * 128:(sub + 1) * 128], xgT[:, kc, c0:c0 + cn])
                                                       for kc in range(KC)], [r_wu[wk], r_xT], r_pu[g_])
                            S.op("act", lambda e, g_=g_, cn=cn: e.activation(out=sg[g_][:, 0:cn], in_=pg[g_][:, 0:cn], func=AF.Silu),
                                 [r_pg[g_]], [r_sg[g_]])
                            S.op("dve", lambda e, g_=g_, cn=cn, c0=c0, f8=f8: e.tensor_tensor(
                                out=aT[:, f8, c0:c0 + cn], in0=sg[g_][:, 0:cn], in1=pu[g_][:, 0:cn], op=ALU.mult),
                                [r_sg[g_], r_pu[g_]], [r_aT])
                for nb in range(8):
                    dk = di % 2; di += 1
                    S.dma("pool", lambda e, dk=dk, ex=ex, nb=nb: e.dma_start(
                        out=wdb[dk][:, :, :], in_=wd[layer, ex, :, nb * 512:(nb + 1) * 512].rearrange("(fc p) n -> p fc n", p=128)),
                        [], [r_wd[dk]])
                    for (s0_, P_) in stiles:
                        yk = yi % 2; yi += 1
                        mm_group(py[yk][0:P_, :], [(aT[:, fc, s0_:s0_ + P_], wdb[dk][:, fc, :]) for fc in range(8)],
                                 [r_aT, r_wd[dk]], r_py[yk])
                        if yk == 0:
                            S.op("act", lambda e, yk=yk, P_=P_: e.activation(out=ysb[yk][0:P_, :], in_=py[yk][0:P_, :], func=AF.Copy),
                                 [r_py[yk]], [r_y[yk]])
                        else:
                            S.op("dve", lambda e, yk=yk, P_=P_: e.tensor_copy(out=ysb[yk][0:P_, :], in_=py[yk][0:P_, :]),
                                 [r_py[yk]], [r_y[yk]])
                        S.dma("sp", lambda e, yk=yk, row0=row0, s0_=s0_, P_=P_, nb=nb: e.dma_start(
                            out=y_d[row0 + s0_:row0 + s0_ + P_, nb * 512:(nb + 1) * 512], in_=ysb[yk][0:P_, :]), [r_y[yk]], [])
        return phase_end("E3_%d" % layer)

    def phase_T3(layer):
        last = layer == 1
        sets = [(0, NTL, 0, offs_l, gsel_l)]
        if not last:
            sets.append((N, NTC, 1, offs_c, gsel_c))
        with ExitStack() as ps:
            sb = lambda n, s, d: ps.enter_context(nc.sbuf_tensor(U(n), s, d))
            x1t = [sb("t3_x%d" % i, [128, D], F32) for i in range(2)]
            macc = [sb("t3_m%d" % i, [128, D], F32) for i in range(2)]
            G = [sb("t3_g%d" % i, [128, D], BF16) for i in range(4)]
            g5 = sb("t3_g5", [128, D], F32)
            r_x = R("x0", "x1"); r_m = R("m0", "m1"); r_G = R("G0", "G1", "G2", "G3"); r_g5, = R("g5")
            if last:
                fg = sb("t3_fg", [128, D], F32)
                junk = sb("t3_junk", [128, D], BF16)
                st = [sb("t3_st%d" % i, [128, 2], F32) for i in range(2)]
                r_fg, r_junk = R("fg", "junk"); r_st = R("st0", "st1")
                load_rows_bcast("sp", fg[:, :], fing[0:1, :], r_fg)
            for i in range(4):
                S.op("pool", lambda e, i=i: e.memset(G[i][:, :], 0.0), [], [r_G[i]])
            bcr = {}

            def mkreg(e):
                bcr["r"] = e.alloc_register(U("t3_bc"))
                return e.reg_mov(bcr["r"], NROW_XG - 1)
            S.raw("pool", mkreg)
            it = 0; gi = 0
            for (tok0, nt, row, offs, gsel) in sets:
                load_rows_bcast("sp", g5[:, :], mod_d[layer, row:row + 1, 5 * D:6 * D], r_g5)
                for c in range(nt):
                    k = it % 2; it += 1
                    t0 = tok0 + c * 128
                    S.dma("sp", lambda e, k=k, t0=t0: e.dma_start(out=x1t[k][:, :], in_=x1_d[t0:t0 + 128, :]), [], [r_x[k]])
                    for e_ in range(NE):
                        g = gi % 4; gi += 1
                        S.dma("pool", lambda e, g=g, c=c, e_=e_, offs=offs: e.indirect_dma_start(
                            out=G[g][:, :], out_offset=None, in_=y_d[:, :],
                            in_offset=bass.IndirectOffsetOnAxis(ap=offs[:, c, e_:e_ + 1], axis=0),
                            bounds_check=bcr["r"], oob_is_err=False), [r_offs], [r_G[g]])
                        if e_ == 0:
                            S.op("dve", lambda e, g=g, k=k, c=c, gsel=gsel: e.tensor_scalar(
                                out=macc[k][:, :], in0=G[g][:, :], scalar1=gsel[:, c, 0:1], scalar2=None, op0=ALU.mult),
                                [r_G[g], r_gsel], [r_m[k]])
                        else:
                            S.op("dve", lambda e, g=g, k=k, c=c, e_=e_, gsel=gsel: e.scalar_tensor_tensor(
                                out=macc[k][:, :], in0=G[g][:, :], scalar=gsel[:, c, e_:e_ + 1], in1=macc[k][:, :],
                                op0=ALU.mult, op1=ALU.add), [r_G[g], r_gsel, r_m[k]], [r_m[k]])
                    S.op("dve", lambda e, k=k: e.tensor_tensor(out=macc[k][:, :], in0=macc[k][:, :], in1=g5[:, :], op=ALU.mult),
                         [r_m[k], r_g5], [r_m[k]])
                    S.op("pool", lambda e, k=k: e.tensor_tensor(out=x1t[k][:, :], in0=x1t[k][:, :], in1=macc[k][:, :], op=ALU.add),
                         [r_x[k], r_m[k]], [r_x[k]])
                    if not last:
                        S.dma("sp", lambda e, k=k, t0=t0: e.dma_start(out=x2_d[t0:t0 + 128, :], in_=x1t[k][:, :]), [r_x[k]], [])
                    else:
                        S.op("dve", lambda e, k=k: e.memset(st[k][:, 0:1], 0.0), [], [r_st[k]])
                        S.op("act", lambda e, k=k: e.activation(out=junk[:, :], in_=x1t[k][:, :], func=AF.Square,
                                                                accum_out=st[k][:, 0:1]), [r_x[k], r_st[k]], [r_junk, r_st[k]])
                        S.op("dve", lambda e, k=k: e.tensor_scalar(out=st[k][:, 1:2], in0=st[k][:, 0:1], scalar1=1.0 / D,
                                                                   scalar2=EPS, op0=ALU.mult, op1=ALU.add), [r_st[k]], [r_st[k]])
                        S.op("act", lambda e, k=k: e.activation(out=st[k][:, 1:2], in_=st[k][:, 1:2], func=AF.Sqrt),
                             [r_st[k]], [r_st[k]])
                        S.op("dve", lambda e, k=k: e.reciprocal(out=st[k][:, 1:2], in_=st[k][:, 1:2]), [r_st[k]], [r_st[k]])
                        S.op("dve", lambda e, k=k: e.scalar_tensor_tensor(out=x1t[k][:, :], in0=x1t[k][:, :], scalar=st[k][:, 1:2],
                                                                          in1=fg[:, :], op0=ALU.mult, op1=ALU.mult),
                             [r_x[k], r_st[k], r_fg], [r_x[k]])
                        S.dma("sp", lambda e, k=k, t0=t0: e.dma_start(out=out_d[t0:t0 + 128, :], in_=x1t[k][:, :]), [r_x[k]], [])
            S.raw("pool", lambda e: (e.free_register(bcr["r"]), None)[1])
        return phase_end("T3_%d" % layer)

    plan = [("A", phase_A), ("T1_0", lambda: phase_T1(0)), ("H1_0", lambda: phase_H1(0)), ("N", phase_N),
            ("T2a_0", lambda: phase_T2a(0)), ("T2b_0", lambda: phase_T2b(0)),
            ("E1_0", lambda: phase_E1(0)), ("E2_0", lambda: phase_E2(0)), ("E3_0", lambda: phase_E3(0)),
            ("T3_0", lambda: phase_T3(0)),
            ("T1_1", lambda: phase_T1(1)), ("H1_1", lambda: phase_H1(1)), ("DA", phase_DA),
            ("T2a_1", lambda: phase_T2a(1)), ("T2b_1", lambda: phase_T2b(1)),
            ("E1_1", lambda: phase_E1(1)), ("E2_1", lambda: phase_E2(1)), ("E3_1", lambda: phase_E3(1)),
            ("T3_1", lambda: phase_T3(1))]
    for name, fn in plan:
        if phases is not None and name not in phases:
            continue
        if fn():
            break

    es.close()
    nc._declared_inputs = list(declared.keys())
    return nc, list(declared.keys())


def _host_constants():
    ident = np.eye(128, dtype=np.float32)
    lstrict = np.triu(np.ones((128, 128), np.float32), 1)
    qc = np.arange(GRID_W)
    col_start = np.clip(qc - 8, 0, GRID_W - 16)
    col_mask = (qc[None, :] >= col_start[:, None]) & (qc[None, :] < col_start[:, None] + 16)
    nmask = np.where(col_mask, 0.0, -1e30).astype(np.float32)
    col_idx = np.clip(qc[None, :] - qc[:, None] + 15, 0, 30)
    t = np.arange(N)
    row = (t // GRID_W).astype(np.float32)
    col = (t % GRID_W).astype(np.float32)
    freq = (10000.0 ** (-np.arange(32, dtype=np.float32) / 32)).astype(np.float32)
    ang = np.concatenate([row[:, None] * freq, col[:, None] * freq], axis=-1).astype(np.float32)
    return dict(ident=ident, lstrict=lstrict, nmask=nmask, col_idx=col_idx,
                rope_cos=np.cos(ang).astype(np.float32), rope_sin=np.sin(ang).astype(np.float32))


def make_in_maps(inp):
    cst = _host_constants()
    f = lambda a: np.ascontiguousarray(np.asarray(a, dtype=np.float32))
    rpb = f(inp["na_rpb"])[0]
    nb = rpb[:, :, cst["col_idx"]]
    nb = np.ascontiguousarray(nb.transpose(0, 2, 1, 3)).reshape(32, 64, 15 * 64)
    lam = np.concatenate([f(inp[k]) for k in ("da_lambda_q1", "da_lambda_k1", "da_lambda_q2", "da_lambda_k2")], 0)
    shared = dict(
        ada_w=f(inp["ada_w"]), ada_b=f(inp["ada_b"]), norm1_g=f(inp["norm1_g"]), norm2_g=f(inp["norm2_g"]),
        final_g=f(inp["final_g"]).reshape(1, D), na_w_qkv=f(inp["na_w_qkv"])[0], da_w_qkv=f(inp["da_w_qkv"])[0],
        na_w_o=f(inp["na_w_o"])[0], da_w_o=f(inp["da_w_o"])[0], nbias=nb, nmask=cst["nmask"], lam=lam,
        subln_g=f(inp["da_subln_g"]).reshape(1, 256), w_router=f(inp["moe_w_router"]), w_gate=f(inp["moe_w_gate"]),
        w_up=f(inp["moe_w_up"]), w_down=f(inp["moe_w_down"]), ident=cst["ident"], lstrict=cst["lstrict"],
        rope_cos=cst["rope_cos"], rope_sin=cst["rope_sin"])
    maps = []
    x = f(inp["x"]); c = f(inp["c"]); ctx = f(inp["ctx"]); cc = f(inp["c_ctx"])
    for b in range(2):
        cv = np.stack([c[b], cc], axis=-1)
        cT = np.ascontiguousarray(cv.reshape(KC, 128, 2).transpose(1, 0, 2))
        m = dict(shared)
        m.update(x=x[b], ctx=ctx[b], cT=cT)
        maps.append(m)
    return maps


def kernel(**inputs):
    nc, names = build_program()
    maps = [{k: m[k] for k in names} for m in make_in_maps(inputs)]
    res = run_bass_kernel_spmd(nc, maps, core_ids=[0, 1])
    return np.stack([np.asarray(res.results[b]["out"], dtype=np.float32) for b in range(2)], 0)
```
